# Optimizing a Trainium2 kernel written in Bass

```python
import jax, jax.numpy as jnp
from jax import lax
import numpy as np

D_MODEL = 1024
BATCH = 4
SEQ = 8192
DEPTH = 1
DEC_BATCH = 128
DEC_SEQ = 1
PAST_LEN = 8192
PAGE_SIZE = 128

HEAD_DIM = 64
N_HEADS_A = D_MODEL // 128
N_HEADS_B = D_MODEL // 128
WIDTH_A = N_HEADS_A * HEAD_DIM
WIDTH_B = N_HEADS_B * HEAD_DIM
MIX_WIDTH = WIDTH_A + WIDTH_B
CHUNK = 128
DILATED_CONFIGS = ((128, 1), (512, 4), (2048, 16))
MAX_WINDOW = 2048
N_GROUPS = 4
EXPERTS_PER_GROUP = 8
TOP_K_INNER = 2
D_EXPERT = D_MODEL // 4
EPS = 1e-6

kernel_name = 'hymba_gmlp_dilated_hiermoe_step'


def rmsnorm(x, g):
    xf = x.astype(jnp.float32)
    r = lax.rsqrt(jnp.mean(xf * xf, axis=-1, keepdims=True) + EPS)
    return (xf * r * g.astype(jnp.float32)).astype(x.dtype)


def mixer_inputs(x, norm_g, w_in, q_gain, k_gain):
    B, T, _ = x.shape
    z = jnp.einsum('btd,de->bte', rmsnorm(x, norm_g), w_in)
    q, k, v, ua, va = jnp.split(z, [WIDTH_B, 2 * WIDTH_B, 3 * WIDTH_B, 3 * WIDTH_B + WIDTH_A], axis=-1)
    hb = lambda t: t.reshape(B, T, N_HEADS_B, HEAD_DIM)
    ha = lambda t: t.reshape(B, T, N_HEADS_A, HEAD_DIM)
    q = rmsnorm(hb(q), q_gain)
    k = rmsnorm(hb(k), k_gain)
    return q, k, hb(v), ha(ua), ha(va)


def chunk_gating(ua, va, v_gain, w_s, b_s):
    B, T = ua.shape[:2]
    u = jax.nn.gelu(ua)
    v = rmsnorm(jax.nn.gelu(va), v_gain)
    nc = -(-T // CHUNK)
    vp = jnp.pad(v, ((0, 0), (0, nc * CHUNK - T), (0, 0), (0, 0)))
    vp = vp.reshape(B, nc, CHUNK, N_HEADS_A, HEAD_DIM)
    mixed = jnp.einsum('hqk,bnkhd->bnqhd', jnp.tril(w_s), vp) + b_s.T[None, None, :, :, None]
    mixed = mixed.reshape(B, nc * CHUNK, N_HEADS_A, HEAD_DIM)[:, :T]
    return u * mixed, v


def band_attention(q, k, v, dil, n):
    B, T, H, Dh = q.shape
    L = T // dil
    nb = -(-L // n)
    pad = nb * n - L

    def blocks(x):
        x = x.astype(jnp.float32).reshape(B, L, dil, H, Dh)
        x = jnp.pad(x, ((0, 0), (0, pad), (0, 0), (0, 0), (0, 0)))
        return x.reshape(B, nb, n, dil, H, Dh)

    def band(xb):
        prev = jnp.pad(xb, ((0, 0), (1, 0), (0, 0), (0, 0), (0, 0), (0, 0)))[:, :nb]
        return jnp.concatenate([prev, xb], axis=2)

    qb = blocks(q)
    kk, vv = band(blocks(k)), band(blocks(v))
    s = jnp.einsum('bnqrhd,bnkrhd->bnrhqk', qb, kk) * HEAD_DIM ** -0.5
    qi = jnp.arange(n)[:, None]
    ki = jnp.arange(2 * n)[None, :]
    dist = qi + n - ki
    valid = (dist >= 0) & (dist <= n) & ((jnp.arange(nb)[:, None, None] > 0) | (ki >= n)[None])
    s = jnp.where(valid[None, :, None, None], s, -jnp.inf)
    m = jnp.max(s, axis=-1, keepdims=True)
    e = jnp.exp(s - m)
    den = jnp.sum(e, axis=-1, keepdims=True)
    o = jnp.einsum('bnrhqk,bnkrhd->bnqrhd', e / den, vv)
    lse = (m + jnp.log(den))[..., 0]
    o = o.reshape(B, nb * n, dil, H, Dh)[:, :L].reshape(B, T, H, Dh)
    lse = lse.transpose(0, 1, 4, 2, 3).reshape(B, nb * n, dil, H)[:, :L].reshape(B, T, H)
    return o, lse


def gather_attention(q, k_all, v_all, offset, dil, n):
    S = q.shape[1]
    idx = offset + jnp.arange(S)[:, None] - dil * jnp.arange(n + 1)[None, :]
    valid = idx >= 0
    idx = jnp.maximum(idx, 0)
    kg = k_all[:, idx].astype(jnp.float32)
    vg = v_all[:, idx].astype(jnp.float32)
    s = jnp.einsum('bshd,bsjhd->bshj', q.astype(jnp.float32), kg) * HEAD_DIM ** -0.5
    s = jnp.where(valid[None, :, None, :], s, -jnp.inf)
    m = jnp.max(s, axis=-1, keepdims=True)
    e = jnp.exp(s - m)
    den = jnp.sum(e, axis=-1, keepdims=True)
    o = jnp.einsum('bshj,bsjhd->bshd', e / den, vg)
    return o, (m + jnp.log(den))[..., 0]


def merge_dilations(outs, lses, dtype):
    w = jax.nn.softmax(jnp.stack(lses, 0), axis=0)
    return jnp.einsum('cbth,cbthd->bthd', w, jnp.stack(outs, 0)).astype(dtype)


def merge_out(x, o_a, o_b, g_a, g_b, w_out):
    B, T, _ = x.shape
    ya = rmsnorm(o_a.reshape(B, T, WIDTH_A), g_a)
    yb = rmsnorm(o_b.reshape(B, T, WIDTH_B), g_b)
    return x + jnp.einsum('bte,ed->btd', jnp.concatenate([ya, yb], axis=-1), w_out)


def hier_moe(h, norm_g, w_r1, b_r1, w_r2, b_r2, w_up, w_gate, w_down):
    hn = rmsnorm(h, norm_g)
    lg = jnp.einsum('btd,dg->btg', hn, w_r1).astype(jnp.float32) + b_r1.astype(jnp.float32)
    g_star = jnp.argmax(lg, axis=-1)
    p_star = jnp.take_along_axis(jax.nn.softmax(lg, axis=-1), g_star[..., None], axis=-1)
    lf = jnp.einsum('btd,gde->btge', hn, w_r2).astype(jnp.float32) + b_r2.astype(jnp.float32)
    lf_sel = jnp.take_along_axis(lf, g_star[..., None, None], axis=2)[:, :, 0]
    top_v, top_i = lax.top_k(lf_sel, TOP_K_INNER)
    w_top = jax.nn.softmax(top_v, axis=-1) * p_star
    gate_e = jnp.sum(jax.nn.one_hot(top_i, EXPERTS_PER_GROUP, dtype=jnp.float32) * w_top[..., None], axis=-2)
    gate = (jax.nn.one_hot(g_star, N_GROUPS, dtype=jnp.float32)[..., None] * gate_e[..., None, :]).astype(hn.dtype)
    out = jnp.zeros_like(h)
    for g in range(N_GROUPS):
        a = jnp.einsum('btd,edf->btef', hn, w_gate[g])
        b = jnp.einsum('btd,edf->btef', hn, w_up[g])
        hid = jax.nn.silu(a) * b * gate[:, :, g, :, None]
        out = out + jnp.einsum('btef,efd->btd', hid, w_down[g])
    return h + out


def setup_inputs(seed: int = 0) -> dict:
    key = jax.random.key(seed)
    ks = jax.random.split(key, 24)
    f32 = jnp.float32
    nrm = lambda k, shape, scale: jax.random.normal(k, shape, f32) * scale
    gain = lambda k, shape: 1.0 + 0.1 * jax.random.normal(k, shape, f32)
    wb = min(MAX_WINDOW, PAST_LEN)
    return {
        'x_prompt': nrm(ks[0], (BATCH, SEQ, D_MODEL), 1.0),
        'x_sample': nrm(ks[1], (DEC_BATCH, DEC_SEQ, D_MODEL), 1.0),
        'cache_k': nrm(ks[2], (DEPTH, DEC_BATCH, wb, N_HEADS_B, HEAD_DIM), 1.0),
        'cache_v': nrm(ks[3], (DEPTH, DEC_BATCH, wb, N_HEADS_B, HEAD_DIM), 1.0),
        'norm1_g': gain(ks[4], (DEPTH, D_MODEL)),
        'w_in': nrm(ks[5], (DEPTH, D_MODEL, 3 * WIDTH_B + 2 * WIDTH_A), D_MODEL ** -0.5),
        'q_gain': gain(ks[6], (DEPTH, N_HEADS_B, HEAD_DIM)),
        'k_gain': gain(ks[7], (DEPTH, N_HEADS_B, HEAD_DIM)),
        'v_gain': gain(ks[8], (DEPTH, N_HEADS_A, HEAD_DIM)),
        'w_spatial': nrm(ks[9], (DEPTH, N_HEADS_A, CHUNK, CHUNK), CHUNK ** -0.5),
        'b_spatial': gain(ks[10], (DEPTH, N_HEADS_A, CHUNK)),
        'out_gain_a': gain(ks[11], (DEPTH, WIDTH_A)),
        'out_gain_b': gain(ks[12], (DEPTH, WIDTH_B)),
        'w_out': nrm(ks[13], (DEPTH, MIX_WIDTH, D_MODEL), MIX_WIDTH ** -0.5),
        'norm2_g': gain(ks[14], (DEPTH, D_MODEL)),
        'w_router1': nrm(ks[15], (DEPTH, D_MODEL, N_GROUPS), D_MODEL ** -0.5),
        'b_router1': nrm(ks[16], (DEPTH, N_GROUPS), 0.01),
        'w_router2': nrm(ks[17], (DEPTH, N_GROUPS, D_MODEL, EXPERTS_PER_GROUP), D_MODEL ** -0.5),
        'b_router2': nrm(ks[18], (DEPTH, N_GROUPS, EXPERTS_PER_GROUP), 0.01),
        'w_up': nrm(ks[19], (DEPTH, N_GROUPS, EXPERTS_PER_GROUP, D_MODEL, D_EXPERT), D_MODEL ** -0.5),
        'w_gate': nrm(ks[20], (DEPTH, N_GROUPS, EXPERTS_PER_GROUP, D_MODEL, D_EXPERT), D_MODEL ** -0.5),
        'w_down': nrm(ks[21], (DEPTH, N_GROUPS, EXPERTS_PER_GROUP, D_EXPERT, D_MODEL), D_EXPERT ** -0.5),
    }


def reference(x_prompt, x_sample, cache_k, cache_v, norm1_g, w_in, q_gain, k_gain, v_gain,
              w_spatial, b_spatial, out_gain_a, out_gain_b, w_out, norm2_g, w_router1, b_router1,
              w_router2, b_router2, w_up, w_gate, w_down):
    yp, ys = x_prompt, x_sample
    wb = cache_k.shape[2]
    nkp, nvp, nks, nvs, nva = [], [], [], [], []
    for l in range(DEPTH):
        moe = lambda h: hier_moe(h, norm2_g[l], w_router1[l], b_router1[l], w_router2[l], b_router2[l],
                                 w_up[l], w_gate[l], w_down[l])
        q, k, v, ua, va = mixer_inputs(yp, norm1_g[l], w_in[l], q_gain[l], k_gain[l])
        res = [band_attention(q, k, v, d, w // d) for (w, d) in DILATED_CONFIGS]
        o_b = merge_dilations([r[0] for r in res], [r[1] for r in res], q.dtype)
        o_a, _ = chunk_gating(ua, va, v_gain[l], w_spatial[l], b_spatial[l])
        nkp.append(k[:, -MAX_WINDOW:])
        nvp.append(v[:, -MAX_WINDOW:])
        yp = moe(merge_out(yp, o_a, o_b, out_gain_a[l], out_gain_b[l], w_out[l]))
        q, k, v, ua, va = mixer_inputs(ys, norm1_g[l], w_in[l], q_gain[l], k_gain[l])
        k_all = jnp.concatenate([cache_k[l].astype(k.dtype), k], axis=1)
        v_all = jnp.concatenate([cache_v[l].astype(v.dtype), v], axis=1)
        res = [gather_attention(q, k_all, v_all, wb, d, w // d) for (w, d) in DILATED_CONFIGS]
        o_b = merge_dilations([r[0] for r in res], [r[1] for r in res], q.dtype)
        o_a, v_rows = chunk_gating(ua, va, v_gain[l], w_spatial[l], b_spatial[l])
        nks.append(k)
        nvs.append(v)
        nva.append(v_rows)
        ys = moe(merge_out(ys, o_a, o_b, out_gain_a[l], out_gain_b[l], w_out[l]))
    new_k_prompt = jnp.stack(nkp, 0)
    new_v_prompt = jnp.stack(nvp, 0)
    new_k_sample = jnp.stack(nks, 0)
    new_v_sample = jnp.stack(nvs, 0)
    new_chunk_v_sample = jnp.stack(nva, 0)
    return (yp, ys, new_k_prompt, new_v_prompt, new_k_sample, new_v_sample, new_chunk_v_sample)
```

```python
import numpy as np
import ml_dtypes
import concourse.bass as bass
import concourse.mybir as mybir
from concourse.bass_utils import run_bass_kernel_spmd

F32 = mybir.dt.float32
BF16 = mybir.dt.bfloat16
U8 = mybir.dt.uint8
ALU = mybir.AluOpType
AF = mybir.ActivationFunctionType
AX = mybir.AxisListType

D = 1024
NCORES = 8
NHALO = 16
NMAIN = 32
RING = 20
NOFF = 17
NSAMP = 16
NTILES = NMAIN + 1
EPS = 1e-6
NDS = 24
GROUPS = [(0, 11), (11, 22), (22, 33)]


class Sched:
    def __init__(self, nc):
        self.nc = nc
        self.ce = ['pe', 'act', 'dve', 'pool']
        self.prog = {e: [] for e in self.ce + ['sp']}
        self.sem = {e: nc.alloc_semaphore("s_" + e) for e in self.ce}
        self.cnt = {e: 0 for e in self.ce}
        self.dsem = [nc.alloc_semaphore("d%d" % i) for i in range(NDS)]
        self.dcnt = [0] * NDS
        self.dnext = 0
        self.waited = {}
        self.lastw = {}
        self.readers = {}

    def _semh(self, sk):
        return self.sem[sk[1]] if sk[0] == 'e' else self.dsem[sk[1]]

    def _deps(self, e, reads, writes):
        need = {}

        def add(tok):
            if tok is None:
                return
            sk, v, te = tok
            if te == e and e == 'pe':
                return
            if v > need.get(sk, 0):
                need[sk] = v
        for k in reads:
            add(self.lastw.get(k))
        for k in writes:
            add(self.lastw.get(k))
            for sk, (v, te) in self.readers.get(k, {}).items():
                add((sk, v, te))
        out = []
        for sk, v in need.items():
            if self.waited.get((e, sk), 0) >= v:
                continue
            self.waited[(e, sk)] = v
            out.append((self._semh(sk), v))
        return out

    def _commit(self, tok, reads, writes):
        sk, v, te = tok
        for k in reads:
            self.readers.setdefault(k, {})[sk] = (v, te)
        for k in writes:
            self.lastw[k] = tok
            self.readers[k] = {}

    def op(self, e, fn, reads=(), writes=()):
        waits = self._deps(e, reads, writes)
        self.cnt[e] += 1
        tok = (('e', e), self.cnt[e], e)
        sem = self.sem[e]

        def emit(eng):
            for s, v in waits:
                eng.wait_ge(s, v)
            fn(eng).then_inc(sem, 1)
        self.prog[e].append(emit)
        self._commit(tok, reads, writes)

    def dma(self, out, in_, reads=(), writes=(), q='sp'):
        waits = self._deps(q, reads, writes)
        i = self.dnext
        self.dnext = (i + 1) % NDS
        prev = self.dcnt[i]
        if prev > 0 and self.waited.get((q, ('d', i)), 0) < prev:
            waits.append((self.dsem[i], prev))
            self.waited[(q, ('d', i))] = prev
        self.dcnt[i] += 16
        tok = (('d', i), self.dcnt[i], 'dma')
        sem = self.dsem[i]

        def emit(eng):
            for s, v in waits:
                eng.wait_ge(s, v)
            eng.dma_start(out=out, in_=in_).then_inc(sem, 16)
        self.prog[q].append(emit)
        self._commit(tok, reads, writes)

    def barrier(self):
        allw = [(('e', e), self.cnt[e]) for e in self.ce if self.cnt[e] > 0]
        allw += [(('d', i), self.dcnt[i]) for i in range(NDS) if self.dcnt[i] > 0]
        for e in self.ce + ['sp']:
            waits = []
            for sk, v in allw:
                if sk == ('e', e) and e == 'pe':
                    continue
                if self.waited.get((e, sk), 0) >= v:
                    continue
                self.waited[(e, sk)] = v
                waits.append((self._semh(sk), v))

            def emit(eng, waits=waits):
                for s, v in waits:
                    eng.wait_ge(s, v)
            self.prog[e].append(emit)
        self.lastw = {}
        self.readers = {}

    def finish(self):
        self.barrier()
        nc = self.nc
        prog = self.prog
        with nc.Block() as block:
            @block.sync
            def _(eng):
                for f in prog['sp']:
                    f(eng)

            @block.tensor
            def _(eng):
                for f in prog['pe']:
                    f(eng)

            @block.scalar
            def _(eng):
                for f in prog['act']:
                    f(eng)

            @block.vector
            def _(eng):
                for f in prog['dve']:
                    f(eng)

            @block.gpsimd
            def _(eng):
                for f in prog['pool']:
                    f(eng)


class Arena:
    def __init__(self, nc, nbytes):
        self.ap = nc.alloc_sbuf_tensor("arena", [128, nbytes], U8).ap()
        self.off = 0
        self.nbytes = nbytes

    def alloc(self, free_shape, dtype):
        esz = 4 if dtype == F32 else 2
        n = int(np.prod(free_shape))
        nb = (n * esz + 31) // 32 * 32
        assert self.off + nb <= self.nbytes, ("arena overflow", self.off, nb)
        v = self.ap[:, self.off:self.off + n * esz].bitcast(dtype)
        self.off += nb
        if len(free_shape) == 2:
            v = v.rearrange("p (a b) -> p a b", b=free_shape[1])
        elif len(free_shape) == 3:
            v = v.rearrange("p (a b c) -> p a b c", b=free_shape[1], c=free_shape[2])
        return v


def build_nc(nhalo=16, nmain=32, groups=((0, 11), (11, 22), (22, 33))):
    global NHALO, NMAIN, NTILES, GROUPS
    NHALO, NMAIN, NTILES, GROUPS = nhalo, nmain, nmain + 1, list(groups)
    nc = bass.Bass("TRN2", target_bir_lowering=False)
    S = Sched(nc)

    def din(name, shape, dt=F32):
        return nc.dram_tensor(name, list(shape), dt, kind="ExternalInput").ap()

    def dout(name, shape, dt=F32):
        return nc.dram_tensor(name, list(shape), dt, kind="ExternalOutput").ap()

    xp = din("xp", [(NHALO + NMAIN) * 128, D])
    xs = din("xs", [NSAMP, D])
    ck = din("ck", [NSAMP, 2048, 512])
    cv = din("cv", [NSAMP, 2048, 512])
    w_in = din("w_in", [D, 2560])
    w_out = din("w_out", [D, D])
    w_r = din("w_r", [D, 36])
    w_gate = din("w_gate", [32, D, 256])
    w_up = din("w_up", [32, D, 256])
    w_down = din("w_down", [32, 256, D])
    wsT_d = din("wsT", [128, 8, 128])
    g1c_d = din("g1c", [128, 8])
    g2c_d = din("g2c", [128, 8])
    ogc_d = din("ogc", [128, 8])
    gqk_d = din("gqk", [128, 8])
    vgain_d = din("vgain", [128, 512])
    bst_d = din("bst", [128, 8])
    ws00_d = din("ws00", [128, 8])
    bs0_d = din("bs0", [128, 8])
    br_d = din("br", [128, 36])
    flag_d = din("flag", [128, 1])
    c_ident = din("c_ident", [128, 128])
    c_bones = din("c_bones", [128, 128])
    c_tri = din("c_tri", [128, 128])
    c_mult = din("c_mult", [128, NOFF, 128])

    y_p = dout("y_p", [NMAIN * 128, D])
    y_s = dout("y_s", [NSAMP, D])
    nk_o = dout("nk", [2048, 512])
    nv_o = dout("nv", [2048, 512])
    nks_o = dout("nks", [NSAMP, 512])
    nvs_o = dout("nvs", [NSAMP, 512])
    nva_o = dout("nva", [NSAMP, 512])

    hs = nc.dram_tensor("hs", [NTILES * 128, D], F32, kind="Internal").ap()
    hnts = nc.dram_tensor("hnts", [128, NTILES, 8, 128], BF16, kind="Internal").ap()

    ar = Arena(nc, 200 * 1024)
    G = ar.alloc([NTILES, 32], F32)
    g2c = ar.alloc([8], F32)
    persist_end = ar.off

    psA = nc.alloc_psum_tensor("psA", [128, 8, 128], F32).ap()
    psB = nc.alloc_psum_tensor("psB", [128, 8, 128], F32).ap()
    psC = nc.alloc_psum_tensor("psC", [128, 2, 512], F32).ap()
    psT = nc.alloc_psum_tensor("psT", [128, 8, 128], BF16).ap()
    psE = nc.alloc_psum_tensor("psE", [128, 512], F32).ap()
    psA_flat = psA.rearrange("p a b -> p (a b)")
    psB_flat = psB.rearrange("p a b -> p (a b)")

    WIN = ar.alloc([8, 2560], BF16)
    WOUT = ar.alloc([8, 1024], BF16)
    WR = ar.alloc([8, 36], BF16)
    WST = ar.alloc([8, 128], BF16)
    MULT = ar.alloc([NOFF, 128], BF16)
    MULTH = ar.alloc([NOFF, 128], BF16)
    identf = ar.alloc([128], F32)
    identb = ar.alloc([128], BF16)
    bonesb = ar.alloc([128], BF16)
    ONES16 = ar.alloc([128], F32)
    g1c = ar.alloc([8], F32)
    ogc = ar.alloc([8], F32)
    GQK = ar.alloc([8], F32)
    VGAIN = ar.alloc([512], F32)
    BST = ar.alloc([8], F32)
    WS00 = ar.alloc([8], F32)
    BS0 = ar.alloc([8], F32)
    BR = ar.alloc([36], F32)
    FLAG = ar.alloc([1], F32)
    EPSC = ar.alloc([1], F32)
    stg_off = ar.off
    STG = [ar.alloc([2560], F32) for _ in range(2)]

    def small_load(dst, src, key):
        S.dma(out=dst, in_=src, writes=[key])

    small_load(g1c, g1c_d, 'g1c')
    small_load(g2c, g2c_d, 'g2c')
    small_load(ogc, ogc_d, 'ogc')
    small_load(GQK, gqk_d, 'gqk')
    small_load(VGAIN, vgain_d, 'vgain')
    small_load(BST, bst_d, 'bst')
    small_load(WS00, ws00_d, 'ws00')
    small_load(BS0, bs0_d, 'bs0')
    small_load(BR, br_d, 'br')
    small_load(FLAG, flag_d, 'flag')
    small_load(identf, c_ident, 'identf')
    S.op('dve', lambda e: e.memset(ONES16, 1.0), writes=['ones16'])
    S.op('dve', lambda e: e.memset(EPSC, EPS), writes=['epsc'])
    S.op('dve', lambda e: e.memset(VA[:, :, :, 64:65], 1.0), writes=['va_ones'])
    for i in range(6):
        S.op('pool', lambda e, i=i: e.memset(VAS[i][:, :, 64:65], 1.0), writes=[('vas', i)])
    S.op('dve', lambda e: e.tensor_copy(out=identb, in_=identf), reads=['identf'], writes=['identb'])
    S.dma(out=STG[0][:, 0:128], in_=c_bones, writes=[('stg', 0)])
    S.op('dve', lambda e: e.tensor_copy(out=bonesb, in_=STG[0][:, 0:128]), reads=[('stg', 0)], writes=['bonesb'])
    S.dma(out=STG[1][:, 0:NOFF * 128], in_=c_mult.rearrange("p a b -> p (a b)"), writes=[('stg', 1)])
    S.op('dve', lambda e: e.tensor_copy(out=MULT.rearrange("p a b -> p (a b)"), in_=STG[1][:, 0:NOFF * 128]),
         reads=[('stg', 1)], writes=['mult'])
    S.op('dve', lambda e: e.tensor_scalar(out=MULTH.rearrange("p a b -> p (a b)"), in0=STG[1][:, 0:NOFF * 128],
                                          scalar1=FLAG[:, 0:1], scalar2=None, op0=ALU.mult),
         reads=[('stg', 1), 'flag'], writes=['multh'])
    S.dma(out=STG[0][:, 0:1024], in_=wsT_d.rearrange("p a b -> p (a b)"), reads=['bonesb'], writes=[('stg', 0)])
    S.dma(out=STG[0][:, 1024:1152], in_=c_tri, writes=[('stg', 0)])
    S.op('dve', lambda e: e.tensor_tensor(
        out=WST, in0=STG[0][:, 0:1024].rearrange("p (a b) -> p a b", b=128),
        in1=STG[0][:, 1024:1152].unsqueeze(1).to_broadcast([128, 8, 128]), op=ALU.mult),
        reads=[('stg', 0)], writes=['wst'])
    S.dma(out=STG[1][:, 0:288].rearrange("p (a b) -> p a b", b=36), in_=w_r.rearrange("(kc p) f -> p kc f", p=128),
          reads=['mult', 'multh'], writes=[('stg', 1)])
    S.op('dve', lambda e: e.tensor_tensor(
        out=WR, in0=STG[1][:, 0:288].rearrange("p (a b) -> p a b", b=36),
        in1=g2c.unsqueeze(2).to_broadcast([128, 8, 36]), op=ALU.mult),
        reads=[('stg', 1), 'g2c'], writes=['wr'])
    for kc in range(8):
        st = STG[kc % 2]
        S.dma(out=st[:, 0:2560], in_=w_in[kc * 128:(kc + 1) * 128, :], reads=['wst', 'wr'], writes=[('stg', kc % 2)])
        eng = 'dve' if kc % 2 == 0 else 'pool'
        S.op(eng, lambda e, st=st, kc=kc: e.tensor_scalar(out=WIN[:, kc, :], in0=st[:, 0:2560], scalar1=g1c[:, kc:kc + 1],
                                                           scalar2=None, op0=ALU.mult),
             reads=[('stg', kc % 2), 'g1c'], writes=['win'])
    for kc in range(8):
        st = STG[kc % 2]
        S.dma(out=st[:, 0:1024], in_=w_out[kc * 128:(kc + 1) * 128, :], writes=[('stg', kc % 2)])
        eng = 'dve' if kc % 2 == 0 else 'pool'
        S.op(eng, lambda e, st=st, kc=kc: e.tensor_scalar(out=WOUT[:, kc, :], in0=st[:, 0:1024], scalar1=ogc[:, kc:kc + 1],
                                                           scalar2=None, op0=ALU.mult),
             reads=[('stg', kc % 2), 'ogc'], writes=['wout'])

    S.barrier()
    ar.off = stg_off
    XT = [ar.alloc([1024], F32) for _ in range(2)]
    xn = ar.alloc([1024], BF16)
    xnT = ar.alloc([8, 128], BF16)
    sq = ar.alloc([8, 128], BF16)
    rr = ar.alloc([8, 128], F32)
    qkn = ar.alloc([8, 128], F32)
    QT = ar.alloc([2, 4, 128], BF16)
    KT = ar.alloc([RING, 4, 128], BF16)
    VA = ar.alloc([RING, 8, 65], BF16)
    kout = ar.alloc([1024], F32)
    vout = ar.alloc([512], F32)
    u_t = ar.alloc([512], F32)
    gg = ar.alloc([512], F32)
    gsq = ar.alloc([512], F32)
    vg = ar.alloc([512], F32)
    vgb = ar.alloc([512], BF16)
    oa = ar.alloc([512], F32)
    ob = ar.alloc([512], F32)
    oab = ar.alloc([1024], BF16)
    oT = ar.alloc([8, 128], BF16)
    P = [ar.alloc([8, 128], BF16) for _ in range(2)]
    hh = ar.alloc([1024], F32)
    hn = ar.alloc([1024], BF16)
    hnT = ar.alloc([8, 128], BF16)
    junk = ar.alloc([1024], BF16)
    ST = ar.alloc([64], F32)
    RL = ar.alloc([96], F32)
    KC = [ar.alloc([512], F32) for _ in range(2)]
    VC = [ar.alloc([512], F32) for _ in range(2)]
    prod = ar.alloc([512], F32)
    SALL = ar.alloc([24], F32)
    VAS = [ar.alloc([8, 65], BF16) for _ in range(6)]
    PZ = [ar.alloc([24, 16], BF16) for _ in range(2)]
    stage1_end = ar.off
    print('stage1 arena bytes', stage1_end)

    def rstd(nt, src, dst, scale, tmpkey):
        S.op('act', lambda e: e.activation(out=dst, in_=src, func=AF.Sqrt, bias=EPSC[:nt, 0:1], scale=scale),
             reads=['st'], writes=['st'])
        S.op('dve', lambda e: e.reciprocal(out=dst, in_=dst), reads=['st'], writes=['st'])

    def transposes_bf(nt, src, dstT, rkey, wkey):
        def f(e):
            last = None
            for kc in range(8):
                last = e.transpose(out=psT[:, kc, :nt], in_=src[:nt, kc * 128:(kc + 1) * 128], identity=identb[:nt, :nt])
            return last
        S.op('pe', f, reads=[rkey], writes=['psT'])
        S.op('act', lambda e: e.copy(out=dstT[:, :, :nt], in_=psT[:, :, :nt]), reads=['psT'], writes=[wkey])

    def v3(ap, nt):
        return ap[:nt].rearrange("p (h d) -> p h d", d=64)

    def v4(ap, nt):
        return ap[:nt].rearrange("p (b h d) -> p b h d", b=2, d=64)

    psO = psC
    psO_num = psC[:, :, 0:260].rearrange("p b (h e) -> p b h e", e=65)[:, :, :, 0:64]
    psO_den = psC[:, :, 64:260:65]

    def load_x(g):
        if g == NHALO + NMAIN:
            S.dma(out=XT[g % 2][:NSAMP], in_=xs, writes=[('xt', g % 2)])
        else:
            S.dma(out=XT[g % 2], in_=xp[g * 128:(g + 1) * 128, :], writes=[('xt', g % 2)])

    import os
    CUT = int(os.environ.get('K_CUT', '99'))
    CUT2 = int(os.environ.get('K_CUT2', '99'))
    CUT3 = int(os.environ.get('K_CUT3', '99'))

    def do_tile(g, kind):
        nt = NSAMP if kind == 'sample' else 128
        xt = XT[g % 2]
        xtk = ('xt', g % 2)
        mt = g - NHALO if kind != 'sample' else NMAIN
        if g == 0:
            load_x(0)
        if g + 1 <= NHALO + NMAIN:
            load_x(g + 1)
        S.op('act', lambda e: e.activation(out=junk[:nt], in_=xt[:nt], func=AF.Square, accum_out=ST[:nt, 0:1]),
             reads=[xtk], writes=['junk', 'st'])
        rstd(nt, ST[:nt, 0:1], ST[:nt, 1:2], 1.0 / D, 'st')
        S.op('dve', lambda e: e.tensor_scalar(out=xn[:nt], in0=xt[:nt], scalar1=ST[:nt, 1:2], scalar2=None, op0=ALU.mult),
             reads=[xtk, 'st'], writes=['xn'])
        transposes_bf(nt, xn, xnT, 'xn', 'xnT')
        fcs = list(range(4, 8)) if kind == 'halo' else list(range(8))
        f0, f1 = fcs[0], fcs[-1] + 1
        nf = f1 - f0

        def fqk(e):
            last = None
            for fc in fcs:
                for kc in range(8):
                    last = e.matmul(psA[:, fc, :nt], lhsT=WIN[:, kc, fc * 128:(fc + 1) * 128], rhs=xnT[:, kc, :nt],
                                    start=(kc == 0), stop=(kc == 7))
            return last
        S.op('pe', fqk, reads=['xnT', 'win'], writes=['psA'])

        def fv(e):
            last = None
            dsts = [(psE, 1024)] if kind == 'halo' else [(psE, 1024), (psC[:, 0, :], 1536), (psC[:, 1, :], 2048)]
            for dst, c0 in dsts:
                for kc in range(8):
                    last = e.matmul(dst[:nt, 0:512], lhsT=xnT[:, kc, :nt], rhs=WIN[:, kc, c0:c0 + 512],
                                    start=(kc == 0), stop=(kc == 7))
            return last
        S.op('pe', fv, reads=['xnT', 'win'], writes=['psE'] if kind == 'halo' else ['psE', 'psC'])
        S.op('act', lambda e: e.activation(out=sq[:, f0:f1, :nt], in_=psA[:, f0:f1, :nt], func=AF.Square),
             reads=['psA'], writes=['sq'])

        def fss(e):
            last = None
            for fc in fcs:
                last = e.matmul(psB[:, fc, :nt], lhsT=bonesb, rhs=sq[:, fc, :nt], start=True, stop=True)
            return last
        S.op('pe', fss, reads=['sq', 'bonesb'], writes=['psB'])
        S.op('act', lambda e: e.activation(out=rr[:, f0:f1, :nt], in_=psB[:, f0:f1, :nt], func=AF.Sqrt, bias=EPSC[:, 0:1],
                                           scale=1.0 / 64), reads=['psB'], writes=['rr'])
        S.op('dve', lambda e: e.reciprocal(out=rr[:, f0:f1, :nt], in_=rr[:, f0:f1, :nt]), reads=['rr'], writes=['rr'])
        S.op('dve', lambda e: e.tensor_tensor(out=qkn[:, f0:f1, :nt], in0=psA[:, f0:f1, :nt],
                                              in1=GQK[:, f0:f1].unsqueeze(2).to_broadcast([128, nf, nt]), op=ALU.mult),
             reads=['psA', 'gqk'], writes=['qkn'])
        S.op('dve', lambda e: e.tensor_tensor(out=qkn[:, f0:f1, :nt], in0=qkn[:, f0:f1, :nt], in1=rr[:, f0:f1, :nt],
                                              op=ALU.mult), reads=['qkn', 'rr'], writes=['qkn'])
        slot = g % RING
        if kind != 'sample':
            S.op('pool', lambda e: e.tensor_copy(out=KT[:, slot, :, :], in_=qkn[:, 4:8, :]), reads=['qkn'],
                 writes=[('kt', slot)])
        if kind == 'main':
            if g == NHALO:
                S.op('pool', lambda e: e.memset(QT, 0.0), writes=['qt'])
            S.op('pool', lambda e: e.tensor_copy(out=QT[0:64, 0, :, :], in_=qkn[0:64, 0:4, :]), reads=['qkn'], writes=['qt'])
            S.op('pool', lambda e: e.tensor_copy(out=QT[64:128, 1, :, :], in_=qkn[64:128, 0:4, :]), reads=['qkn'],
                 writes=['qt'])
        if kind == 'main' and CUT <= 1:
            return
        KO0 = NHALO + max(NMAIN - 16, 0)
        want_kout = (kind == 'sample') or (kind == 'main' and g >= KO0)
        if want_kout:
            tf = list(range(8)) if kind == 'sample' else list(range(4, 8))

            def ftr(e):
                last = None
                for fc in tf:
                    last = e.transpose(out=psB_flat[:nt, fc * 128:(fc + 1) * 128], in_=qkn[:, fc, :nt], identity=identf)
                return last
            S.op('pe', ftr, reads=['qkn', 'identf'], writes=['psB'])
            t0 = tf[0] * 128
            S.op('dve', lambda e: e.tensor_copy(out=kout[:nt, t0:1024], in_=psB_flat[:nt, t0:1024]), reads=['psB'],
                 writes=['kout'])
            if kind == 'sample':
                S.dma(out=nks_o, in_=kout[:nt, 512:1024], reads=['kout'])
            else:
                r0 = (g - KO0) * 128
                S.dma(out=nk_o[r0:r0 + 128, :], in_=kout[:, 512:1024], reads=['kout'])
        if kind == 'main' and CUT <= 2:
            return
        if kind != 'sample':
            S.op('act', lambda e: e.copy(out=VA[:nt, slot, :, 0:64], in_=v3(psE, nt)), reads=['psE', 'va_ones'],
                 writes=[('va', slot)])
        if want_kout:
            S.op('dve', lambda e: e.tensor_copy(out=vout[:nt], in_=psE[:nt, 0:512]), reads=['psE'], writes=['vout'])
            if kind == 'sample':
                S.dma(out=nvs_o, in_=vout[:nt], reads=['vout'])
            else:
                r0 = (g - KO0) * 128
                S.dma(out=nv_o[r0:r0 + 128, :], in_=vout, reads=['vout'])
        if kind == 'main' and CUT <= 3:
            return
        if kind == 'halo':
            return
        S.op('act', lambda e: e.activation(out=u_t[:nt], in_=psC[:nt, 0, :], func=AF.Gelu_apprx_tanh), reads=['psC'],
             writes=['u'])
        S.op('act', lambda e: e.activation(out=gg[:nt], in_=psC[:nt, 1, :], func=AF.Gelu_apprx_tanh), reads=['psC'],
             writes=['gg'])
        if kind == 'main' and CUT2 <= 31:
            return
        S.op('dve', lambda e: e.tensor_tensor(out=gsq[:nt], in0=gg[:nt], in1=gg[:nt], op=ALU.mult), reads=['gg'],
             writes=['gsq'])
        S.op('dve', lambda e: e.tensor_reduce(out=ST[:nt, 8:16], in_=v3(gsq, nt), axis=AX.X, op=ALU.add), reads=['gsq'],
             writes=['st'])
        if kind == 'main' and CUT2 <= 32:
            return
        rstd(nt, ST[:nt, 8:16], ST[:nt, 16:24], 1.0 / 64, 'st')
        if kind == 'main' and CUT2 <= 33:
            return
        S.op('dve', lambda e: e.tensor_tensor(out=v3(vg, nt), in0=v3(gg, nt),
                                              in1=ST[:nt, 16:24].unsqueeze(2).to_broadcast([nt, 8, 64]), op=ALU.mult),
             reads=['gg', 'st'], writes=['vg'])
        S.op('dve', lambda e: e.tensor_tensor(out=vg[:nt], in0=vg[:nt], in1=VGAIN[:nt], op=ALU.mult),
             reads=['vg', 'vgain'], writes=['vg'])
        if kind == 'main' and CUT2 <= 34:
            return
        if kind == 'sample':
            S.dma(out=nva_o, in_=vg[:nt], reads=['vg'])
            S.op('dve', lambda e: e.tensor_tensor(out=v3(oa, nt), in0=v3(vg, nt),
                                                  in1=WS00[:nt].unsqueeze(2).to_broadcast([nt, 8, 64]), op=ALU.mult),
                 reads=['vg', 'ws00'], writes=['oa'])
            S.op('dve', lambda e: e.tensor_tensor(out=v3(oa, nt), in0=v3(oa, nt),
                                                  in1=BS0[:nt].unsqueeze(2).to_broadcast([nt, 8, 64]), op=ALU.add),
                 reads=['oa', 'bs0'], writes=['oa'])
        else:
            S.op('pool', lambda e: e.tensor_copy(out=vgb, in_=vg), reads=['vg'], writes=['vgb'])

            def fmix(e):
                last = None
                for h in range(8):
                    last = e.matmul(psE[:, h * 64:(h + 1) * 64], lhsT=WST[:, h, :], rhs=vgb[:, h * 64:(h + 1) * 64],
                                    start=True, stop=True)
                return last
            S.op('pe', fmix, reads=['vgb', 'wst'], writes=['psE'])
            S.op('dve', lambda e: e.tensor_tensor(out=v3(oa, nt), in0=v3(psE, nt),
                                                  in1=BST.unsqueeze(2).to_broadcast([128, 8, 64]), op=ALU.add),
                 reads=['psE', 'bst'], writes=['oa'])
        if kind == 'main' and CUT2 <= 35:
            return
        S.op('dve', lambda e: e.tensor_tensor(out=oa[:nt], in0=oa[:nt], in1=u_t[:nt], op=ALU.mult), reads=['oa', 'u'],
             writes=['oa'])
        if kind == 'main' and CUT2 <= 36:
            return
        S.op('act', lambda e: e.activation(out=junk[:nt, 0:512], in_=oa[:nt], func=AF.Square, accum_out=ST[:nt, 24:25]),
             reads=['oa'], writes=['junk', 'st'])
        if kind == 'main' and CUT2 <= 37:
            return
        rstd(nt, ST[:nt, 24:25], ST[:nt, 25:26], 1.0 / 512, 'st')
        if kind == 'main' and CUT2 <= 38:
            return
        S.op('dve', lambda e: e.tensor_scalar(out=oab[:nt, 0:512], in0=oa[:nt], scalar1=ST[:nt, 25:26], scalar2=None,
                                              op0=ALU.mult), reads=['oa', 'st'], writes=['oab'])
        if kind == 'main' and CUT <= 4:
            return
        if kind == 'main':
            def s_op(i):
                o = NOFF - 1 - i
                kt = g - o
                sl = kt % RING
                ps = psA if i % 2 == 0 else psB
                pk = 'psA' if i % 2 == 0 else 'psB'

                def f(e):
                    last = None
                    for h in range(8):
                        a, hp = h % 2, h // 2
                        last = e.matmul(ps[:, h, :], lhsT=KT[:, sl, hp, :], rhs=QT[:, a, hp, :], start=True, stop=True)
                    return last
                S.op('pe', f, reads=[('kt', sl), 'qt'], writes=[pk])
            s_op(0)
            for i in range(NOFF):
                o = NOFF - 1 - i
                kt = g - o
                sl = kt % RING
                ps = psA if i % 2 == 0 else psB
                pk = 'psA' if i % 2 == 0 else 'psB'
                Pi = P[i % 2]
                pik = ('P', i % 2)
                if i + 1 < NOFF:
                    s_op(i + 1)
                if CUT3 < 2:
                    continue
                S.op('act', lambda e, ps=ps, Pi=Pi: e.activation(out=Pi, in_=ps, func=AF.Exp, scale=0.125), reads=[pk],
                     writes=[pik])
                if CUT3 < 3:
                    continue
                M = MULTH if kt < NHALO else MULT
                S.op('pool', lambda e, Pi=Pi, M=M, o=o: e.tensor_tensor(
                    out=Pi, in0=Pi, in1=M[:, o, :].unsqueeze(1).to_broadcast([128, 8, 128]), op=ALU.mult),
                    reads=[pik, 'mult', 'multh'], writes=[pik])

                if CUT3 < 4:
                    continue

                def fpv(e, Pi=Pi, sl=sl, i=i):
                    last = None
                    for h in range(8):
                        last = e.matmul(psO[:, h // 4, (h % 4) * 65:(h % 4) * 65 + 65], lhsT=Pi[:, h, :],
                                        rhs=VA[:, sl, h, :], start=(i == 0 and h % 4 == 0), stop=(i == NOFF - 1 and h % 4 == 3),
                                        skip_group_check=True)
                    return last
                S.op('pe', fpv, reads=[pik, ('va', sl)], writes=['psC'])
            if CUT3 < 5:
                return
            S.op('dve', lambda e: e.reciprocal(out=ST[:, 32:40].rearrange("p (b h) -> p b h", b=2), in_=psO_den),
                 reads=['psC'], writes=['st'])
            if CUT3 < 6:
                return
            S.op('dve', lambda e: e.tensor_tensor(
                out=v4(ob, nt), in0=psO_num,
                in1=ST[:, 32:40].rearrange("p (b h) -> p b h", b=2).unsqueeze(3).to_broadcast([128, 2, 4, 64]),
                op=ALU.mult), reads=['psC', 'st'], writes=['ob'])
        else:
            sample_attention()
        if kind == 'main' and CUT <= 5:
            return
        S.op('act', lambda e: e.activation(out=junk[:nt, 0:512], in_=ob[:nt], func=AF.Square, accum_out=ST[:nt, 26:27]),
             reads=['ob'], writes=['junk', 'st'])
        rstd(nt, ST[:nt, 26:27], ST[:nt, 27:28], 1.0 / 512, 'st')
        S.op('dve', lambda e: e.tensor_scalar(out=oab[:nt, 512:1024], in0=ob[:nt], scalar1=ST[:nt, 27:28], scalar2=None,
                                              op0=ALU.mult), reads=['ob', 'st'], writes=['oab'])
        transposes_bf(nt, oab, oT, 'oab', 'oT')

        def fwo(e):
            last = None
            for half in range(2):
                for kc in range(8):
                    last = e.matmul(psA_flat[:nt, half * 512:(half + 1) * 512], lhsT=oT[:, kc, :nt],
                                    rhs=WOUT[:, kc, half * 512:(half + 1) * 512], start=(kc == 0), stop=(kc == 7))
            return last
        S.op('pe', fwo, reads=['oT', 'wout'], writes=['psA'])
        S.op('dve', lambda e: e.tensor_tensor(out=hh[:nt], in0=psA_flat[:nt], in1=xt[:nt], op=ALU.add),
             reads=['psA', xtk], writes=['hh'])
        S.dma(out=hs[mt * 128:mt * 128 + nt, :], in_=hh[:nt], reads=['hh'], writes=[('hs', mt)])
        if kind == 'main' and CUT <= 6:
            return
        S.op('act', lambda e: e.activation(out=junk[:nt], in_=hh[:nt], func=AF.Square, accum_out=ST[:nt, 28:29]),
             reads=['hh'], writes=['junk', 'st'])
        rstd(nt, ST[:nt, 28:29], ST[:nt, 29:30], 1.0 / D, 'st')
        S.op('dve', lambda e: e.tensor_scalar(out=hn[:nt], in0=hh[:nt], scalar1=ST[:nt, 29:30], scalar2=None, op0=ALU.mult),
             reads=['hh', 'st'], writes=['hn'])
        transposes_bf(nt, hn, hnT, 'hn', 'hnT')
        S.dma(out=hnts[:, mt, :, :nt], in_=hnT[:, :, :nt], reads=['hnT'], writes=[('hnts', mt)])
        if kind == 'main' and CUT <= 7:
            return

        def frt(e):
            last = None
            for kc in range(8):
                last = e.matmul(psE[:nt, 0:36], lhsT=hnT[:, kc, :nt], rhs=WR[:, kc, :], start=(kc == 0), stop=(kc == 7))
            return last
        S.op('pe', frt, reads=['hnT', 'wr'], writes=['psE'])
        L = RL[:nt, 0:36]
        S.op('dve', lambda e: e.tensor_tensor(out=L, in0=psE[:nt, 0:36], in1=BR[:nt], op=ALU.add), reads=['psE', 'br'],
             writes=['rl'])
        S.op('dve', lambda e: e.tensor_reduce(out=RL[:nt, 36:37], in_=RL[:nt, 0:4], axis=AX.X, op=ALU.max), reads=['rl'],
             writes=['rl'])
        S.op('dve', lambda e: e.tensor_scalar(out=RL[:nt, 40:44], in0=RL[:nt, 0:4], scalar1=RL[:nt, 36:37], scalar2=None,
                                              op0=ALU.is_equal), reads=['rl'], writes=['rl'])
        S.op('dve', lambda e: e.tensor_scalar(out=RL[:nt, 37:38], in0=RL[:nt, 36:37], scalar1=-1.0, scalar2=None,
                                              op0=ALU.mult), reads=['rl'], writes=['rl'])
        S.op('act', lambda e: e.activation(out=RL[:nt, 44:48], in_=RL[:nt, 0:4], func=AF.Exp, bias=RL[:nt, 37:38], scale=1.0,
                                           accum_out=RL[:nt, 38:39]), reads=['rl'], writes=['rl'])
        S.op('dve', lambda e: e.reciprocal(out=RL[:nt, 39:40], in_=RL[:nt, 38:39]), reads=['rl'], writes=['rl'])
        S.op('dve', lambda e: e.tensor_tensor(
            out=RL[:nt, 48:80].rearrange("p (g x) -> p g x", x=8), in0=RL[:nt, 4:36].rearrange("p (g x) -> p g x", x=8),
            in1=RL[:nt, 40:44].unsqueeze(2).to_broadcast([nt, 4, 8]), op=ALU.mult), reads=['rl'], writes=['rl'])
        S.op('dve', lambda e: e.tensor_reduce(out=RL[:nt, 80:88], in_=RL[:nt, 48:80].rearrange("p (g x) -> p x g", x=8),
                                              axis=AX.X, op=ALU.add), reads=['rl'], writes=['rl'])
        S.op('dve', lambda e: e.max(out=RL[:nt, 88:96], in_=RL[:nt, 80:88]), reads=['rl'], writes=['rl'])
        S.op('dve', lambda e: e.tensor_tensor(out=ST[:nt, 40:41], in0=RL[:nt, 89:90], in1=RL[:nt, 88:89], op=ALU.subtract),
             reads=['rl'], writes=['st'])
        S.op('act', lambda e: e.activation(out=ST[:nt, 41:42], in_=ST[:nt, 40:41], func=AF.Exp), reads=['st'], writes=['st'])
        S.op('dve', lambda e: e.tensor_scalar(out=ST[:nt, 41:42], in0=ST[:nt, 41:42], scalar1=1.0, scalar2=None, op0=ALU.add),
             reads=['st'], writes=['st'])
        S.op('dve', lambda e: e.reciprocal(out=ST[:nt, 42:43], in_=ST[:nt, 41:42]), reads=['st'], writes=['st'])
        S.op('dve', lambda e: e.tensor_scalar(out=ST[:nt, 43:44], in0=ST[:nt, 42:43], scalar1=-1.0, scalar2=1.0, op0=ALU.mult,
                                              op1=ALU.add), reads=['st'], writes=['st'])
        S.op('dve', lambda e: e.tensor_tensor(out=ST[:nt, 42:44], in0=ST[:nt, 42:44],
                                              in1=RL[:nt, 39:40].to_broadcast([nt, 2]), op=ALU.mult), reads=['st', 'rl'],
             writes=['st'])
        S.op('dve', lambda e: e.tensor_scalar(out=ST[:nt, 44:52], in0=RL[:nt, 80:88], scalar1=RL[:nt, 88:89],
                                              scalar2=ST[:nt, 42:43], op0=ALU.is_equal, op1=ALU.mult), reads=['rl', 'st'],
             writes=['st'])
        S.op('dve', lambda e: e.tensor_scalar(out=ST[:nt, 52:60], in0=RL[:nt, 80:88], scalar1=RL[:nt, 89:90],
                                              scalar2=ST[:nt, 43:44], op0=ALU.is_equal, op1=ALU.mult), reads=['rl', 'st'],
             writes=['st'])
        S.op('dve', lambda e: e.tensor_tensor(out=ST[:nt, 44:52], in0=ST[:nt, 44:52], in1=ST[:nt, 52:60], op=ALU.add),
             reads=['st'], writes=['st'])
        S.op('dve', lambda e: e.tensor_tensor(
            out=G[:nt, mt, :].rearrange("p (g x) -> p g x", x=8),
            in0=RL[:nt, 40:44].unsqueeze(2).to_broadcast([nt, 4, 8]),
            in1=ST[:nt, 44:52].unsqueeze(1).to_broadcast([nt, 4, 8]), op=ALU.mult), reads=['rl', 'st'], writes=['G'])

    def sample_attention():
        nt = NSAMP
        qtok = kout[:nt, 0:512]
        ktok = kout[:nt, 512:1024]
        rows = [(1920, 1), (1536, 4), (0, 16)]
        for b in range(NSAMP):
            S.op('dve', lambda e, b=b: e.tensor_scalar(out=gsq[:nt], in0=qtok, scalar1=identf[:nt, b:b + 1], scalar2=None,
                                                        op0=ALU.mult), reads=['kout', 'identf'], writes=['gsq'])

            def fqb(e, b=b):
                return e.matmul(psE[:, 0:512], lhsT=ONES16[0:16, :], rhs=gsq[:nt], start=True, stop=True)
            S.op('pe', fqb, reads=['gsq', 'ones16'], writes=['psE'])
            pz = PZ[b % 2]
            pzk = ('pz', b % 2)
            S.op('pool', lambda e, pz=pz: e.memset(pz, 0.0), writes=[pzk])
            for c, (r0, stp) in enumerate(rows):
                bi = (b * 3 + c) % 2
                kc_t, vc_t = KC[bi], VC[bi]
                S.dma(out=kc_t, in_=ck[b, r0:r0 + 128 * stp:stp, :], writes=[('kc', bi)])
                S.dma(out=vc_t, in_=cv[b, r0:r0 + 128 * stp:stp, :], writes=[('vc', bi)])
                S.op('dve', lambda e, kc_t=kc_t: e.tensor_tensor(out=prod, in0=kc_t, in1=psE[:, 0:512], op=ALU.mult),
                     reads=[('kc', bi), 'psE'], writes=['prod'])
                S.op('dve', lambda e, c=c: e.tensor_reduce(out=SALL[:, c * 8:(c + 1) * 8],
                                                           in_=prod.rearrange("p (h d) -> p h d", d=64), axis=AX.X,
                                                           op=ALU.add), reads=['prod'], writes=['sall'])
                vi = (b % 2) * 3 + c
                S.op('act', lambda e, vi=vi, vc_t=vc_t: e.copy(out=VAS[vi][:, :, 0:64],
                                                               in_=vc_t.rearrange("p (h d) -> p h d", d=64)),
                     reads=[('vc', bi), ('vas', vi)], writes=[('vas', vi)])
            S.op('act', lambda e, pz=pz, b=b: e.activation(out=pz[:, :, b], in_=SALL[:, 0:24], func=AF.Exp, scale=0.125),
                 reads=['sall', pzk], writes=[pzk])

            def fpv(e, b=b, pz=pz):
                last = None
                for h in range(8):
                    for c in range(3):
                        vi = (b % 2) * 3 + c
                        last = e.matmul(psO[:nt, h // 4, (h % 4) * 65:(h % 4) * 65 + 65], lhsT=pz[:, c * 8 + h, :],
                                        rhs=VAS[vi][:, h, :], start=(b == 0 and c == 0 and h % 4 == 0),
                                        stop=(b == NSAMP - 1 and c == 2 and h % 4 == 3), skip_group_check=True)
                return last
            S.op('pe', fpv, reads=[pzk] + [('vas', (b % 2) * 3 + c) for c in range(3)], writes=['psC'])
        S.op('dve', lambda e: e.tensor_tensor(out=prod[:nt], in0=qtok, in1=ktok, op=ALU.mult), reads=['kout'], writes=['prod'])
        S.op('dve', lambda e: e.tensor_reduce(out=ST[:nt, 32:40], in_=prod[:nt].rearrange("p (h d) -> p h d", d=64),
                                              axis=AX.X, op=ALU.add), reads=['prod'], writes=['st'])
        S.op('act', lambda e: e.activation(out=ST[:nt, 32:40], in_=ST[:nt, 32:40], func=AF.Exp, scale=0.125), reads=['st'],
             writes=['st'])
        S.op('dve', lambda e: e.tensor_scalar(out=ST[:nt, 32:40], in0=ST[:nt, 32:40], scalar1=3.0, scalar2=None,
                                              op0=ALU.mult), reads=['st'], writes=['st'])
        es = ST[:nt, 32:40].rearrange("p (b h) -> p b h", b=2)
        dn = ST[:nt, 48:56].rearrange("p (b h) -> p b h", b=2)
        S.op('dve', lambda e: e.tensor_tensor(out=dn, in0=psO_den[:nt], in1=es, op=ALU.add), reads=['psC', 'st'],
             writes=['st'])
        S.op('dve', lambda e: e.reciprocal(out=dn, in_=dn), reads=['st'], writes=['st'])
        S.op('dve', lambda e: e.tensor_tensor(out=v4(ob, nt), in0=v4(vout, nt),
                                              in1=es.unsqueeze(3).to_broadcast([nt, 2, 4, 64]), op=ALU.mult),
             reads=['vout', 'st'], writes=['ob'])
        S.op('dve', lambda e: e.tensor_tensor(out=v4(ob, nt), in0=v4(ob, nt), in1=psO_num[:nt], op=ALU.add),
             reads=['ob', 'psC'], writes=['ob'])
        S.op('dve', lambda e: e.tensor_tensor(out=v4(ob, nt), in0=v4(ob, nt),
                                              in1=dn.unsqueeze(3).to_broadcast([nt, 2, 4, 64]), op=ALU.mult),
             reads=['ob', 'st'], writes=['ob'])

    import os
    UPTO = int(os.environ.get("K_UPTO", "9"))
    if UPTO >= 1:
        for g in range(NHALO):
            do_tile(g, 'halo')
    if UPTO >= 2:
        for g in range(NHALO, NHALO + NMAIN):
            do_tile(g, 'main')
    if UPTO >= 3:
        do_tile(NHALO + NMAIN, 'sample')
    if UPTO < 4:
        S.finish()
        return nc

    S.barrier()
    ar.off = persist_end
    NG = 11
    ACC = ar.alloc([NG, 1024], F32)
    HNT = ar.alloc([NG, 8, 128], BF16)
    WGf = [ar.alloc([8, 256], F32) for _ in range(2)]
    WUf = [ar.alloc([8, 256], F32) for _ in range(2)]
    WDf = [ar.alloc([2, 1024], F32) for _ in range(2)]
    WGb = [ar.alloc([8, 256], BF16) for _ in range(2)]
    WUb = [ar.alloc([8, 256], BF16) for _ in range(2)]
    WDb = [ar.alloc([2, 1024], BF16) for _ in range(2)]
    SIL = [ar.alloc([2, 128], F32) for _ in range(2)]
    HID = [ar.alloc([2, 128], BF16) for _ in range(2)]
    psAB = [psC[:, 0, :].rearrange("p (a b) -> p a b", b=128), psC[:, 1, :].rearrange("p (a b) -> p a b", b=128)]
    psD = [psA_flat, psB_flat]
    psDk = ['psA', 'psB']
    it = 0
    for (t0, t1) in GROUPS:
        for t in range(t0, t1):
            nt = NSAMP if t == NMAIN else 128
            j = t - t0
            S.dma(out=ACC[:nt, j, :], in_=hs[t * 128:t * 128 + nt, :], writes=[('acc', j)])
            S.dma(out=HNT[:, j, :, :nt], in_=hnts[:, t, :, :nt], writes=[('hnt', j)])
        for ex in range(32):
            wb = ex % 2
            S.dma(out=WGf[wb], in_=w_gate[ex].rearrange("(kc p) f -> p kc f", p=128), writes=[('wgf', wb)])
            S.dma(out=WUf[wb], in_=w_up[ex].rearrange("(kc p) f -> p kc f", p=128), writes=[('wuf', wb)])
            S.dma(out=WDf[wb], in_=w_down[ex].rearrange("(kc p) f -> p kc f", p=128), writes=[('wdf', wb)])
            S.op('pool', lambda e, wb=wb: e.tensor_tensor(out=WGb[wb], in0=WGf[wb],
                                                           in1=g2c.unsqueeze(2).to_broadcast([128, 8, 256]), op=ALU.mult),
                 reads=[('wgf', wb)], writes=[('wgb', wb)])
            S.op('pool', lambda e, wb=wb: e.tensor_tensor(out=WUb[wb], in0=WUf[wb],
                                                           in1=g2c.unsqueeze(2).to_broadcast([128, 8, 256]), op=ALU.mult),
                 reads=[('wuf', wb)], writes=[('wub', wb)])
            S.op('pool', lambda e, wb=wb: e.tensor_copy(out=WDb[wb], in_=WDf[wb]), reads=[('wdf', wb)], writes=[('wdb', wb)])
            for t in range(t0, t1):
                nt = NSAMP if t == NMAIN else 128
                j = t - t0
                pb = it % 2
                it += 1
                pab = psAB[pb]
                pabk = ('psab', pb)

                def fgu(e, wb=wb, j=j, nt=nt, pab=pab):
                    last = None
                    for m, W in enumerate((WGb[wb], WUb[wb])):
                        for fcx in range(2):
                            for kc in range(8):
                                last = e.matmul(pab[:, m * 2 + fcx, :nt], lhsT=W[:, kc, fcx * 128:(fcx + 1) * 128],
                                                rhs=HNT[:, j, kc, :nt], start=(kc == 0), stop=(kc == 7))
                    return last
                S.op('pe', fgu, reads=[('wgb', wb), ('wub', wb), ('hnt', j)], writes=[pabk])
                S.op('act', lambda e, pb=pb, nt=nt, pab=pab: e.activation(out=SIL[pb][:, :, :nt], in_=pab[:, 0:2, :nt],
                                                                           func=AF.Silu), reads=[pabk], writes=[('sil', pb)])
                S.op('dve', lambda e, pb=pb, nt=nt, pab=pab: e.tensor_tensor(out=HID[pb][:, :, :nt], in0=SIL[pb][:, :, :nt],
                                                                              in1=pab[:, 2:4, :nt], op=ALU.mult),
                     reads=[pabk, ('sil', pb)], writes=[('hid', pb)])
                pd = psD[pb]

                def fdn(e, wb=wb, pb=pb, nt=nt, pd=pd):
                    last = None
                    for half in range(2):
                        for fcx in range(2):
                            last = e.matmul(pd[:nt, half * 512:(half + 1) * 512], lhsT=HID[pb][:, fcx, :nt],
                                            rhs=WDb[wb][:, fcx, half * 512:(half + 1) * 512], start=(fcx == 0),
                                            stop=(fcx == 1))
                    return last
                S.op('pe', fdn, reads=[('hid', pb), ('wdb', wb)], writes=[psDk[pb]])
                S.op('dve', lambda e, j=j, nt=nt, pd=pd, t=t, ex=ex: e.scalar_tensor_tensor(
                    out=ACC[:nt, j, :], in0=pd[:nt, :], scalar=G[:nt, t, ex:ex + 1], in1=ACC[:nt, j, :], op0=ALU.mult,
                    op1=ALU.add), reads=[psDk[pb], ('acc', j)], writes=[('acc', j)])
        for t in range(t0, t1):
            j = t - t0
            if t == NMAIN:
                S.dma(out=y_s, in_=ACC[:NSAMP, j, :], reads=[('acc', j)])
            else:
                S.dma(out=y_p[t * 128:(t + 1) * 128, :], in_=ACC[:, j, :], reads=[('acc', j)])
    S.finish()
    return nc


_NC_CACHE = {}


def _consts():
    ident = np.eye(128, dtype=np.float32)
    p = np.arange(128)
    bones = (p[:, None] // 64 == p[None, :] // 64).astype(np.float32)
    tri = (p[:, None] <= p[None, :]).astype(np.float32)
    mult = np.zeros((128, NOFF, 128), np.float32)
    for o in range(NOFF):
        delta = 128 * o + p[None, :] - p[:, None]
        m = ((delta >= 0) & (delta <= 128)).astype(np.float32)
        m += ((delta >= 0) & (delta <= 512) & (delta % 4 == 0)).astype(np.float32)
        m += ((delta >= 0) & (delta <= 2048) & (delta % 16 == 0)).astype(np.float32)
        mult[:, o, :] = m
    sel = np.zeros((16, 16, 128), np.float32)
    for b in range(16):
        sel[b, b, :] = 1.0
    return ident, bones, tri, mult, sel


def kernel(x_prompt, x_sample, cache_k, cache_v, norm1_g, w_in, q_gain, k_gain, v_gain,
           w_spatial, b_spatial, out_gain_a, out_gain_b, w_out, norm2_g, w_router1, b_router1,
           w_router2, b_router2, w_up, w_gate, w_down):
    f = lambda a: np.ascontiguousarray(np.asarray(a, dtype=np.float32))
    x_prompt, x_sample = f(x_prompt), f(x_sample)
    cache_k, cache_v = np.asarray(cache_k, dtype=np.float32), np.asarray(cache_v, dtype=np.float32)
    if 'nc' not in _NC_CACHE:
        _NC_CACHE['nc'] = build_nc()
    nc = _NC_CACHE['nc']
    ident, bones, tri, mult, sel = _consts()
    col = lambda v: f(np.asarray(v).reshape(8, 128).T)
    g1c, g2c = col(norm1_g[0]), col(norm2_g[0])
    ogc = col(np.concatenate([np.asarray(out_gain_a[0]), np.asarray(out_gain_b[0])]))
    qg = np.asarray(q_gain[0]).reshape(4, 128).T
    kg = np.asarray(k_gain[0]).reshape(4, 128).T
    gqk = f(np.concatenate([qg, kg], axis=1))
    vgain = f(np.broadcast_to(np.asarray(v_gain[0]).reshape(1, 512), (128, 512)))
    bst = f(np.asarray(b_spatial[0]).T)
    ws00 = f(np.broadcast_to(np.asarray(w_spatial[0])[:, 0, 0].reshape(1, 8), (128, 8)))
    bs0 = f(np.broadcast_to(np.asarray(b_spatial[0])[:, 0].reshape(1, 8), (128, 8)))
    br = f(np.broadcast_to(np.concatenate([np.asarray(b_router1[0]).reshape(4), np.asarray(b_router2[0]).reshape(32)]
                                          ).reshape(1, 36), (128, 36)))
    wsT = f(np.transpose(np.asarray(w_spatial[0]), (2, 0, 1)))
    w_r = f(np.concatenate([np.asarray(w_router1[0]),
                            np.transpose(np.asarray(w_router2[0]), (1, 0, 2)).reshape(D, 32)], axis=1))
    wg = f(np.asarray(w_gate[0]).reshape(32, D, 256))
    wu = f(np.asarray(w_up[0]).reshape(32, D, 256))
    wd = f(np.asarray(w_down[0]).reshape(32, 256, D))
    w_in0, w_out0 = f(w_in[0]), f(w_out[0])
    in_maps = []
    for c in range(NCORES):
        b, hf = c // 2, c % 2
        xp = np.zeros((48 * 128, D), np.float32)
        if hf == 1:
            xp[:2048] = x_prompt[b, 2048:4096]
        xp[2048:] = x_prompt[b, hf * 4096:(hf + 1) * 4096]
        in_maps.append({
            "xp": xp, "xs": f(x_sample[16 * c:16 * c + 16, 0]),
            "ck": f(cache_k[0, 16 * c:16 * c + 16].reshape(16, 2048, 512)),
            "cv": f(cache_v[0, 16 * c:16 * c + 16].reshape(16, 2048, 512)),
            "w_in": w_in0, "w_out": w_out0, "w_r": w_r, "w_gate": wg, "w_up": wu, "w_down": wd, "wsT": wsT,
            "g1c": g1c, "g2c": g2c, "ogc": ogc, "gqk": gqk, "vgain": vgain, "bst": bst, "ws00": ws00, "bs0": bs0,
            "br": br, "flag": np.full((128, 1), float(hf), np.float32),
            "c_ident": ident, "c_bones": bones, "c_tri": tri, "c_mult": mult,
        })
    res = run_bass_kernel_spmd(nc, in_maps, core_ids=list(range(NCORES)))
    R = res.results
    B, T = 4, 8192
    y_prompt = np.zeros((B, T, D), np.float32)
    y_sample = np.zeros((128, 1, D), np.float32)
    nkp = np.zeros((1, B, 2048, 8, 64), np.float32)
    nvp = np.zeros((1, B, 2048, 8, 64), np.float32)
    nks = np.zeros((1, 128, 1, 8, 64), np.float32)
    nvs = np.zeros((1, 128, 1, 8, 64), np.float32)
    nva = np.zeros((1, 128, 1, 8, 64), np.float32)
    for c in range(NCORES):
        b, hf = c // 2, c % 2
        r = R[c]
        y_prompt[b, hf * 4096:(hf + 1) * 4096] = r["y_p"]
        y_sample[16 * c:16 * c + 16, 0] = r["y_s"]
        if hf == 1:
            nkp[0, b] = r["nk"].reshape(2048, 8, 64)
            nvp[0, b] = r["nv"].reshape(2048, 8, 64)
        nks[0, 16 * c:16 * c + 16, 0] = r["nks"].reshape(16, 8, 64)
        nvs[0, 16 * c:16 * c + 16, 0] = r["nvs"].reshape(16, 8, 64)
        nva[0, 16 * c:16 * c + 16, 0] = r["nva"].reshape(16, 8, 64)
    return (y_prompt, y_sample, nkp, nvp, nks, nvs, nva)
```

```python
import numpy as np
import ml_dtypes
import concourse.bass as bass
import concourse.mybir as mybir
from concourse.bass_utils import run_bass_kernel_spmd

F32 = mybir.dt.float32
BF16 = mybir.dt.bfloat16
U8 = mybir.dt.uint8
ALU = mybir.AluOpType
AF = mybir.ActivationFunctionType
AX = mybir.AxisListType

D = 1024
NCORES = 8
NHALO = 16
NMAIN = 32
RING = 20
NOFF = 17
NSAMP = 16
NTILES = NMAIN + 1
EPS = 1e-6
NDS = 24
GROUPS = [(0, 11), (11, 22), (22, 33)]


class Sched:
    def __init__(self, nc):
        self.nc = nc
        self.ce = ['pe', 'act', 'dve', 'pool']
        self.prog = {e: [] for e in self.ce + ['sp']}
        self.sem = {e: nc.alloc_semaphore("s_" + e) for e in self.ce}
        self.cnt = {e: 0 for e in self.ce}
        self.dsem = [nc.alloc_semaphore("d%d" % i) for i in range(NDS)]
        self.dcnt = [0] * NDS
        self.dnext = 0
        self.waited = {}
        self.lastw = {}
        self.readers = {}

    def _semh(self, sk):
        return self.sem[sk[1]] if sk[0] == 'e' else self.dsem[sk[1]]

    def _deps(self, e, reads, writes):
        need = {}

        def add(tok):
            if tok is None:
                return
            sk, v, te = tok
            if te == e and e == 'pe':
                return
            if v > need.get(sk, 0):
                need[sk] = v
        for k in reads:
            add(self.lastw.get(k))
        for k in writes:
            add(self.lastw.get(k))
            for sk, (v, te) in self.readers.get(k, {}).items():
                add((sk, v, te))
        out = []
        for sk, v in need.items():
            if self.waited.get((e, sk), 0) >= v:
                continue
            self.waited[(e, sk)] = v
            out.append((self._semh(sk), v))
        return out

    def _commit(self, tok, reads, writes):
        sk, v, te = tok
        for k in reads:
            self.readers.setdefault(k, {})[sk] = (v, te)
        for k in writes:
            self.lastw[k] = tok
            self.readers[k] = {}

    def op(self, e, fn, reads=(), writes=()):
        waits = self._deps(e, reads, writes)
        self.cnt[e] += 1
        tok = (('e', e), self.cnt[e], e)
        sem = self.sem[e]

        def emit(eng):
            for s, v in waits:
                eng.wait_ge(s, v)
            fn(eng).then_inc(sem, 1)
        self.prog[e].append(emit)
        self._commit(tok, reads, writes)

    def dma(self, out, in_, reads=(), writes=(), q='sp'):
        waits = self._deps(q, reads, writes)
        i = self.dnext
        self.dnext = (i + 1) % NDS
        prev = self.dcnt[i]
        if prev > 0 and self.waited.get((q, ('d', i)), 0) < prev:
            waits.append((self.dsem[i], prev))
            self.waited[(q, ('d', i))] = prev
        self.dcnt[i] += 16
        tok = (('d', i), self.dcnt[i], 'dma')
        sem = self.dsem[i]

        def emit(eng):
            for s, v in waits:
                eng.wait_ge(s, v)
            eng.dma_start(out=out, in_=in_).then_inc(sem, 16)
        self.prog[q].append(emit)
        self._commit(tok, reads, writes)

    def barrier(self):
        allw = [(('e', e), self.cnt[e]) for e in self.ce if self.cnt[e] > 0]
        allw += [(('d', i), self.dcnt[i]) for i in range(NDS) if self.dcnt[i] > 0]
        for e in self.ce + ['sp']:
            waits = []
            for sk, v in allw:
                if sk == ('e', e) and e == 'pe':
                    continue
                if self.waited.get((e, sk), 0) >= v:
                    continue
                self.waited[(e, sk)] = v
                waits.append((self._semh(sk), v))

            def emit(eng, waits=waits):
                for s, v in waits:
                    eng.wait_ge(s, v)
            self.prog[e].append(emit)
        self.lastw = {}
        self.readers = {}

    def finish(self):
        self.barrier()
        nc = self.nc
        prog = self.prog
        with nc.Block() as block:
            @block.sync
            def _(eng):
                for f in prog['sp']:
                    f(eng)

            @block.tensor
            def _(eng):
                for f in prog['pe']:
                    f(eng)

            @block.scalar
            def _(eng):
                for f in prog['act']:
                    f(eng)

            @block.vector
            def _(eng):
                for f in prog['dve']:
                    f(eng)

            @block.gpsimd
            def _(eng):
                for f in prog['pool']:
                    f(eng)


class Arena:
    def __init__(self, nc, nbytes):
        self.ap = nc.alloc_sbuf_tensor("arena", [128, nbytes], U8).ap()
        self.off = 0
        self.nbytes = nbytes

    def alloc(self, free_shape, dtype):
        esz = 4 if dtype == F32 else 2
        n = int(np.prod(free_shape))
        nb = (n * esz + 31) // 32 * 32
        assert self.off + nb <= self.nbytes, ("arena overflow", self.off, nb)
        v = self.ap[:, self.off:self.off + n * esz].bitcast(dtype)
        self.off += nb
        if len(free_shape) == 2:
            v = v.rearrange("p (a b) -> p a b", b=free_shape[1])
        elif len(free_shape) == 3:
            v = v.rearrange("p (a b c) -> p a b c", b=free_shape[1], c=free_shape[2])
        return v


def build_nc(nhalo=16, nmain=32, groups=((0, 11), (11, 22), (22, 33))):
    global NHALO, NMAIN, NTILES, GROUPS
    NHALO, NMAIN, NTILES, GROUPS = nhalo, nmain, nmain + 1, list(groups)
    nc = bass.Bass("TRN2", target_bir_lowering=False)
    S = Sched(nc)

    def din(name, shape, dt=F32):
        return nc.dram_tensor(name, list(shape), dt, kind="ExternalInput").ap()

    def dout(name, shape, dt=F32):
        return nc.dram_tensor(name, list(shape), dt, kind="ExternalOutput").ap()

    xp = din("xp", [(NHALO + NMAIN) * 128, D])
    xs = din("xs", [NSAMP, D])
    ck = din("ck", [NSAMP, 2048, 512])
    cv = din("cv", [NSAMP, 2048, 512])
    w_in = din("w_in", [D, 2560])
    w_out = din("w_out", [D, D])
    w_r = din("w_r", [D, 36])
    w_gate = din("w_gate", [32, D, 256])
    w_up = din("w_up", [32, D, 256])
    w_down = din("w_down", [32, 256, D])
    wsT_d = din("wsT", [128, 8, 128])
    g1c_d = din("g1c", [128, 8])
    g2c_d = din("g2c", [128, 8])
    ogc_d = din("ogc", [128, 8])
    gqk_d = din("gqk", [128, 8])
    vgain_d = din("vgain", [128, 512])
    bst_d = din("bst", [128, 8])
    ws00_d = din("ws00", [128, 8])
    bs0_d = din("bs0", [128, 8])
    br_d = din("br", [128, 36])
    flag_d = din("flag", [128, 1])
    c_ident = din("c_ident", [128, 128])
    c_bones = din("c_bones", [128, 128])
    c_tri = din("c_tri", [128, 128])
    c_mult = din("c_mult", [128, NOFF, 128])

    y_p = dout("y_p", [NMAIN * 128, D])
    y_s = dout("y_s", [NSAMP, D])
    nk_o = dout("nk", [2048, 512])
    nv_o = dout("nv", [2048, 512])
    nks_o = dout("nks", [NSAMP, 512])
    nvs_o = dout("nvs", [NSAMP, 512])
    nva_o = dout("nva", [NSAMP, 512])

    hs = nc.dram_tensor("hs", [NTILES * 128, D], F32, kind="Internal").ap()
    hnts = nc.dram_tensor("hnts", [128, NTILES, 8, 128], BF16, kind="Internal").ap()

    ar = Arena(nc, 200 * 1024)
    G = ar.alloc([NTILES, 32], F32)
    g2c = ar.alloc([8], F32)
    persist_end = ar.off

    psA = nc.alloc_psum_tensor("psA", [128, 8, 128], F32).ap()
    psB = nc.alloc_psum_tensor("psB", [128, 8, 128], F32).ap()
    psC = nc.alloc_psum_tensor("psC", [128, 2, 512], F32).ap()
    psT = nc.alloc_psum_tensor("psT", [128, 8, 128], BF16).ap()
    psE = nc.alloc_psum_tensor("psE", [128, 512], F32).ap()
    psA_flat = psA.rearrange("p a b -> p (a b)")
    psB_flat = psB.rearrange("p a b -> p (a b)")

    WIN = ar.alloc([8, 2560], BF16)
    WOUT = ar.alloc([8, 1024], BF16)
    WR = ar.alloc([8, 36], BF16)
    WST = ar.alloc([8, 128], BF16)
    MULT = ar.alloc([NOFF, 128], BF16)
    MULTH = ar.alloc([NOFF, 128], BF16)
    identf = ar.alloc([128], F32)
    identb = ar.alloc([128], BF16)
    bonesb = ar.alloc([128], BF16)
    ONES16 = ar.alloc([128], F32)
    g1c = ar.alloc([8], F32)
    ogc = ar.alloc([8], F32)
    GQK = ar.alloc([8], F32)
    VGAIN = ar.alloc([512], F32)
    BST = ar.alloc([8], F32)
    WS00 = ar.alloc([8], F32)
    BS0 = ar.alloc([8], F32)
    BR = ar.alloc([36], F32)
    FLAG = ar.alloc([1], F32)
    EPSC = ar.alloc([1], F32)
    stg_off = ar.off
    STG = [ar.alloc([2560], F32) for _ in range(2)]

    def small_load(dst, src, key):
        S.dma(out=dst, in_=src, writes=[key])

    small_load(g1c, g1c_d, 'g1c')
    small_load(g2c, g2c_d, 'g2c')
    small_load(ogc, ogc_d, 'ogc')
    small_load(GQK, gqk_d, 'gqk')
    small_load(VGAIN, vgain_d, 'vgain')
    small_load(BST, bst_d, 'bst')
    small_load(WS00, ws00_d, 'ws00')
    small_load(BS0, bs0_d, 'bs0')
    small_load(BR, br_d, 'br')
    small_load(FLAG, flag_d, 'flag')
    small_load(identf, c_ident, 'identf')
    S.op('dve', lambda e: e.memset(ONES16, 1.0), writes=['ones16'])
    S.op('dve', lambda e: e.memset(EPSC, EPS), writes=['epsc'])
    S.op('dve', lambda e: e.memset(VA[:, :, :, 64:65], 1.0), writes=['va_ones'])
    for i in range(6):
        S.op('pool', lambda e, i=i: e.memset(VAS[i][:, :, 64:65], 1.0), writes=[('vas', i)])
    S.op('dve', lambda e: e.tensor_copy(out=identb, in_=identf), reads=['identf'], writes=['identb'])
    S.dma(out=STG[0][:, 0:128], in_=c_bones, writes=[('stg', 0)])
    S.op('dve', lambda e: e.tensor_copy(out=bonesb, in_=STG[0][:, 0:128]), reads=[('stg', 0)], writes=['bonesb'])
    S.dma(out=STG[1][:, 0:NOFF * 128], in_=c_mult.rearrange("p a b -> p (a b)"), writes=[('stg', 1)])
    S.op('dve', lambda e: e.tensor_copy(out=MULT.rearrange("p a b -> p (a b)"), in_=STG[1][:, 0:NOFF * 128]),
         reads=[('stg', 1)], writes=['mult'])
    S.op('dve', lambda e: e.tensor_scalar(out=MULTH.rearrange("p a b -> p (a b)"), in0=STG[1][:, 0:NOFF * 128],
                                          scalar1=FLAG[:, 0:1], scalar2=None, op0=ALU.mult),
         reads=[('stg', 1), 'flag'], writes=['multh'])
    S.dma(out=STG[0][:, 0:1024], in_=wsT_d.rearrange("p a b -> p (a b)"), reads=['bonesb'], writes=[('stg', 0)])
    S.dma(out=STG[0][:, 1024:1152], in_=c_tri, writes=[('stg', 0)])
    S.op('dve', lambda e: e.tensor_tensor(
        out=WST, in0=STG[0][:, 0:1024].rearrange("p (a b) -> p a b", b=128),
        in1=STG[0][:, 1024:1152].unsqueeze(1).to_broadcast([128, 8, 128]), op=ALU.mult),
        reads=[('stg', 0)], writes=['wst'])
    S.dma(out=STG[1][:, 0:288].rearrange("p (a b) -> p a b", b=36), in_=w_r.rearrange("(kc p) f -> p kc f", p=128),
          reads=['mult', 'multh'], writes=[('stg', 1)])
    S.op('dve', lambda e: e.tensor_tensor(
        out=WR, in0=STG[1][:, 0:288].rearrange("p (a b) -> p a b", b=36),
        in1=g2c.unsqueeze(2).to_broadcast([128, 8, 36]), op=ALU.mult),
        reads=[('stg', 1), 'g2c'], writes=['wr'])
    for kc in range(8):
        st = STG[kc % 2]
        S.dma(out=st[:, 0:2560], in_=w_in[kc * 128:(kc + 1) * 128, :], reads=['wst', 'wr'], writes=[('stg', kc % 2)])
        eng = 'dve' if kc % 2 == 0 else 'pool'
        S.op(eng, lambda e, st=st, kc=kc: e.tensor_scalar(out=WIN[:, kc, :], in0=st[:, 0:2560], scalar1=g1c[:, kc:kc + 1],
                                                           scalar2=None, op0=ALU.mult),
             reads=[('stg', kc % 2), 'g1c'], writes=['win'])
    for kc in range(8):
        st = STG[kc % 2]
        S.dma(out=st[:, 0:1024], in_=w_out[kc * 128:(kc + 1) * 128, :], writes=[('stg', kc % 2)])
        eng = 'dve' if kc % 2 == 0 else 'pool'
        S.op(eng, lambda e, st=st, kc=kc: e.tensor_scalar(out=WOUT[:, kc, :], in0=st[:, 0:1024], scalar1=ogc[:, kc:kc + 1],
                                                           scalar2=None, op0=ALU.mult),
             reads=[('stg', kc % 2), 'ogc'], writes=['wout'])

    S.barrier()
    ar.off = stg_off
    XT = [ar.alloc([1024], F32) for _ in range(2)]
    xn = ar.alloc([1024], BF16)
    xnT = ar.alloc([8, 128], BF16)
    sq = ar.alloc([8, 128], BF16)
    rr = ar.alloc([8, 128], F32)
    qkn = ar.alloc([8, 128], F32)
    QT = ar.alloc([2, 4, 128], BF16)
    KT = ar.alloc([RING, 4, 128], BF16)
    VA = ar.alloc([RING, 8, 65], BF16)
    kout = ar.alloc([1024], F32)
    vout = ar.alloc([512], F32)
    u_t = ar.alloc([512], F32)
    gg = ar.alloc([512], F32)
    gsq = ar.alloc([512], F32)
    vg = ar.alloc([512], F32)
    vgb = ar.alloc([512], BF16)
    oa = ar.alloc([512], F32)
    ob = ar.alloc([512], F32)
    oab = ar.alloc([1024], BF16)
    oT = ar.alloc([8, 128], BF16)
    P = [ar.alloc([8, 128], BF16) for _ in range(2)]
    hh = ar.alloc([1024], F32)
    hn = ar.alloc([1024], BF16)
    hnT = ar.alloc([8, 128], BF16)
    junk = ar.alloc([1024], BF16)
    ST = ar.alloc([64], F32)
    RL = ar.alloc([96], F32)
    KC = [ar.alloc([512], F32) for _ in range(2)]
    VC = [ar.alloc([512], F32) for _ in range(2)]
    prod = ar.alloc([512], F32)
    SALL = ar.alloc([24], F32)
    VAS = [ar.alloc([8, 65], BF16) for _ in range(6)]
    PZ = [ar.alloc([24, 16], BF16) for _ in range(2)]
    stage1_end = ar.off
    print('stage1 arena bytes', stage1_end)

    class Defer:
        def __init__(self):
            self.q = []

        def op(self, *a, **k):
            self.q.append(lambda: S.op(*a, **k))

        def dma(self, *a, **k):
            self.q.append(lambda: S.dma(*a, **k))

        def pop(self, n):
            for _ in range(n):
                if self.q:
                    self.q.pop(0)()

        def flush(self):
            self.pop(len(self.q))

    DQ = Defer()

    def rstd(nt, src, dst, scale, tmpkey, Q=None):
        Q = Q or S
        Q.op('act', lambda e: e.activation(out=dst, in_=src, func=AF.Sqrt, bias=EPSC[:nt, 0:1], scale=scale),
             reads=['st'], writes=['st'])
        Q.op('dve', lambda e: e.reciprocal(out=dst, in_=dst), reads=['st'], writes=['st'])

    def transposes_bf(nt, src, dstT, rkey, wkey):
        def f(e):
            last = None
            for kc in range(8):
                last = e.transpose(out=psT[:, kc, :nt], in_=src[:nt, kc * 128:(kc + 1) * 128], identity=identb[:nt, :nt])
            return last
        S.op('pe', f, reads=[rkey], writes=['psT'])
        S.op('act', lambda e: e.copy(out=dstT[:, :, :nt], in_=psT[:, :, :nt]), reads=['psT'], writes=[wkey])

    def v3(ap, nt):
        return ap[:nt].rearrange("p (h d) -> p h d", d=64)

    def v4(ap, nt):
        return ap[:nt].rearrange("p (b h d) -> p b h d", b=2, d=64)

    psO = psC
    psO_num = psC[:, :, 0:260].rearrange("p b (h e) -> p b h e", e=65)[:, :, :, 0:64]
    psO_den = psC[:, :, 64:260:65]

    def load_x(g):
        if g == NHALO + NMAIN:
            S.dma(out=XT[g % 2][:NSAMP], in_=xs, writes=[('xt', g % 2)])
        else:
            S.dma(out=XT[g % 2], in_=xp[g * 128:(g + 1) * 128, :], writes=[('xt', g % 2)])

    import os
    CUT = int(os.environ.get('K_CUT', '99'))
    CUT2 = int(os.environ.get('K_CUT2', '99'))
    CUT3 = int(os.environ.get('K_CUT3', '99'))

    def do_tile(g, kind):
        nt = NSAMP if kind == 'sample' else 128
        xt = XT[g % 2]
        xtk = ('xt', g % 2)
        mt = g - NHALO if kind != 'sample' else NMAIN
        if g == 0:
            load_x(0)
        if g + 1 <= NHALO + NMAIN:
            load_x(g + 1)
        S.op('act', lambda e: e.activation(out=junk[:nt], in_=xt[:nt], func=AF.Square, accum_out=ST[:nt, 0:1]),
             reads=[xtk], writes=['junk', 'st'])
        rstd(nt, ST[:nt, 0:1], ST[:nt, 1:2], 1.0 / D, 'st')
        S.op('dve', lambda e: e.tensor_scalar(out=xn[:nt], in0=xt[:nt], scalar1=ST[:nt, 1:2], scalar2=None, op0=ALU.mult),
             reads=[xtk, 'st'], writes=['xn'])
        transposes_bf(nt, xn, xnT, 'xn', 'xnT')
        fcs = list(range(4, 8)) if kind == 'halo' else list(range(8))
        f0, f1 = fcs[0], fcs[-1] + 1
        nf = f1 - f0

        def fqk(e):
            last = None
            for fc in fcs:
                for kc in range(8):
                    last = e.matmul(psA[:, fc, :nt], lhsT=WIN[:, kc, fc * 128:(fc + 1) * 128], rhs=xnT[:, kc, :nt],
                                    start=(kc == 0), stop=(kc == 7))
            return last
        S.op('pe', fqk, reads=['xnT', 'win'], writes=['psA'])

        def fv(e):
            last = None
            dsts = [(psE, 1024)] if kind == 'halo' else [(psE, 1024), (psC[:, 0, :], 1536), (psC[:, 1, :], 2048)]
            for dst, c0 in dsts:
                for kc in range(8):
                    last = e.matmul(dst[:nt, 0:512], lhsT=xnT[:, kc, :nt], rhs=WIN[:, kc, c0:c0 + 512],
                                    start=(kc == 0), stop=(kc == 7))
            return last
        S.op('pe', fv, reads=['xnT', 'win'], writes=['psE'] if kind == 'halo' else ['psE', 'psC'])
        S.op('act', lambda e: e.activation(out=sq[:, f0:f1, :nt], in_=psA[:, f0:f1, :nt], func=AF.Square),
             reads=['psA'], writes=['sq'])

        def fss(e):
            last = None
            for fc in fcs:
                last = e.matmul(psB[:, fc, :nt], lhsT=bonesb, rhs=sq[:, fc, :nt], start=True, stop=True)
            return last
        S.op('pe', fss, reads=['sq', 'bonesb'], writes=['psB'])
        S.op('act', lambda e: e.activation(out=rr[:, f0:f1, :nt], in_=psB[:, f0:f1, :nt], func=AF.Sqrt, bias=EPSC[:, 0:1],
                                           scale=1.0 / 64), reads=['psB'], writes=['rr'])
        S.op('dve', lambda e: e.reciprocal(out=rr[:, f0:f1, :nt], in_=rr[:, f0:f1, :nt]), reads=['rr'], writes=['rr'])
        S.op('dve', lambda e: e.tensor_tensor(out=qkn[:, f0:f1, :nt], in0=psA[:, f0:f1, :nt],
                                              in1=GQK[:, f0:f1].unsqueeze(2).to_broadcast([128, nf, nt]), op=ALU.mult),
             reads=['psA', 'gqk'], writes=['qkn'])
        S.op('dve', lambda e: e.tensor_tensor(out=qkn[:, f0:f1, :nt], in0=qkn[:, f0:f1, :nt], in1=rr[:, f0:f1, :nt],
                                              op=ALU.mult), reads=['qkn', 'rr'], writes=['qkn'])
        slot = g % RING
        if kind != 'sample':
            S.op('dve', lambda e: e.tensor_copy(out=KT[:, slot, :, :], in_=qkn[:, 4:8, :]), reads=['qkn'],
                 writes=[('kt', slot)])
        if kind == 'main':
            if g == NHALO:
                S.op('dve', lambda e: e.memset(QT, 0.0), writes=['qt'])
            S.op('act', lambda e: e.copy(out=QT[0:64, 0, :, :], in_=qkn[0:64, 0:4, :]), reads=['qkn'], writes=['qt'])
            S.op('act', lambda e: e.copy(out=QT[64:128, 1, :, :], in_=qkn[64:128, 0:4, :]), reads=['qkn'],
                 writes=['qt'])
        if kind == 'main' and CUT <= 1:
            return
        KO0 = NHALO + max(NMAIN - 16, 0)
        want_kout = (kind == 'sample') or (kind == 'main' and g >= KO0)
        if want_kout:
            tf = list(range(8)) if kind == 'sample' else list(range(4, 8))

            def ftr(e):
                last = None
                for fc in tf:
                    last = e.transpose(out=psB_flat[:nt, fc * 128:(fc + 1) * 128], in_=qkn[:, fc, :nt], identity=identf)
                return last
            S.op('pe', ftr, reads=['qkn', 'identf'], writes=['psB'])
            t0 = tf[0] * 128
            S.op('dve', lambda e: e.tensor_copy(out=kout[:nt, t0:1024], in_=psB_flat[:nt, t0:1024]), reads=['psB'],
                 writes=['kout'])
            if kind == 'sample':
                S.dma(out=nks_o, in_=kout[:nt, 512:1024], reads=['kout'])
            else:
                r0 = (g - KO0) * 128
                S.dma(out=nk_o[r0:r0 + 128, :], in_=kout[:, 512:1024], reads=['kout'])
        if kind == 'main' and CUT <= 2:
            return
        if kind != 'sample':
            S.op('act', lambda e: e.copy(out=VA[:nt, slot, :, 0:64], in_=v3(psE, nt)), reads=['psE', 'va_ones'],
                 writes=[('va', slot)])
        if want_kout:
            S.op('dve', lambda e: e.tensor_copy(out=vout[:nt], in_=psE[:nt, 0:512]), reads=['psE'], writes=['vout'])
            if kind == 'sample':
                S.dma(out=nvs_o, in_=vout[:nt], reads=['vout'])
            else:
                r0 = (g - KO0) * 128
                S.dma(out=nv_o[r0:r0 + 128, :], in_=vout, reads=['vout'])
        if kind == 'main' and CUT <= 3:
            return
        if kind == 'halo':
            return
        S.op('act', lambda e: e.activation(out=u_t[:nt], in_=psC[:nt, 0, :], func=AF.Gelu_apprx_tanh), reads=['psC'],
             writes=['u'])
        S.op('act', lambda e: e.activation(out=gg[:nt], in_=psC[:nt, 1, :], func=AF.Gelu_apprx_tanh), reads=['psC'],
             writes=['gg'])
        Q = DQ if kind == 'main' else S
        if kind == 'sample':
            DQ.flush()
        Q.op('dve', lambda e: e.tensor_tensor(out=gsq[:nt], in0=gg[:nt], in1=gg[:nt], op=ALU.mult), reads=['gg'],
             writes=['gsq'])
        Q.op('dve', lambda e: e.tensor_reduce(out=ST[:nt, 8:16], in_=v3(gsq, nt), axis=AX.X, op=ALU.add), reads=['gsq'],
             writes=['st'])
        rstd(nt, ST[:nt, 8:16], ST[:nt, 16:24], 1.0 / 64, 'st', Q)
        Q.op('dve', lambda e: e.tensor_tensor(out=v3(vg, nt), in0=v3(gg, nt),
                                              in1=ST[:nt, 16:24].unsqueeze(2).to_broadcast([nt, 8, 64]), op=ALU.mult),
             reads=['gg', 'st'], writes=['vg'])
        Q.op('dve', lambda e: e.tensor_tensor(out=vg[:nt], in0=vg[:nt], in1=VGAIN[:nt], op=ALU.mult),
             reads=['vg', 'vgain'], writes=['vg'])
        if kind == 'sample':
            Q.dma(out=nva_o, in_=vg[:nt], reads=['vg'])
            Q.op('dve', lambda e: e.tensor_tensor(out=v3(oa, nt), in0=v3(vg, nt),
                                                  in1=WS00[:nt].unsqueeze(2).to_broadcast([nt, 8, 64]), op=ALU.mult),
                 reads=['vg', 'ws00'], writes=['oa'])
            Q.op('dve', lambda e: e.tensor_tensor(out=v3(oa, nt), in0=v3(oa, nt),
                                                  in1=BS0[:nt].unsqueeze(2).to_broadcast([nt, 8, 64]), op=ALU.add),
                 reads=['oa', 'bs0'], writes=['oa'])
        else:
            Q.op('act', lambda e: e.copy(out=vgb, in_=vg), reads=['vg'], writes=['vgb'])

            def fmix(e):
                last = None
                for h in range(8):
                    last = e.matmul(psE[:, h * 64:(h + 1) * 64], lhsT=WST[:, h, :], rhs=vgb[:, h * 64:(h + 1) * 64],
                                    start=True, stop=True)
                return last
            Q.op('pe', fmix, reads=['vgb', 'wst'], writes=['psE'])
            Q.op('dve', lambda e: e.tensor_tensor(out=v3(oa, nt), in0=v3(psE, nt),
                                                  in1=BST.unsqueeze(2).to_broadcast([128, 8, 64]), op=ALU.add),
                 reads=['psE', 'bst'], writes=['oa'])
        Q.op('dve', lambda e: e.tensor_tensor(out=oa[:nt], in0=oa[:nt], in1=u_t[:nt], op=ALU.mult), reads=['oa', 'u'],
             writes=['oa'])
        Q.op('act', lambda e: e.activation(out=junk[:nt, 0:512], in_=oa[:nt], func=AF.Square, accum_out=ST[:nt, 24:25]),
             reads=['oa'], writes=['junk', 'st'])
        rstd(nt, ST[:nt, 24:25], ST[:nt, 25:26], 1.0 / 512, 'st', Q)
        Q.op('dve', lambda e: e.tensor_scalar(out=oab[:nt, 0:512], in0=oa[:nt], scalar1=ST[:nt, 25:26], scalar2=None,
                                              op0=ALU.mult), reads=['oa', 'st'], writes=['oab'])
        if kind == 'main' and CUT <= 4:
            return
        if kind == 'main':
            def s_op(i):
                o = NOFF - 1 - i
                kt = g - o
                sl = kt % RING
                ps = psA if i % 2 == 0 else psB
                pk = 'psA' if i % 2 == 0 else 'psB'

                def f(e):
                    last = None
                    for h in range(8):
                        a, hp = h % 2, h // 2
                        last = e.matmul(ps[:, h, :], lhsT=KT[:, sl, hp, :], rhs=QT[:, a, hp, :], start=True, stop=True)
                    return last
                S.op('pe', f, reads=[('kt', sl), 'qt'], writes=[pk])
            s_op(0)
            for i in range(NOFF):
                o = NOFF - 1 - i
                kt = g - o
                sl = kt % RING
                ps = psA if i % 2 == 0 else psB
                pk = 'psA' if i % 2 == 0 else 'psB'
                Pi = P[i % 2]
                pik = ('P', i % 2)
                if i + 1 < NOFF:
                    s_op(i + 1)
                if CUT3 < 2:
                    continue
                S.op('act', lambda e, ps=ps, Pi=Pi: e.activation(out=Pi, in_=ps, func=AF.Exp, scale=0.125), reads=[pk],
                     writes=[pik])
                if CUT3 < 3:
                    continue
                M = MULTH if kt < NHALO else MULT
                S.op('dve', lambda e, Pi=Pi, M=M, o=o: e.tensor_tensor(
                    out=Pi, in0=Pi, in1=M[:, o, :].unsqueeze(1).to_broadcast([128, 8, 128]), op=ALU.mult),
                    reads=[pik, 'mult', 'multh'], writes=[pik])

                if CUT3 < 4:
                    continue

                def fpv(e, Pi=Pi, sl=sl, i=i):
                    last = None
                    for h in range(8):
                        last = e.matmul(psO[:, h // 4, (h % 4) * 65:(h % 4) * 65 + 65], lhsT=Pi[:, h, :],
                                        rhs=VA[:, sl, h, :], start=(i == 0 and h % 4 == 0), stop=(i == NOFF - 1 and h % 4 == 3),
                                        skip_group_check=True)
                    return last
                S.op('pe', fpv, reads=[pik, ('va', sl)], writes=['psC'])
                DQ.pop(-(-len(DQ.q) // (NOFF - i)))
            DQ.flush()
            if CUT3 < 5:
                return
            S.op('dve', lambda e: e.reciprocal(out=ST[:, 32:40].rearrange("p (b h) -> p b h", b=2), in_=psO_den),
                 reads=['psC'], writes=['st'])
            if CUT3 < 6:
                return
            S.op('dve', lambda e: e.tensor_tensor(
                out=v4(ob, nt), in0=psO_num,
                in1=ST[:, 32:40].rearrange("p (b h) -> p b h", b=2).unsqueeze(3).to_broadcast([128, 2, 4, 64]),
                op=ALU.mult), reads=['psC', 'st'], writes=['ob'])
        else:
            sample_attention()
        if kind == 'main' and CUT <= 5:
            return
        S.op('act', lambda e: e.activation(out=junk[:nt, 0:512], in_=ob[:nt], func=AF.Square, accum_out=ST[:nt, 26:27]),
             reads=['ob'], writes=['junk', 'st'])
        rstd(nt, ST[:nt, 26:27], ST[:nt, 27:28], 1.0 / 512, 'st')
        S.op('dve', lambda e: e.tensor_scalar(out=oab[:nt, 512:1024], in0=ob[:nt], scalar1=ST[:nt, 27:28], scalar2=None,
                                              op0=ALU.mult), reads=['ob', 'st'], writes=['oab'])
        transposes_bf(nt, oab, oT, 'oab', 'oT')

        def fwo(e):
            last = None
            for half in range(2):
                for kc in range(8):
                    last = e.matmul(psA_flat[:nt, half * 512:(half + 1) * 512], lhsT=oT[:, kc, :nt],
                                    rhs=WOUT[:, kc, half * 512:(half + 1) * 512], start=(kc == 0), stop=(kc == 7))
            return last
        S.op('pe', fwo, reads=['oT', 'wout'], writes=['psA'])
        S.op('dve', lambda e: e.tensor_tensor(out=hh[:nt], in0=psA_flat[:nt], in1=xt[:nt], op=ALU.add),
             reads=['psA', xtk], writes=['hh'])
        S.dma(out=hs[mt * 128:mt * 128 + nt, :], in_=hh[:nt], reads=['hh'], writes=[('hs', mt)])
        if kind == 'main' and CUT <= 6:
            return
        S.op('act', lambda e: e.activation(out=junk[:nt], in_=hh[:nt], func=AF.Square, accum_out=ST[:nt, 28:29]),
             reads=['hh'], writes=['junk', 'st'])
        rstd(nt, ST[:nt, 28:29], ST[:nt, 29:30], 1.0 / D, 'st')
        S.op('dve', lambda e: e.tensor_scalar(out=hn[:nt], in0=hh[:nt], scalar1=ST[:nt, 29:30], scalar2=None, op0=ALU.mult),
             reads=['hh', 'st'], writes=['hn'])
        transposes_bf(nt, hn, hnT, 'hn', 'hnT')
        S.dma(out=hnts[:, mt, :, :nt], in_=hnT[:, :, :nt], reads=['hnT'], writes=[('hnts', mt)])
        if kind == 'main' and CUT <= 7:
            return

        def frt(e):
            last = None
            for kc in range(8):
                last = e.matmul(psE[:nt, 0:36], lhsT=hnT[:, kc, :nt], rhs=WR[:, kc, :], start=(kc == 0), stop=(kc == 7))
            return last
        S.op('pe', frt, reads=['hnT', 'wr'], writes=['psE'])
        L = RL[:nt, 0:36]
        S.op('dve', lambda e: e.tensor_tensor(out=L, in0=psE[:nt, 0:36], in1=BR[:nt], op=ALU.add), reads=['psE', 'br'],
             writes=['rl'])
        RQ = DQ if kind == 'main' else S
        RQ.op('dve', lambda e: e.tensor_reduce(out=RL[:nt, 36:37], in_=RL[:nt, 0:4], axis=AX.X, op=ALU.max), reads=['rl'],
             writes=['rl'])
        RQ.op('dve', lambda e: e.tensor_scalar(out=RL[:nt, 40:44], in0=RL[:nt, 0:4], scalar1=RL[:nt, 36:37], scalar2=None,
                                              op0=ALU.is_equal), reads=['rl'], writes=['rl'])
        RQ.op('dve', lambda e: e.tensor_scalar(out=RL[:nt, 37:38], in0=RL[:nt, 36:37], scalar1=-1.0, scalar2=None,
                                              op0=ALU.mult), reads=['rl'], writes=['rl'])
        RQ.op('act', lambda e: e.activation(out=RL[:nt, 44:48], in_=RL[:nt, 0:4], func=AF.Exp, bias=RL[:nt, 37:38], scale=1.0,
                                           accum_out=RL[:nt, 38:39]), reads=['rl'], writes=['rl'])
        RQ.op('dve', lambda e: e.reciprocal(out=RL[:nt, 39:40], in_=RL[:nt, 38:39]), reads=['rl'], writes=['rl'])
        RQ.op('dve', lambda e: e.tensor_tensor(
            out=RL[:nt, 48:80].rearrange("p (g x) -> p g x", x=8), in0=RL[:nt, 4:36].rearrange("p (g x) -> p g x", x=8),
            in1=RL[:nt, 40:44].unsqueeze(2).to_broadcast([nt, 4, 8]), op=ALU.mult), reads=['rl'], writes=['rl'])
        RQ.op('dve', lambda e: e.tensor_reduce(out=RL[:nt, 80:88], in_=RL[:nt, 48:80].rearrange("p (g x) -> p x g", x=8),
                                              axis=AX.X, op=ALU.add), reads=['rl'], writes=['rl'])
        RQ.op('dve', lambda e: e.max(out=RL[:nt, 88:96], in_=RL[:nt, 80:88]), reads=['rl'], writes=['rl'])
        RQ.op('dve', lambda e: e.tensor_tensor(out=ST[:nt, 40:41], in0=RL[:nt, 89:90], in1=RL[:nt, 88:89], op=ALU.subtract),
             reads=['rl'], writes=['st'])
        RQ.op('act', lambda e: e.activation(out=ST[:nt, 41:42], in_=ST[:nt, 40:41], func=AF.Exp), reads=['st'], writes=['st'])
        RQ.op('dve', lambda e: e.tensor_scalar(out=ST[:nt, 41:42], in0=ST[:nt, 41:42], scalar1=1.0, scalar2=None, op0=ALU.add),
             reads=['st'], writes=['st'])
        RQ.op('dve', lambda e: e.reciprocal(out=ST[:nt, 42:43], in_=ST[:nt, 41:42]), reads=['st'], writes=['st'])
        RQ.op('dve', lambda e: e.tensor_scalar(out=ST[:nt, 43:44], in0=ST[:nt, 42:43], scalar1=-1.0, scalar2=1.0, op0=ALU.mult,
                                              op1=ALU.add), reads=['st'], writes=['st'])
        RQ.op('dve', lambda e: e.tensor_tensor(out=ST[:nt, 42:44], in0=ST[:nt, 42:44],
                                              in1=RL[:nt, 39:40].to_broadcast([nt, 2]), op=ALU.mult), reads=['st', 'rl'],
             writes=['st'])
        RQ.op('dve', lambda e: e.tensor_scalar(out=ST[:nt, 44:52], in0=RL[:nt, 80:88], scalar1=RL[:nt, 88:89],
                                              scalar2=ST[:nt, 42:43], op0=ALU.is_equal, op1=ALU.mult), reads=['rl', 'st'],
             writes=['st'])
        RQ.op('dve', lambda e: e.tensor_scalar(out=ST[:nt, 52:60], in0=RL[:nt, 80:88], scalar1=RL[:nt, 89:90],
                                              scalar2=ST[:nt, 43:44], op0=ALU.is_equal, op1=ALU.mult), reads=['rl', 'st'],
             writes=['st'])
        RQ.op('dve', lambda e: e.tensor_tensor(out=ST[:nt, 44:52], in0=ST[:nt, 44:52], in1=ST[:nt, 52:60], op=ALU.add),
             reads=['st'], writes=['st'])
        RQ.op('dve', lambda e: e.tensor_tensor(
            out=G[:nt, mt, :].rearrange("p (g x) -> p g x", x=8),
            in0=RL[:nt, 40:44].unsqueeze(2).to_broadcast([nt, 4, 8]),
            in1=ST[:nt, 44:52].unsqueeze(1).to_broadcast([nt, 4, 8]), op=ALU.mult), reads=['rl', 'st'], writes=['G'])

    def sample_attention():
        nt = NSAMP
        qtok = kout[:nt, 0:512]
        ktok = kout[:nt, 512:1024]
        rows = [(1920, 1), (1536, 4), (0, 16)]
        for b in range(NSAMP):
            S.op('dve', lambda e, b=b: e.tensor_scalar(out=gsq[:nt], in0=qtok, scalar1=identf[:nt, b:b + 1], scalar2=None,
                                                        op0=ALU.mult), reads=['kout', 'identf'], writes=['gsq'])

            def fqb(e, b=b):
                return e.matmul(psE[:, 0:512], lhsT=ONES16[0:16, :], rhs=gsq[:nt], start=True, stop=True)
            S.op('pe', fqb, reads=['gsq', 'ones16'], writes=['psE'])
            pz = PZ[b % 2]
            pzk = ('pz', b % 2)
            S.op('pool', lambda e, pz=pz: e.memset(pz, 0.0), writes=[pzk])
            for c, (r0, stp) in enumerate(rows):
                bi = (b * 3 + c) % 2
                kc_t, vc_t = KC[bi], VC[bi]
                S.dma(out=kc_t, in_=ck[b, r0:r0 + 128 * stp:stp, :], writes=[('kc', bi)])
                S.dma(out=vc_t, in_=cv[b, r0:r0 + 128 * stp:stp, :], writes=[('vc', bi)])
                S.op('dve', lambda e, kc_t=kc_t: e.tensor_tensor(out=prod, in0=kc_t, in1=psE[:, 0:512], op=ALU.mult),
                     reads=[('kc', bi), 'psE'], writes=['prod'])
                S.op('dve', lambda e, c=c: e.tensor_reduce(out=SALL[:, c * 8:(c + 1) * 8],
                                                           in_=prod.rearrange("p (h d) -> p h d", d=64), axis=AX.X,
                                                           op=ALU.add), reads=['prod'], writes=['sall'])
                vi = (b % 2) * 3 + c
                S.op('act', lambda e, vi=vi, vc_t=vc_t: e.copy(out=VAS[vi][:, :, 0:64],
                                                               in_=vc_t.rearrange("p (h d) -> p h d", d=64)),
                     reads=[('vc', bi), ('vas', vi)], writes=[('vas', vi)])
            S.op('act', lambda e, pz=pz, b=b: e.activation(out=pz[:, :, b], in_=SALL[:, 0:24], func=AF.Exp, scale=0.125),
                 reads=['sall', pzk], writes=[pzk])

            def fpv(e, b=b, pz=pz):
                last = None
                for h in range(8):
                    for c in range(3):
                        vi = (b % 2) * 3 + c
                        last = e.matmul(psO[:nt, h // 4, (h % 4) * 65:(h % 4) * 65 + 65], lhsT=pz[:, c * 8 + h, :],
                                        rhs=VAS[vi][:, h, :], start=(b == 0 and c == 0 and h % 4 == 0),
                                        stop=(b == NSAMP - 1 and c == 2 and h % 4 == 3), skip_group_check=True)
                return last
            S.op('pe', fpv, reads=[pzk] + [('vas', (b % 2) * 3 + c) for c in range(3)], writes=['psC'])
        S.op('dve', lambda e: e.tensor_tensor(out=prod[:nt], in0=qtok, in1=ktok, op=ALU.mult), reads=['kout'], writes=['prod'])
        S.op('dve', lambda e: e.tensor_reduce(out=ST[:nt, 32:40], in_=prod[:nt].rearrange("p (h d) -> p h d", d=64),
                                              axis=AX.X, op=ALU.add), reads=['prod'], writes=['st'])
        S.op('act', lambda e: e.activation(out=ST[:nt, 32:40], in_=ST[:nt, 32:40], func=AF.Exp, scale=0.125), reads=['st'],
             writes=['st'])
        S.op('dve', lambda e: e.tensor_scalar(out=ST[:nt, 32:40], in0=ST[:nt, 32:40], scalar1=3.0, scalar2=None,
                                              op0=ALU.mult), reads=['st'], writes=['st'])
        es = ST[:nt, 32:40].rearrange("p (b h) -> p b h", b=2)
        dn = ST[:nt, 48:56].rearrange("p (b h) -> p b h", b=2)
        S.op('dve', lambda e: e.tensor_tensor(out=dn, in0=psO_den[:nt], in1=es, op=ALU.add), reads=['psC', 'st'],
             writes=['st'])
        S.op('dve', lambda e: e.reciprocal(out=dn, in_=dn), reads=['st'], writes=['st'])
        S.op('dve', lambda e: e.tensor_tensor(out=v4(ob, nt), in0=v4(vout, nt),
                                              in1=es.unsqueeze(3).to_broadcast([nt, 2, 4, 64]), op=ALU.mult),
             reads=['vout', 'st'], writes=['ob'])
        S.op('dve', lambda e: e.tensor_tensor(out=v4(ob, nt), in0=v4(ob, nt), in1=psO_num[:nt], op=ALU.add),
             reads=['ob', 'psC'], writes=['ob'])
        S.op('dve', lambda e: e.tensor_tensor(out=v4(ob, nt), in0=v4(ob, nt),
                                              in1=dn.unsqueeze(3).to_broadcast([nt, 2, 4, 64]), op=ALU.mult),
             reads=['ob', 'st'], writes=['ob'])

    import os
    UPTO = int(os.environ.get("K_UPTO", "9"))
    if UPTO >= 1:
        for g in range(NHALO):
            do_tile(g, 'halo')
    if UPTO >= 2:
        for g in range(NHALO, NHALO + NMAIN):
            do_tile(g, 'main')
    if UPTO >= 3:
        do_tile(NHALO + NMAIN, 'sample')
    if UPTO < 4:
        DQ.flush()
        S.finish()
        return nc

    DQ.flush()
    S.barrier()
    ar.off = persist_end
    NG = 11
    ACC = ar.alloc([NG, 1024], F32)
    HNT = ar.alloc([NG, 8, 128], BF16)
    WGf = [ar.alloc([8, 256], F32) for _ in range(2)]
    WUf = [ar.alloc([8, 256], F32) for _ in range(2)]
    WDf = [ar.alloc([2, 1024], F32) for _ in range(2)]
    WGb = [ar.alloc([8, 256], BF16) for _ in range(2)]
    WUb = [ar.alloc([8, 256], BF16) for _ in range(2)]
    WDb = [ar.alloc([2, 1024], BF16) for _ in range(2)]
    SIL = [ar.alloc([2, 128], F32) for _ in range(2)]
    HID = [ar.alloc([2, 128], BF16) for _ in range(2)]
    psAB = [psC[:, 0, :].rearrange("p (a b) -> p a b", b=128), psC[:, 1, :].rearrange("p (a b) -> p a b", b=128)]
    psD = [psA_flat, psB_flat]
    psDk = ['psA', 'psB']
    it = 0
    for (t0, t1) in GROUPS:
        for t in range(t0, t1):
            nt = NSAMP if t == NMAIN else 128
            j = t - t0
            S.dma(out=ACC[:nt, j, :], in_=hs[t * 128:t * 128 + nt, :], writes=[('acc', j)])
            S.dma(out=HNT[:, j, :, :nt], in_=hnts[:, t, :, :nt], writes=[('hnt', j)])
        def prep(ex):
            wb = ex % 2
            S.dma(out=WGf[wb], in_=w_gate[ex].rearrange("(kc p) f -> p kc f", p=128), writes=[('wgf', wb)])
            S.dma(out=WUf[wb], in_=w_up[ex].rearrange("(kc p) f -> p kc f", p=128), writes=[('wuf', wb)])
            S.dma(out=WDf[wb], in_=w_down[ex].rearrange("(kc p) f -> p kc f", p=128), writes=[('wdf', wb)])
            S.op('dve', lambda e, wb=wb: e.tensor_tensor(out=WGb[wb], in0=WGf[wb],
                                                          in1=g2c.unsqueeze(2).to_broadcast([128, 8, 256]), op=ALU.mult),
                 reads=[('wgf', wb)], writes=[('wgb', wb)])
            S.op('dve', lambda e, wb=wb: e.tensor_tensor(out=WUb[wb], in0=WUf[wb],
                                                          in1=g2c.unsqueeze(2).to_broadcast([128, 8, 256]), op=ALU.mult),
                 reads=[('wuf', wb)], writes=[('wub', wb)])
            S.op('act', lambda e, wb=wb: e.copy(out=WDb[wb], in_=WDf[wb]), reads=[('wdf', wb)], writes=[('wdb', wb)])

        units = [(ex, t) for ex in range(32) for t in range(t0, t1)]

        def gu(u):
            ex, t = units[u]
            wb = ex % 2
            nt = NSAMP if t == NMAIN else 128
            j = t - t0
            pab = psAB[u % 2]

            def fgu(e):
                last = None
                for m, W in enumerate((WGb[wb], WUb[wb])):
                    for fcx in range(2):
                        for kc in range(8):
                            last = e.matmul(pab[:, m * 2 + fcx, :nt], lhsT=W[:, kc, fcx * 128:(fcx + 1) * 128],
                                            rhs=HNT[:, j, kc, :nt], start=(kc == 0), stop=(kc == 7))
                return last
            S.op('pe', fgu, reads=[('wgb', wb), ('wub', wb), ('hnt', j)], writes=[('psab', u % 2)])

        prep(0)
        gu(0)
        for u, (ex, t) in enumerate(units):
            wb = ex % 2
            nt = NSAMP if t == NMAIN else 128
            j = t - t0
            pb = u % 2
            pab = psAB[pb]
            pabk = ('psab', pb)
            if t == t0 and ex + 1 < 32:
                prep(ex + 1)
            if u + 1 < len(units):
                gu(u + 1)
            S.op('act', lambda e, pb=pb, nt=nt, pab=pab: e.activation(out=SIL[pb][:, :, :nt], in_=pab[:, 0:2, :nt],
                                                                       func=AF.Silu), reads=[pabk], writes=[('sil', pb)])
            S.op('dve', lambda e, pb=pb, nt=nt, pab=pab: e.tensor_tensor(out=HID[pb][:, :, :nt], in0=SIL[pb][:, :, :nt],
                                                                          in1=pab[:, 2:4, :nt], op=ALU.mult),
                 reads=[pabk, ('sil', pb)], writes=[('hid', pb)])
            pd = psD[pb]

            def fdn(e, wb=wb, pb=pb, nt=nt, pd=pd):
                last = None
                for half in range(2):
                    for fcx in range(2):
                        last = e.matmul(pd[:nt, half * 512:(half + 1) * 512], lhsT=HID[pb][:, fcx, :nt],
                                        rhs=WDb[wb][:, fcx, half * 512:(half + 1) * 512], start=(fcx == 0),
                                        stop=(fcx == 1))
                return last
            S.op('pe', fdn, reads=[('hid', pb), ('wdb', wb)], writes=[psDk[pb]])
            S.op('dve', lambda e, j=j, nt=nt, pd=pd, t=t, ex=ex: e.scalar_tensor_tensor(
                out=ACC[:nt, j, :], in0=pd[:nt, :], scalar=G[:nt, t, ex:ex + 1], in1=ACC[:nt, j, :], op0=ALU.mult,
                op1=ALU.add), reads=[psDk[pb], ('acc', j)], writes=[('acc', j)])
        for t in range(t0, t1):
            j = t - t0
            if t == NMAIN:
                S.dma(out=y_s, in_=ACC[:NSAMP, j, :], reads=[('acc', j)])
            else:
                S.dma(out=y_p[t * 128:(t + 1) * 128, :], in_=ACC[:, j, :], reads=[('acc', j)])
    S.finish()
    return nc


_NC_CACHE = {}


def _consts():
    ident = np.eye(128, dtype=np.float32)
    p = np.arange(128)
    bones = (p[:, None] // 64 == p[None, :] // 64).astype(np.float32)
    tri = (p[:, None] <= p[None, :]).astype(np.float32)
    mult = np.zeros((128, NOFF, 128), np.float32)
    for o in range(NOFF):
        delta = 128 * o + p[None, :] - p[:, None]
        m = ((delta >= 0) & (delta <= 128)).astype(np.float32)
        m += ((delta >= 0) & (delta <= 512) & (delta % 4 == 0)).astype(np.float32)
        m += ((delta >= 0) & (delta <= 2048) & (delta % 16 == 0)).astype(np.float32)
        mult[:, o, :] = m
    sel = np.zeros((16, 16, 128), np.float32)
    for b in range(16):
        sel[b, b, :] = 1.0
    return ident, bones, tri, mult, sel


def kernel(x_prompt, x_sample, cache_k, cache_v, norm1_g, w_in, q_gain, k_gain, v_gain,
           w_spatial, b_spatial, out_gain_a, out_gain_b, w_out, norm2_g, w_router1, b_router1,
           w_router2, b_router2, w_up, w_gate, w_down):
    f = lambda a: np.ascontiguousarray(np.asarray(a, dtype=np.float32))
    x_prompt, x_sample = f(x_prompt), f(x_sample)
    cache_k, cache_v = np.asarray(cache_k, dtype=np.float32), np.asarray(cache_v, dtype=np.float32)
    if 'nc' not in _NC_CACHE:
        _NC_CACHE['nc'] = build_nc()
    nc = _NC_CACHE['nc']
    ident, bones, tri, mult, sel = _consts()
    col = lambda v: f(np.asarray(v).reshape(8, 128).T)
    g1c, g2c = col(norm1_g[0]), col(norm2_g[0])
    ogc = col(np.concatenate([np.asarray(out_gain_a[0]), np.asarray(out_gain_b[0])]))
    qg = np.asarray(q_gain[0]).reshape(4, 128).T
    kg = np.asarray(k_gain[0]).reshape(4, 128).T
    gqk = f(np.concatenate([qg, kg], axis=1))
    vgain = f(np.broadcast_to(np.asarray(v_gain[0]).reshape(1, 512), (128, 512)))
    bst = f(np.asarray(b_spatial[0]).T)
    ws00 = f(np.broadcast_to(np.asarray(w_spatial[0])[:, 0, 0].reshape(1, 8), (128, 8)))
    bs0 = f(np.broadcast_to(np.asarray(b_spatial[0])[:, 0].reshape(1, 8), (128, 8)))
    br = f(np.broadcast_to(np.concatenate([np.asarray(b_router1[0]).reshape(4), np.asarray(b_router2[0]).reshape(32)]
                                          ).reshape(1, 36), (128, 36)))
    wsT = f(np.transpose(np.asarray(w_spatial[0]), (2, 0, 1)))
    w_r = f(np.concatenate([np.asarray(w_router1[0]),
                            np.transpose(np.asarray(w_router2[0]), (1, 0, 2)).reshape(D, 32)], axis=1))
    wg = f(np.asarray(w_gate[0]).reshape(32, D, 256))
    wu = f(np.asarray(w_up[0]).reshape(32, D, 256))
    wd = f(np.asarray(w_down[0]).reshape(32, 256, D))
    w_in0, w_out0 = f(w_in[0]), f(w_out[0])
    in_maps = []
    for c in range(NCORES):
        b, hf = c // 2, c % 2
        xp = np.zeros((48 * 128, D), np.float32)
        if hf == 1:
            xp[:2048] = x_prompt[b, 2048:4096]
        xp[2048:] = x_prompt[b, hf * 4096:(hf + 1) * 4096]
        in_maps.append({
            "xp": xp, "xs": f(x_sample[16 * c:16 * c + 16, 0]),
            "ck": f(cache_k[0, 16 * c:16 * c + 16].reshape(16, 2048, 512)),
            "cv": f(cache_v[0, 16 * c:16 * c + 16].reshape(16, 2048, 512)),
            "w_in": w_in0, "w_out": w_out0, "w_r": w_r, "w_gate": wg, "w_up": wu, "w_down": wd, "wsT": wsT,
            "g1c": g1c, "g2c": g2c, "ogc": ogc, "gqk": gqk, "vgain": vgain, "bst": bst, "ws00": ws00, "bs0": bs0,
            "br": br, "flag": np.full((128, 1), float(hf), np.float32),
            "c_ident": ident, "c_bones": bones, "c_tri": tri, "c_mult": mult,
        })
    res = run_bass_kernel_spmd(nc, in_maps, core_ids=list(range(NCORES)))
    R = res.results
    B, T = 4, 8192
    y_prompt = np.zeros((B, T, D), np.float32)
    y_sample = np.zeros((128, 1, D), np.float32)
    nkp = np.zeros((1, B, 2048, 8, 64), np.float32)
    nvp = np.zeros((1, B, 2048, 8, 64), np.float32)
    nks = np.zeros((1, 128, 1, 8, 64), np.float32)
    nvs = np.zeros((1, 128, 1, 8, 64), np.float32)
    nva = np.zeros((1, 128, 1, 8, 64), np.float32)
    for c in range(NCORES):
        b, hf = c // 2, c % 2
        r = R[c]
        y_prompt[b, hf * 4096:(hf + 1) * 4096] = r["y_p"]
        y_sample[16 * c:16 * c + 16, 0] = r["y_s"]
        if hf == 1:
            nkp[0, b] = r["nk"].reshape(2048, 8, 64)
            nvp[0, b] = r["nv"].reshape(2048, 8, 64)
        nks[0, 16 * c:16 * c + 16, 0] = r["nks"].reshape(16, 8, 64)
        nvs[0, 16 * c:16 * c + 16, 0] = r["nvs"].reshape(16, 8, 64)
        nva[0, 16 * c:16 * c + 16, 0] = r["nva"].reshape(16, 8, 64)
    return (y_prompt, y_sample, nkp, nvp, nks, nvs, nva)
```

```python
import numpy as np
import ml_dtypes
import concourse.bass as bass
import concourse.mybir as mybir
from concourse.bass_utils import run_bass_kernel_spmd

F32 = mybir.dt.float32
BF16 = mybir.dt.bfloat16
U8 = mybir.dt.uint8
ALU = mybir.AluOpType
AF = mybir.ActivationFunctionType
AX = mybir.AxisListType

D = 1024
NCORES = 8
NHALO = 16
NMAIN = 32
RING = 20
NOFF = 17
NSAMP = 16
NTILES = NMAIN + 1
EPS = 1e-6
NDS = 24
GROUPS = [(0, 11), (11, 22), (22, 33)]


class Sched:
    def __init__(self, nc):
        self.nc = nc
        self.ce = ['pe', 'act', 'dve', 'pool']
        self.prog = {e: [] for e in self.ce + ['sp']}
        self.sem = {e: nc.alloc_semaphore("s_" + e) for e in self.ce}
        self.cnt = {e: 0 for e in self.ce}
        self.dsem = [nc.alloc_semaphore("d%d" % i) for i in range(NDS)]
        self.dcnt = [0] * NDS
        self.dnext = 0
        self.waited = {}
        self.lastw = {}
        self.readers = {}

    def _semh(self, sk):
        return self.sem[sk[1]] if sk[0] == 'e' else self.dsem[sk[1]]

    def _deps(self, e, reads, writes):
        need = {}

        def add(tok):
            if tok is None:
                return
            sk, v, te = tok
            if te == e and e == 'pe':
                return
            if v > need.get(sk, 0):
                need[sk] = v
        for k in reads:
            add(self.lastw.get(k))
        for k in writes:
            add(self.lastw.get(k))
            for sk, (v, te) in self.readers.get(k, {}).items():
                add((sk, v, te))
        out = []
        for sk, v in need.items():
            if self.waited.get((e, sk), 0) >= v:
                continue
            self.waited[(e, sk)] = v
            out.append((self._semh(sk), v))
        return out

    def _commit(self, tok, reads, writes):
        sk, v, te = tok
        for k in reads:
            self.readers.setdefault(k, {})[sk] = (v, te)
        for k in writes:
            self.lastw[k] = tok
            self.readers[k] = {}

    def op(self, e, fn, reads=(), writes=()):
        waits = self._deps(e, reads, writes)
        self.cnt[e] += 1
        tok = (('e', e), self.cnt[e], e)
        sem = self.sem[e]

        def emit(eng):
            for s, v in waits:
                eng.wait_ge(s, v)
            fn(eng).then_inc(sem, 1)
        self.prog[e].append(emit)
        self._commit(tok, reads, writes)

    def dma(self, out, in_, reads=(), writes=(), q='sp'):
        waits = self._deps(q, reads, writes)
        i = self.dnext
        self.dnext = (i + 1) % NDS
        prev = self.dcnt[i]
        if prev > 0 and self.waited.get((q, ('d', i)), 0) < prev:
            waits.append((self.dsem[i], prev))
            self.waited[(q, ('d', i))] = prev
        self.dcnt[i] += 16
        tok = (('d', i), self.dcnt[i], 'dma')
        sem = self.dsem[i]

        def emit(eng):
            for s, v in waits:
                eng.wait_ge(s, v)
            eng.dma_start(out=out, in_=in_).then_inc(sem, 16)
        self.prog[q].append(emit)
        self._commit(tok, reads, writes)

    def barrier(self):
        allw = [(('e', e), self.cnt[e]) for e in self.ce if self.cnt[e] > 0]
        allw += [(('d', i), self.dcnt[i]) for i in range(NDS) if self.dcnt[i] > 0]
        for e in self.ce + ['sp']:
            waits = []
            for sk, v in allw:
                if sk == ('e', e) and e == 'pe':
                    continue
                if self.waited.get((e, sk), 0) >= v:
                    continue
                self.waited[(e, sk)] = v
                waits.append((self._semh(sk), v))

            def emit(eng, waits=waits):
                for s, v in waits:
                    eng.wait_ge(s, v)
            self.prog[e].append(emit)
        self.lastw = {}
        self.readers = {}

    def finish(self):
        self.barrier()
        nc = self.nc
        prog = self.prog
        with nc.Block() as block:
            @block.sync
            def _(eng):
                for f in prog['sp']:
                    f(eng)

            @block.tensor
            def _(eng):
                for f in prog['pe']:
                    f(eng)

            @block.scalar
            def _(eng):
                for f in prog['act']:
                    f(eng)

            @block.vector
            def _(eng):
                for f in prog['dve']:
                    f(eng)

            @block.gpsimd
            def _(eng):
                for f in prog['pool']:
                    f(eng)


class Arena:
    def __init__(self, nc, nbytes):
        self.ap = nc.alloc_sbuf_tensor("arena", [128, nbytes], U8).ap()
        self.off = 0
        self.nbytes = nbytes

    def alloc(self, free_shape, dtype):
        esz = 4 if dtype == F32 else 2
        n = int(np.prod(free_shape))
        nb = (n * esz + 31) // 32 * 32
        assert self.off + nb <= self.nbytes, ("arena overflow", self.off, nb)
        v = self.ap[:, self.off:self.off + n * esz].bitcast(dtype)
        self.off += nb
        if len(free_shape) == 2:
            v = v.rearrange("p (a b) -> p a b", b=free_shape[1])
        elif len(free_shape) == 3:
            v = v.rearrange("p (a b c) -> p a b c", b=free_shape[1], c=free_shape[2])
        return v


def build_nc(nhalo=16, nmain=32, groups=((0, 11), (11, 22), (22, 33))):
    global NHALO, NMAIN, NTILES, GROUPS
    NHALO, NMAIN, NTILES, GROUPS = nhalo, nmain, nmain + 1, list(groups)
    nc = bass.Bass("TRN2", target_bir_lowering=False)
    S = Sched(nc)

    def din(name, shape, dt=F32):
        return nc.dram_tensor(name, list(shape), dt, kind="ExternalInput").ap()

    def dout(name, shape, dt=F32):
        return nc.dram_tensor(name, list(shape), dt, kind="ExternalOutput").ap()

    xp = din("xp", [(NHALO + NMAIN) * 128, D])
    xs = din("xs", [NSAMP, D])
    ck = din("ck", [NSAMP, 2048, 512])
    cv = din("cv", [NSAMP, 2048, 512])
    w_in = din("w_in", [D, 2560])
    w_out = din("w_out", [D, D])
    w_r = din("w_r", [D, 36])
    w_gate = din("w_gate", [32, D, 256])
    w_up = din("w_up", [32, D, 256])
    w_down = din("w_down", [32, 256, D])
    wsT_d = din("wsT", [128, 8, 128])
    g1c_d = din("g1c", [128, 8])
    g2c_d = din("g2c", [128, 8])
    ogc_d = din("ogc", [128, 8])
    gqk_d = din("gqk", [128, 8])
    vgain_d = din("vgain", [128, 512])
    bst_d = din("bst", [128, 8])
    ws00_d = din("ws00", [128, 8])
    bs0_d = din("bs0", [128, 8])
    br_d = din("br", [128, 36])
    flag_d = din("flag", [128, 1])
    c_ident = din("c_ident", [128, 128])
    c_bones = din("c_bones", [128, 128])
    c_tri = din("c_tri", [128, 128])
    c_mult = din("c_mult", [128, NOFF, 128])

    y_p = dout("y_p", [NMAIN * 128, D])
    y_s = dout("y_s", [NSAMP, D])
    nk_o = dout("nk", [2048, 512])
    nv_o = dout("nv", [2048, 512])
    nks_o = dout("nks", [NSAMP, 512])
    nvs_o = dout("nvs", [NSAMP, 512])
    nva_o = dout("nva", [NSAMP, 512])

    hs = nc.dram_tensor("hs", [NTILES * 128, D], F32, kind="Internal").ap()
    hnts = nc.dram_tensor("hnts", [128, NTILES, 8, 128], BF16, kind="Internal").ap()

    ar = Arena(nc, 200 * 1024)
    G = ar.alloc([NTILES, 32], F32)
    g2c = ar.alloc([8], F32)
    persist_end = ar.off

    psA = nc.alloc_psum_tensor("psA", [128, 8, 128], F32).ap()
    psB = nc.alloc_psum_tensor("psB", [128, 8, 128], F32).ap()
    psC = nc.alloc_psum_tensor("psC", [128, 2, 512], F32).ap()
    psT = nc.alloc_psum_tensor("psT", [128, 8, 128], BF16).ap()
    psE = nc.alloc_psum_tensor("psE", [128, 512], F32).ap()
    psA_flat = psA.rearrange("p a b -> p (a b)")
    psB_flat = psB.rearrange("p a b -> p (a b)")

    WIN = ar.alloc([8, 2560], BF16)
    WOUT = ar.alloc([8, 1024], BF16)
    WR = ar.alloc([8, 36], BF16)
    WST = ar.alloc([8, 128], BF16)
    MULT = ar.alloc([NOFF, 128], BF16)
    MULTH = ar.alloc([NOFF, 128], BF16)
    identf = ar.alloc([128], F32)
    identb = ar.alloc([128], BF16)
    bonesb = ar.alloc([128], BF16)
    ONES16 = ar.alloc([128], F32)
    g1c = ar.alloc([8], F32)
    ogc = ar.alloc([8], F32)
    GQK = ar.alloc([8], F32)
    VGAIN = ar.alloc([512], F32)
    BST = ar.alloc([8], F32)
    WS00 = ar.alloc([8], F32)
    BS0 = ar.alloc([8], F32)
    BR = ar.alloc([36], F32)
    FLAG = ar.alloc([1], F32)
    EPSC = ar.alloc([1], F32)
    stg_off = ar.off
    STG = [ar.alloc([2560], F32) for _ in range(2)]

    def small_load(dst, src, key):
        S.dma(out=dst, in_=src, writes=[key])

    small_load(g1c, g1c_d, 'g1c')
    small_load(g2c, g2c_d, 'g2c')
    small_load(ogc, ogc_d, 'ogc')
    small_load(GQK, gqk_d, 'gqk')
    small_load(VGAIN, vgain_d, 'vgain')
    small_load(BST, bst_d, 'bst')
    small_load(WS00, ws00_d, 'ws00')
    small_load(BS0, bs0_d, 'bs0')
    small_load(BR, br_d, 'br')
    small_load(FLAG, flag_d, 'flag')
    small_load(identf, c_ident, 'identf')
    S.op('dve', lambda e: e.memset(ONES16, 1.0), writes=['ones16'])
    S.op('dve', lambda e: e.memset(EPSC, EPS), writes=['epsc'])
    S.op('dve', lambda e: e.memset(VA[:, :, :, 64:65], 1.0), writes=['va_ones'])
    for i in range(6):
        S.op('pool', lambda e, i=i: e.memset(VAS[i][:, :, 64:65], 1.0), writes=[('vas', i)])
    S.op('dve', lambda e: e.tensor_copy(out=identb, in_=identf), reads=['identf'], writes=['identb'])
    S.dma(out=STG[0][:, 0:128], in_=c_bones, writes=[('stg', 0)])
    S.op('dve', lambda e: e.tensor_copy(out=bonesb, in_=STG[0][:, 0:128]), reads=[('stg', 0)], writes=['bonesb'])
    S.dma(out=STG[1][:, 0:NOFF * 128], in_=c_mult.rearrange("p a b -> p (a b)"), writes=[('stg', 1)])
    S.op('dve', lambda e: e.tensor_copy(out=MULT.rearrange("p a b -> p (a b)"), in_=STG[1][:, 0:NOFF * 128]),
         reads=[('stg', 1)], writes=['mult'])
    S.op('dve', lambda e: e.tensor_scalar(out=MULTH.rearrange("p a b -> p (a b)"), in0=STG[1][:, 0:NOFF * 128],
                                          scalar1=FLAG[:, 0:1], scalar2=None, op0=ALU.mult),
         reads=[('stg', 1), 'flag'], writes=['multh'])
    S.dma(out=STG[0][:, 0:1024], in_=wsT_d.rearrange("p a b -> p (a b)"), reads=['bonesb'], writes=[('stg', 0)])
    S.dma(out=STG[0][:, 1024:1152], in_=c_tri, writes=[('stg', 0)])
    S.op('dve', lambda e: e.tensor_tensor(
        out=WST, in0=STG[0][:, 0:1024].rearrange("p (a b) -> p a b", b=128),
        in1=STG[0][:, 1024:1152].unsqueeze(1).to_broadcast([128, 8, 128]), op=ALU.mult),
        reads=[('stg', 0)], writes=['wst'])
    S.dma(out=STG[1][:, 0:288].rearrange("p (a b) -> p a b", b=36), in_=w_r.rearrange("(kc p) f -> p kc f", p=128),
          reads=['mult', 'multh'], writes=[('stg', 1)])
    S.op('dve', lambda e: e.tensor_tensor(
        out=WR, in0=STG[1][:, 0:288].rearrange("p (a b) -> p a b", b=36),
        in1=g2c.unsqueeze(2).to_broadcast([128, 8, 36]), op=ALU.mult),
        reads=[('stg', 1), 'g2c'], writes=['wr'])
    for kc in range(8):
        st = STG[kc % 2]
        S.dma(out=st[:, 0:2560], in_=w_in[kc * 128:(kc + 1) * 128, :], reads=['wst', 'wr'], writes=[('stg', kc % 2)])
        eng = 'dve' if kc % 2 == 0 else 'pool'
        S.op(eng, lambda e, st=st, kc=kc: e.tensor_scalar(out=WIN[:, kc, :], in0=st[:, 0:2560], scalar1=g1c[:, kc:kc + 1],
                                                           scalar2=None, op0=ALU.mult),
             reads=[('stg', kc % 2), 'g1c'], writes=['win'])
    for kc in range(8):
        st = STG[kc % 2]
        S.dma(out=st[:, 0:1024], in_=w_out[kc * 128:(kc + 1) * 128, :], writes=[('stg', kc % 2)])
        eng = 'dve' if kc % 2 == 0 else 'pool'
        S.op(eng, lambda e, st=st, kc=kc: e.tensor_scalar(out=WOUT[:, kc, :], in0=st[:, 0:1024], scalar1=ogc[:, kc:kc + 1],
                                                           scalar2=None, op0=ALU.mult),
             reads=[('stg', kc % 2), 'ogc'], writes=['wout'])

    S.barrier()
    ar.off = stg_off
    XT = [ar.alloc([1024], F32) for _ in range(2)]
    xn = ar.alloc([1024], BF16)
    xnT = ar.alloc([8, 128], BF16)
    sq = ar.alloc([8, 128], BF16)
    rr = ar.alloc([8, 128], F32)
    qkn = ar.alloc([8, 128], F32)
    QT = ar.alloc([4, 2, 128], BF16)
    KT = ar.alloc([RING, 4, 128], BF16)
    VA = ar.alloc([RING, 8, 65], BF16)
    kout = ar.alloc([1024], F32)
    vout = ar.alloc([512], F32)
    u_t = ar.alloc([512], F32)
    gg = ar.alloc([512], F32)
    gsq = ar.alloc([512], F32)
    vg = ar.alloc([512], F32)
    vgb = ar.alloc([512], BF16)
    oa = ar.alloc([512], F32)
    ob = ar.alloc([512], F32)
    oab = ar.alloc([1024], BF16)
    oT = ar.alloc([8, 128], BF16)
    P = [ar.alloc([8, 128], BF16) for _ in range(2)]
    hh = ar.alloc([1024], F32)
    hn = ar.alloc([1024], BF16)
    hnT = ar.alloc([8, 128], BF16)
    junk = ar.alloc([1024], BF16)
    ST = ar.alloc([64], F32)
    RL = ar.alloc([96], F32)
    KC = [ar.alloc([512], F32) for _ in range(2)]
    VC = [ar.alloc([512], F32) for _ in range(2)]
    prod = ar.alloc([512], F32)
    SALL = ar.alloc([24], F32)
    VAS = [ar.alloc([8, 65], BF16) for _ in range(6)]
    PZ = [ar.alloc([24, 16], BF16) for _ in range(2)]
    stage1_end = ar.off
    print('stage1 arena bytes', stage1_end)

    class Defer:
        def __init__(self):
            self.q = []

        def op(self, *a, **k):
            self.q.append(lambda: S.op(*a, **k))

        def dma(self, *a, **k):
            self.q.append(lambda: S.dma(*a, **k))

        def pop(self, n):
            for _ in range(n):
                if self.q:
                    self.q.pop(0)()

        def flush(self):
            self.pop(len(self.q))

    DQ = Defer()

    def rstd(nt, src, dst, scale, tmpkey, Q=None):
        Q = Q or S
        Q.op('act', lambda e: e.activation(out=dst, in_=src, func=AF.Sqrt, bias=EPSC[:nt, 0:1], scale=scale),
             reads=['st'], writes=['st'])
        Q.op('dve', lambda e: e.reciprocal(out=dst, in_=dst), reads=['st'], writes=['st'])

    def transposes_bf(nt, src, dstT, rkey, wkey):
        def f(e):
            last = None
            for kc in range(8):
                last = e.transpose(out=psT[:, kc, :nt], in_=src[:nt, kc * 128:(kc + 1) * 128], identity=identb[:nt, :nt])
            return last
        S.op('pe', f, reads=[rkey], writes=['psT'])
        S.op('act', lambda e: e.copy(out=dstT[:, :, :nt], in_=psT[:, :, :nt]), reads=['psT'], writes=[wkey])

    def v3(ap, nt):
        return ap[:nt].rearrange("p (h d) -> p h d", d=64)

    def v4(ap, nt):
        return ap[:nt].rearrange("p (b h d) -> p b h d", b=2, d=64)

    psO = psC
    psO_num = psC[:, :, 0:260].rearrange("p b (h e) -> p b h e", e=65)[:, :, :, 0:64]
    psO_den = psC[:, :, 64:260:65]

    def load_x(g):
        if g == NHALO + NMAIN:
            S.dma(out=XT[g % 2][:NSAMP], in_=xs, writes=[('xt', g % 2)])
        else:
            S.dma(out=XT[g % 2], in_=xp[g * 128:(g + 1) * 128, :], writes=[('xt', g % 2)])

    import os
    CUT = int(os.environ.get('K_CUT', '99'))
    CUT2 = int(os.environ.get('K_CUT2', '99'))
    CUT3 = int(os.environ.get('K_CUT3', '99'))

    def do_tile(g, kind):
        nt = NSAMP if kind == 'sample' else 128
        xt = XT[g % 2]
        xtk = ('xt', g % 2)
        mt = g - NHALO if kind != 'sample' else NMAIN
        if g == 0:
            load_x(0)
        if g + 1 <= NHALO + NMAIN:
            load_x(g + 1)
        S.op('act', lambda e: e.activation(out=junk[:nt], in_=xt[:nt], func=AF.Square, accum_out=ST[:nt, 0:1]),
             reads=[xtk], writes=['junk', 'st'])
        rstd(nt, ST[:nt, 0:1], ST[:nt, 1:2], 1.0 / D, 'st')
        S.op('dve', lambda e: e.tensor_scalar(out=xn[:nt], in0=xt[:nt], scalar1=ST[:nt, 1:2], scalar2=None, op0=ALU.mult),
             reads=[xtk, 'st'], writes=['xn'])
        transposes_bf(nt, xn, xnT, 'xn', 'xnT')
        fcs = list(range(4, 8)) if kind == 'halo' else list(range(8))
        f0, f1 = fcs[0], fcs[-1] + 1
        nf = f1 - f0

        def fqk(e):
            last = None
            for fc in fcs:
                for kc in range(8):
                    last = e.matmul(psA[:, fc, :nt], lhsT=WIN[:, kc, fc * 128:(fc + 1) * 128], rhs=xnT[:, kc, :nt],
                                    start=(kc == 0), stop=(kc == 7))
            return last
        S.op('pe', fqk, reads=['xnT', 'win'], writes=['psA'])

        def fv(e):
            last = None
            dsts = [(psE, 1024)] if kind == 'halo' else [(psE, 1024), (psC[:, 0, :], 1536), (psC[:, 1, :], 2048)]
            for dst, c0 in dsts:
                for kc in range(8):
                    last = e.matmul(dst[:nt, 0:512], lhsT=xnT[:, kc, :nt], rhs=WIN[:, kc, c0:c0 + 512],
                                    start=(kc == 0), stop=(kc == 7))
            return last
        S.op('pe', fv, reads=['xnT', 'win'], writes=['psE'] if kind == 'halo' else ['psE', 'psC'])
        S.op('act', lambda e: e.activation(out=sq[:, f0:f1, :nt], in_=psA[:, f0:f1, :nt], func=AF.Square),
             reads=['psA'], writes=['sq'])

        def fss(e):
            last = None
            for fc in fcs:
                last = e.matmul(psB[:, fc, :nt], lhsT=bonesb, rhs=sq[:, fc, :nt], start=True, stop=True)
            return last
        S.op('pe', fss, reads=['sq', 'bonesb'], writes=['psB'])
        S.op('act', lambda e: e.activation(out=rr[:, f0:f1, :nt], in_=psB[:, f0:f1, :nt], func=AF.Sqrt, bias=EPSC[:, 0:1],
                                           scale=1.0 / 64), reads=['psB'], writes=['rr'])
        S.op('dve', lambda e: e.reciprocal(out=rr[:, f0:f1, :nt], in_=rr[:, f0:f1, :nt]), reads=['rr'], writes=['rr'])
        S.op('dve', lambda e: e.tensor_tensor(out=qkn[:, f0:f1, :nt], in0=psA[:, f0:f1, :nt],
                                              in1=GQK[:, f0:f1].unsqueeze(2).to_broadcast([128, nf, nt]), op=ALU.mult),
             reads=['psA', 'gqk'], writes=['qkn'])
        S.op('dve', lambda e: e.tensor_tensor(out=qkn[:, f0:f1, :nt], in0=qkn[:, f0:f1, :nt], in1=rr[:, f0:f1, :nt],
                                              op=ALU.mult), reads=['qkn', 'rr'], writes=['qkn'])
        slot = g % RING
        if kind != 'sample':
            S.op('dve', lambda e: e.tensor_copy(out=KT[:, slot, :, :], in_=qkn[:, 4:8, :]), reads=['qkn'],
                 writes=[('kt', slot)])
        if kind == 'main':
            if g == NHALO:
                S.op('dve', lambda e: e.memset(QT, 0.0), writes=['qt'])
            S.op('act', lambda e: e.copy(out=QT[0:64, :, 0, :], in_=qkn[0:64, 0:4, :]), reads=['qkn'], writes=['qt'])
            S.op('act', lambda e: e.copy(out=QT[64:128, :, 1, :], in_=qkn[64:128, 0:4, :]), reads=['qkn'],
                 writes=['qt'])
        if kind == 'main' and CUT <= 1:
            return
        KO0 = NHALO + max(NMAIN - 16, 0)
        want_kout = (kind == 'sample') or (kind == 'main' and g >= KO0)
        if want_kout:
            tf = list(range(8)) if kind == 'sample' else list(range(4, 8))

            def ftr(e):
                last = None
                for fc in tf:
                    last = e.transpose(out=psB_flat[:nt, fc * 128:(fc + 1) * 128], in_=qkn[:, fc, :nt], identity=identf)
                return last
            S.op('pe', ftr, reads=['qkn', 'identf'], writes=['psB'])
            t0 = tf[0] * 128
            S.op('dve', lambda e: e.tensor_copy(out=kout[:nt, t0:1024], in_=psB_flat[:nt, t0:1024]), reads=['psB'],
                 writes=['kout'])
            if kind == 'sample':
                S.dma(out=nks_o, in_=kout[:nt, 512:1024], reads=['kout'])
            else:
                r0 = (g - KO0) * 128
                S.dma(out=nk_o[r0:r0 + 128, :], in_=kout[:, 512:1024], reads=['kout'])
        if kind == 'main' and CUT <= 2:
            return
        if kind != 'sample':
            S.op('act', lambda e: e.copy(out=VA[:nt, slot, :, 0:64], in_=v3(psE, nt)), reads=['psE', 'va_ones'],
                 writes=[('va', slot)])
        if want_kout:
            S.op('dve', lambda e: e.tensor_copy(out=vout[:nt], in_=psE[:nt, 0:512]), reads=['psE'], writes=['vout'])
            if kind == 'sample':
                S.dma(out=nvs_o, in_=vout[:nt], reads=['vout'])
            else:
                r0 = (g - KO0) * 128
                S.dma(out=nv_o[r0:r0 + 128, :], in_=vout, reads=['vout'])
        if kind == 'main' and CUT <= 3:
            return
        if kind == 'halo':
            return
        S.op('act', lambda e: e.activation(out=u_t[:nt], in_=psC[:nt, 0, :], func=AF.Gelu_apprx_tanh), reads=['psC'],
             writes=['u'])
        S.op('act', lambda e: e.activation(out=gg[:nt], in_=psC[:nt, 1, :], func=AF.Gelu_apprx_tanh), reads=['psC'],
             writes=['gg'])
        Q = DQ if kind == 'main' else S
        if kind == 'sample':
            DQ.flush()
        Q.op('dve', lambda e: e.tensor_tensor(out=gsq[:nt], in0=gg[:nt], in1=gg[:nt], op=ALU.mult), reads=['gg'],
             writes=['gsq'])
        Q.op('dve', lambda e: e.tensor_reduce(out=ST[:nt, 8:16], in_=v3(gsq, nt), axis=AX.X, op=ALU.add), reads=['gsq'],
             writes=['st'])
        rstd(nt, ST[:nt, 8:16], ST[:nt, 16:24], 1.0 / 64, 'st', Q)
        Q.op('dve', lambda e: e.tensor_tensor(out=v3(vg, nt), in0=v3(gg, nt),
                                              in1=ST[:nt, 16:24].unsqueeze(2).to_broadcast([nt, 8, 64]), op=ALU.mult),
             reads=['gg', 'st'], writes=['vg'])
        Q.op('dve', lambda e: e.tensor_tensor(out=vg[:nt], in0=vg[:nt], in1=VGAIN[:nt], op=ALU.mult),
             reads=['vg', 'vgain'], writes=['vg'])
        if kind == 'sample':
            Q.dma(out=nva_o, in_=vg[:nt], reads=['vg'])
            Q.op('dve', lambda e: e.tensor_tensor(out=v3(oa, nt), in0=v3(vg, nt),
                                                  in1=WS00[:nt].unsqueeze(2).to_broadcast([nt, 8, 64]), op=ALU.mult),
                 reads=['vg', 'ws00'], writes=['oa'])
            Q.op('dve', lambda e: e.tensor_tensor(out=v3(oa, nt), in0=v3(oa, nt),
                                                  in1=BS0[:nt].unsqueeze(2).to_broadcast([nt, 8, 64]), op=ALU.add),
                 reads=['oa', 'bs0'], writes=['oa'])
        else:
            Q.op('act', lambda e: e.copy(out=vgb, in_=vg), reads=['vg'], writes=['vgb'])

            def fmix(e):
                last = None
                for h in range(8):
                    last = e.matmul(psE[:, h * 64:(h + 1) * 64], lhsT=WST[:, h, :], rhs=vgb[:, h * 64:(h + 1) * 64],
                                    start=True, stop=True)
                return last
            Q.op('pe', fmix, reads=['vgb', 'wst'], writes=['psE'])
            Q.op('dve', lambda e: e.tensor_tensor(out=v3(oa, nt), in0=v3(psE, nt),
                                                  in1=BST.unsqueeze(2).to_broadcast([128, 8, 64]), op=ALU.add),
                 reads=['psE', 'bst'], writes=['oa'])
        Q.op('dve', lambda e: e.tensor_tensor(out=oa[:nt], in0=oa[:nt], in1=u_t[:nt], op=ALU.mult), reads=['oa', 'u'],
             writes=['oa'])
        Q.op('act', lambda e: e.activation(out=junk[:nt, 0:512], in_=oa[:nt], func=AF.Square, accum_out=ST[:nt, 24:25]),
             reads=['oa'], writes=['junk', 'st'])
        rstd(nt, ST[:nt, 24:25], ST[:nt, 25:26], 1.0 / 512, 'st', Q)
        Q.op('dve', lambda e: e.tensor_scalar(out=oab[:nt, 0:512], in0=oa[:nt], scalar1=ST[:nt, 25:26], scalar2=None,
                                              op0=ALU.mult), reads=['oa', 'st'], writes=['oab'])
        if kind == 'main' and CUT <= 4:
            return
        if kind == 'main':
            def s_op(i):
                o = NOFF - 1 - i
                kt = g - o
                sl = kt % RING
                ps = psA if i % 2 == 0 else psB
                pk = 'psA' if i % 2 == 0 else 'psB'

                def f(e):
                    last = None
                    for hp in range(4):
                        last = e.matmul(ps[:, 2 * hp:2 * hp + 2, :], lhsT=KT[:, sl, hp, :], rhs=QT[:, hp, :, :],
                                        start=True, stop=True)
                    return last
                S.op('pe', f, reads=[('kt', sl), 'qt'], writes=[pk])
            s_op(0)
            for i in range(NOFF):
                o = NOFF - 1 - i
                kt = g - o
                sl = kt % RING
                ps = psA if i % 2 == 0 else psB
                pk = 'psA' if i % 2 == 0 else 'psB'
                Pi = P[i % 2]
                pik = ('P', i % 2)
                if i + 1 < NOFF:
                    s_op(i + 1)
                if CUT3 < 2:
                    continue
                S.op('act', lambda e, ps=ps, Pi=Pi: e.activation(out=Pi, in_=ps, func=AF.Exp, scale=0.125), reads=[pk],
                     writes=[pik])
                if CUT3 < 3:
                    continue
                M = MULTH if kt < NHALO else MULT
                S.op('dve', lambda e, Pi=Pi, M=M, o=o: e.tensor_tensor(
                    out=Pi, in0=Pi, in1=M[:, o, :].unsqueeze(1).to_broadcast([128, 8, 128]), op=ALU.mult),
                    reads=[pik, 'mult', 'multh'], writes=[pik])

                if CUT3 < 4:
                    continue

                def fpv(e, Pi=Pi, sl=sl, i=i):
                    last = None
                    for h in range(8):
                        last = e.matmul(psO[:, h // 4, (h % 4) * 65:(h % 4) * 65 + 65], lhsT=Pi[:, h, :],
                                        rhs=VA[:, sl, h, :], start=(i == 0 and h % 4 == 0), stop=(i == NOFF - 1 and h % 4 == 3),
                                        skip_group_check=True)
                    return last
                S.op('pe', fpv, reads=[pik, ('va', sl)], writes=['psC'])
                DQ.pop(-(-len(DQ.q) // (NOFF - i)))
            DQ.flush()
            if CUT3 < 5:
                return
            S.op('dve', lambda e: e.reciprocal(out=ST[:, 32:40].rearrange("p (b h) -> p b h", b=2), in_=psO_den),
                 reads=['psC'], writes=['st'])
            if CUT3 < 6:
                return
            S.op('dve', lambda e: e.tensor_tensor(
                out=v4(ob, nt), in0=psO_num,
                in1=ST[:, 32:40].rearrange("p (b h) -> p b h", b=2).unsqueeze(3).to_broadcast([128, 2, 4, 64]),
                op=ALU.mult), reads=['psC', 'st'], writes=['ob'])
        else:
            sample_attention()
        if kind == 'main' and CUT <= 5:
            return
        S.op('act', lambda e: e.activation(out=junk[:nt, 0:512], in_=ob[:nt], func=AF.Square, accum_out=ST[:nt, 26:27]),
             reads=['ob'], writes=['junk', 'st'])
        rstd(nt, ST[:nt, 26:27], ST[:nt, 27:28], 1.0 / 512, 'st')
        S.op('dve', lambda e: e.tensor_scalar(out=oab[:nt, 512:1024], in0=ob[:nt], scalar1=ST[:nt, 27:28], scalar2=None,
                                              op0=ALU.mult), reads=['ob', 'st'], writes=['oab'])
        transposes_bf(nt, oab, oT, 'oab', 'oT')

        def fwo(e):
            last = None
            for half in range(2):
                for kc in range(8):
                    last = e.matmul(psA_flat[:nt, half * 512:(half + 1) * 512], lhsT=oT[:, kc, :nt],
                                    rhs=WOUT[:, kc, half * 512:(half + 1) * 512], start=(kc == 0), stop=(kc == 7))
            return last
        S.op('pe', fwo, reads=['oT', 'wout'], writes=['psA'])
        S.op('dve', lambda e: e.tensor_tensor(out=hh[:nt], in0=psA_flat[:nt], in1=xt[:nt], op=ALU.add),
             reads=['psA', xtk], writes=['hh'])
        S.dma(out=hs[mt * 128:mt * 128 + nt, :], in_=hh[:nt], reads=['hh'], writes=[('hs', mt)])
        if kind == 'main' and CUT <= 6:
            return
        S.op('act', lambda e: e.activation(out=junk[:nt], in_=hh[:nt], func=AF.Square, accum_out=ST[:nt, 28:29]),
             reads=['hh'], writes=['junk', 'st'])
        rstd(nt, ST[:nt, 28:29], ST[:nt, 29:30], 1.0 / D, 'st')
        S.op('dve', lambda e: e.tensor_scalar(out=hn[:nt], in0=hh[:nt], scalar1=ST[:nt, 29:30], scalar2=None, op0=ALU.mult),
             reads=['hh', 'st'], writes=['hn'])
        transposes_bf(nt, hn, hnT, 'hn', 'hnT')
        S.dma(out=hnts[:, mt, :, :nt], in_=hnT[:, :, :nt], reads=['hnT'], writes=[('hnts', mt)])
        if kind == 'main' and CUT <= 7:
            return

        def frt(e):
            last = None
            for kc in range(8):
                last = e.matmul(psE[:nt, 0:36], lhsT=hnT[:, kc, :nt], rhs=WR[:, kc, :], start=(kc == 0), stop=(kc == 7))
            return last
        S.op('pe', frt, reads=['hnT', 'wr'], writes=['psE'])
        L = RL[:nt, 0:36]
        S.op('dve', lambda e: e.tensor_tensor(out=L, in0=psE[:nt, 0:36], in1=BR[:nt], op=ALU.add), reads=['psE', 'br'],
             writes=['rl'])
        RQ = DQ if kind == 'main' else S
        RQ.op('dve', lambda e: e.tensor_reduce(out=RL[:nt, 36:37], in_=RL[:nt, 0:4], axis=AX.X, op=ALU.max), reads=['rl'],
             writes=['rl'])
        RQ.op('dve', lambda e: e.tensor_scalar(out=RL[:nt, 40:44], in0=RL[:nt, 0:4], scalar1=RL[:nt, 36:37], scalar2=None,
                                              op0=ALU.is_equal), reads=['rl'], writes=['rl'])
        RQ.op('dve', lambda e: e.tensor_scalar(out=RL[:nt, 37:38], in0=RL[:nt, 36:37], scalar1=-1.0, scalar2=None,
                                              op0=ALU.mult), reads=['rl'], writes=['rl'])
        RQ.op('act', lambda e: e.activation(out=RL[:nt, 44:48], in_=RL[:nt, 0:4], func=AF.Exp, bias=RL[:nt, 37:38], scale=1.0,
                                           accum_out=RL[:nt, 38:39]), reads=['rl'], writes=['rl'])
        RQ.op('dve', lambda e: e.reciprocal(out=RL[:nt, 39:40], in_=RL[:nt, 38:39]), reads=['rl'], writes=['rl'])
        RQ.op('dve', lambda e: e.tensor_tensor(
            out=RL[:nt, 48:80].rearrange("p (g x) -> p g x", x=8), in0=RL[:nt, 4:36].rearrange("p (g x) -> p g x", x=8),
            in1=RL[:nt, 40:44].unsqueeze(2).to_broadcast([nt, 4, 8]), op=ALU.mult), reads=['rl'], writes=['rl'])
        RQ.op('dve', lambda e: e.tensor_reduce(out=RL[:nt, 80:88], in_=RL[:nt, 48:80].rearrange("p (g x) -> p x g", x=8),
                                              axis=AX.X, op=ALU.add), reads=['rl'], writes=['rl'])
        RQ.op('dve', lambda e: e.max(out=RL[:nt, 88:96], in_=RL[:nt, 80:88]), reads=['rl'], writes=['rl'])
        RQ.op('dve', lambda e: e.tensor_tensor(out=ST[:nt, 40:41], in0=RL[:nt, 89:90], in1=RL[:nt, 88:89], op=ALU.subtract),
             reads=['rl'], writes=['st'])
        RQ.op('act', lambda e: e.activation(out=ST[:nt, 41:42], in_=ST[:nt, 40:41], func=AF.Exp), reads=['st'], writes=['st'])
        RQ.op('dve', lambda e: e.tensor_scalar(out=ST[:nt, 41:42], in0=ST[:nt, 41:42], scalar1=1.0, scalar2=None, op0=ALU.add),
             reads=['st'], writes=['st'])
        RQ.op('dve', lambda e: e.reciprocal(out=ST[:nt, 42:43], in_=ST[:nt, 41:42]), reads=['st'], writes=['st'])
        RQ.op('dve', lambda e: e.tensor_scalar(out=ST[:nt, 43:44], in0=ST[:nt, 42:43], scalar1=-1.0, scalar2=1.0, op0=ALU.mult,
                                              op1=ALU.add), reads=['st'], writes=['st'])
        RQ.op('dve', lambda e: e.tensor_tensor(out=ST[:nt, 42:44], in0=ST[:nt, 42:44],
                                              in1=RL[:nt, 39:40].to_broadcast([nt, 2]), op=ALU.mult), reads=['st', 'rl'],
             writes=['st'])
        RQ.op('dve', lambda e: e.tensor_scalar(out=ST[:nt, 44:52], in0=RL[:nt, 80:88], scalar1=RL[:nt, 88:89],
                                              scalar2=ST[:nt, 42:43], op0=ALU.is_equal, op1=ALU.mult), reads=['rl', 'st'],
             writes=['st'])
        RQ.op('dve', lambda e: e.tensor_scalar(out=ST[:nt, 52:60], in0=RL[:nt, 80:88], scalar1=RL[:nt, 89:90],
                                              scalar2=ST[:nt, 43:44], op0=ALU.is_equal, op1=ALU.mult), reads=['rl', 'st'],
             writes=['st'])
        RQ.op('dve', lambda e: e.tensor_tensor(out=ST[:nt, 44:52], in0=ST[:nt, 44:52], in1=ST[:nt, 52:60], op=ALU.add),
             reads=['st'], writes=['st'])
        RQ.op('dve', lambda e: e.tensor_tensor(
            out=G[:nt, mt, :].rearrange("p (g x) -> p g x", x=8),
            in0=RL[:nt, 40:44].unsqueeze(2).to_broadcast([nt, 4, 8]),
            in1=ST[:nt, 44:52].unsqueeze(1).to_broadcast([nt, 4, 8]), op=ALU.mult), reads=['rl', 'st'], writes=['G'])

    def sample_attention():
        nt = NSAMP
        qtok = kout[:nt, 0:512]
        ktok = kout[:nt, 512:1024]
        rows = [(1920, 1), (1536, 4), (0, 16)]
        for b in range(NSAMP):
            S.op('dve', lambda e, b=b: e.tensor_scalar(out=gsq[:nt], in0=qtok, scalar1=identf[:nt, b:b + 1], scalar2=None,
                                                        op0=ALU.mult), reads=['kout', 'identf'], writes=['gsq'])

            def fqb(e, b=b):
                return e.matmul(psE[:, 0:512], lhsT=ONES16[0:16, :], rhs=gsq[:nt], start=True, stop=True)
            S.op('pe', fqb, reads=['gsq', 'ones16'], writes=['psE'])
            pz = PZ[b % 2]
            pzk = ('pz', b % 2)
            S.op('pool', lambda e, pz=pz: e.memset(pz, 0.0), writes=[pzk])
            for c, (r0, stp) in enumerate(rows):
                bi = (b * 3 + c) % 2
                kc_t, vc_t = KC[bi], VC[bi]
                S.dma(out=kc_t, in_=ck[b, r0:r0 + 128 * stp:stp, :], writes=[('kc', bi)])
                S.dma(out=vc_t, in_=cv[b, r0:r0 + 128 * stp:stp, :], writes=[('vc', bi)])
                S.op('dve', lambda e, kc_t=kc_t: e.tensor_tensor(out=prod, in0=kc_t, in1=psE[:, 0:512], op=ALU.mult),
                     reads=[('kc', bi), 'psE'], writes=['prod'])
                S.op('dve', lambda e, c=c: e.tensor_reduce(out=SALL[:, c * 8:(c + 1) * 8],
                                                           in_=prod.rearrange("p (h d) -> p h d", d=64), axis=AX.X,
                                                           op=ALU.add), reads=['prod'], writes=['sall'])
                vi = (b % 2) * 3 + c
                S.op('act', lambda e, vi=vi, vc_t=vc_t: e.copy(out=VAS[vi][:, :, 0:64],
                                                               in_=vc_t.rearrange("p (h d) -> p h d", d=64)),
                     reads=[('vc', bi), ('vas', vi)], writes=[('vas', vi)])
            S.op('act', lambda e, pz=pz, b=b: e.activation(out=pz[:, :, b], in_=SALL[:, 0:24], func=AF.Exp, scale=0.125),
                 reads=['sall', pzk], writes=[pzk])

            def fpv(e, b=b, pz=pz):
                last = None
                for h in range(8):
                    for c in range(3):
                        vi = (b % 2) * 3 + c
                        last = e.matmul(psO[:nt, h // 4, (h % 4) * 65:(h % 4) * 65 + 65], lhsT=pz[:, c * 8 + h, :],
                                        rhs=VAS[vi][:, h, :], start=(b == 0 and c == 0 and h % 4 == 0),
                                        stop=(b == NSAMP - 1 and c == 2 and h % 4 == 3), skip_group_check=True)
                return last
            S.op('pe', fpv, reads=[pzk] + [('vas', (b % 2) * 3 + c) for c in range(3)], writes=['psC'])
        S.op('dve', lambda e: e.tensor_tensor(out=prod[:nt], in0=qtok, in1=ktok, op=ALU.mult), reads=['kout'], writes=['prod'])
        S.op('dve', lambda e: e.tensor_reduce(out=ST[:nt, 32:40], in_=prod[:nt].rearrange("p (h d) -> p h d", d=64),
                                              axis=AX.X, op=ALU.add), reads=['prod'], writes=['st'])
        S.op('act', lambda e: e.activation(out=ST[:nt, 32:40], in_=ST[:nt, 32:40], func=AF.Exp, scale=0.125), reads=['st'],
             writes=['st'])
        S.op('dve', lambda e: e.tensor_scalar(out=ST[:nt, 32:40], in0=ST[:nt, 32:40], scalar1=3.0, scalar2=None,
                                              op0=ALU.mult), reads=['st'], writes=['st'])
        es = ST[:nt, 32:40].rearrange("p (b h) -> p b h", b=2)
        dn = ST[:nt, 48:56].rearrange("p (b h) -> p b h", b=2)
        S.op('dve', lambda e: e.tensor_tensor(out=dn, in0=psO_den[:nt], in1=es, op=ALU.add), reads=['psC', 'st'],
             writes=['st'])
        S.op('dve', lambda e: e.reciprocal(out=dn, in_=dn), reads=['st'], writes=['st'])
        S.op('dve', lambda e: e.tensor_tensor(out=v4(ob, nt), in0=v4(vout, nt),
                                              in1=es.unsqueeze(3).to_broadcast([nt, 2, 4, 64]), op=ALU.mult),
             reads=['vout', 'st'], writes=['ob'])
        S.op('dve', lambda e: e.tensor_tensor(out=v4(ob, nt), in0=v4(ob, nt), in1=psO_num[:nt], op=ALU.add),
             reads=['ob', 'psC'], writes=['ob'])
        S.op('dve', lambda e: e.tensor_tensor(out=v4(ob, nt), in0=v4(ob, nt),
                                              in1=dn.unsqueeze(3).to_broadcast([nt, 2, 4, 64]), op=ALU.mult),
             reads=['ob', 'st'], writes=['ob'])

    import os
    UPTO = int(os.environ.get("K_UPTO", "9"))
    if UPTO >= 1:
        for g in range(NHALO):
            do_tile(g, 'halo')
    if UPTO >= 2:
        for g in range(NHALO, NHALO + NMAIN):
            do_tile(g, 'main')
    if UPTO >= 3:
        do_tile(NHALO + NMAIN, 'sample')
    if UPTO < 4:
        DQ.flush()
        S.finish()
        return nc

    DQ.flush()
    S.barrier()
    ar.off = persist_end
    NG = 11
    ACC = ar.alloc([NG, 1024], F32)
    HNT = ar.alloc([NG, 8, 128], BF16)
    WGf = [ar.alloc([8, 256], F32) for _ in range(2)]
    WUf = [ar.alloc([8, 256], F32) for _ in range(2)]
    WDf = [ar.alloc([2, 1024], F32) for _ in range(2)]
    WGb = [ar.alloc([8, 256], BF16) for _ in range(2)]
    WUb = [ar.alloc([8, 256], BF16) for _ in range(2)]
    WDb = [ar.alloc([2, 1024], BF16) for _ in range(2)]
    SIL2 = [ar.alloc([2, 256], F32) for _ in range(2)]
    HID2 = [ar.alloc([2, 256], BF16) for _ in range(2)]
    psAB2 = [psB_flat.rearrange("p (q n) -> p q n", n=256), psC.rearrange("p b (q n) -> p (b q) n", n=256)]
    psAB = [psC[:, 0, :].rearrange("p (a b) -> p a b", b=128), psC[:, 1, :].rearrange("p (a b) -> p a b", b=128)]
    psD = [psA_flat, psB_flat]
    psDk = ['psA', 'psB']
    it = 0
    for (t0, t1) in GROUPS:
        for t in range(t0, t1):
            nt = NSAMP if t == NMAIN else 128
            j = t - t0
            S.dma(out=ACC[:nt, j, :], in_=hs[t * 128:t * 128 + nt, :], writes=[('acc', j, 0), ('acc', j, 1)])
            S.dma(out=HNT[:, j, :, :nt], in_=hnts[:, t, :, :nt], writes=[('hnt', j)])
        def prep(ex):
            wb = ex % 2
            S.dma(out=WGf[wb], in_=w_gate[ex].rearrange("(kc p) f -> p kc f", p=128), writes=[('wgf', wb)])
            S.dma(out=WUf[wb], in_=w_up[ex].rearrange("(kc p) f -> p kc f", p=128), writes=[('wuf', wb)])
            S.dma(out=WDf[wb], in_=w_down[ex].rearrange("(kc p) f -> p kc f", p=128), writes=[('wdf', wb)])
            S.op('dve', lambda e, wb=wb: e.tensor_tensor(out=WGb[wb], in0=WGf[wb],
                                                          in1=g2c.unsqueeze(2).to_broadcast([128, 8, 256]), op=ALU.mult),
                 reads=[('wgf', wb)], writes=[('wgb', wb)])
            S.op('dve', lambda e, wb=wb: e.tensor_tensor(out=WUb[wb], in0=WUf[wb],
                                                          in1=g2c.unsqueeze(2).to_broadcast([128, 8, 256]), op=ALU.mult),
                 reads=[('wuf', wb)], writes=[('wub', wb)])
            S.op('act', lambda e, wb=wb: e.copy(out=WDb[wb], in_=WDf[wb]), reads=[('wdf', wb)], writes=[('wdb', wb)])

        tl = list(range(t0, t1))
        batches = []
        while tl:
            if len(tl) >= 2 and tl[1] != NMAIN and tl[0] != NMAIN:
                batches.append(tl[:2]); tl = tl[2:]
            else:
                batches.append(tl[:1]); tl = tl[1:]
        units = [(ex, bt) for ex in range(32) for bt in batches]

        def gu(u):
            ex, bt = units[u]
            wb = ex % 2
            j = bt[0] - t0
            nb = len(bt)
            ncol = 128 * nb if bt[0] != NMAIN else NSAMP
            pab = psAB2[u % 2]
            if nb == 2:
                rhs_of = lambda kc: HNT[:, j:j + 2, kc, :]
                out_of = lambda q: pab[:, q, :].rearrange("p (a b) -> p a b", b=128)
            else:
                rhs_of = lambda kc: HNT[:, j, kc, :ncol]
                out_of = lambda q: pab[:, q, :ncol]

            def fgu(e):
                last = None
                for m, W in enumerate((WGb[wb], WUb[wb])):
                    for fcx in range(2):
                        for kc in range(8):
                            last = e.matmul(out_of(m * 2 + fcx), lhsT=W[:, kc, fcx * 128:(fcx + 1) * 128],
                                            rhs=rhs_of(kc), start=(kc == 0), stop=(kc == 7))
                return last
            S.op('pe', fgu, reads=[('wgb', wb), ('wub', wb)] + [('hnt', t - t0) for t in bt], writes=[('psab', u % 2)])

        prep(0)
        gu(0)
        hcnt = 0
        for u, (ex, bt) in enumerate(units):
            wb = ex % 2
            pb = u % 2
            pab = psAB2[pb]
            pabk = ('psab', pb)
            ncol = 128 * len(bt) if bt[0] != NMAIN else NSAMP
            if bt[0] == t0 and ex + 1 < 32:
                prep(ex + 1)
            if u + 1 < len(units):
                gu(u + 1)
            S.op('act', lambda e, pb=pb, ncol=ncol, pab=pab: e.activation(out=SIL2[pb][:, :, :ncol], in_=pab[:, 0:2, :ncol],
                                                                           func=AF.Silu), reads=[pabk], writes=[('sil', pb)])
            S.op('dve', lambda e, pb=pb, ncol=ncol, pab=pab: e.tensor_tensor(out=HID2[pb][:, :, :ncol],
                                                                              in0=SIL2[pb][:, :, :ncol],
                                                                              in1=pab[:, 2:4, :ncol], op=ALU.mult),
                 reads=[pabk, ('sil', pb)], writes=[('hid', pb)])
            for bi, t in enumerate(bt):
                nt = NSAMP if t == NMAIN else 128
                j = t - t0
                c0 = bi * 128
                for half in range(2):
                    hb = hcnt % 2
                    hcnt += 1
                    pdh = psA_flat[:, hb * 512:(hb + 1) * 512]
                    pdk = ('psd', hb)

                    def fdn(e, wb=wb, pb=pb, nt=nt, pdh=pdh, half=half, c0=c0):
                        last = None
                        for fcx in range(2):
                            last = e.matmul(pdh[:nt, :], lhsT=HID2[pb][:, fcx, c0:c0 + nt],
                                            rhs=WDb[wb][:, fcx, half * 512:(half + 1) * 512], start=(fcx == 0),
                                            stop=(fcx == 1))
                        return last
                    S.op('pe', fdn, reads=[('hid', pb), ('wdb', wb)], writes=[pdk])
                    S.op('dve', lambda e, j=j, nt=nt, pdh=pdh, t=t, ex=ex, half=half: e.scalar_tensor_tensor(
                        out=ACC[:nt, j, half * 512:(half + 1) * 512], in0=pdh[:nt, :], scalar=G[:nt, t, ex:ex + 1],
                        in1=ACC[:nt, j, half * 512:(half + 1) * 512], op0=ALU.mult, op1=ALU.add),
                        reads=[pdk, ('acc', j, half)], writes=[('acc', j, half)])
        for t in range(t0, t1):
            j = t - t0
            if t == NMAIN:
                S.dma(out=y_s, in_=ACC[:NSAMP, j, :], reads=[('acc', j, 0), ('acc', j, 1)])
            else:
                S.dma(out=y_p[t * 128:(t + 1) * 128, :], in_=ACC[:, j, :], reads=[('acc', j, 0), ('acc', j, 1)])
    S.finish()
    return nc


_NC_CACHE = {}


def _consts():
    ident = np.eye(128, dtype=np.float32)
    p = np.arange(128)
    bones = (p[:, None] // 64 == p[None, :] // 64).astype(np.float32)
    tri = (p[:, None] <= p[None, :]).astype(np.float32)
    mult = np.zeros((128, NOFF, 128), np.float32)
    for o in range(NOFF):
        delta = 128 * o + p[None, :] - p[:, None]
        m = ((delta >= 0) & (delta <= 128)).astype(np.float32)
        m += ((delta >= 0) & (delta <= 512) & (delta % 4 == 0)).astype(np.float32)
        m += ((delta >= 0) & (delta <= 2048) & (delta % 16 == 0)).astype(np.float32)
        mult[:, o, :] = m
    sel = np.zeros((16, 16, 128), np.float32)
    for b in range(16):
        sel[b, b, :] = 1.0
    return ident, bones, tri, mult, sel


def kernel(x_prompt, x_sample, cache_k, cache_v, norm1_g, w_in, q_gain, k_gain, v_gain,
           w_spatial, b_spatial, out_gain_a, out_gain_b, w_out, norm2_g, w_router1, b_router1,
           w_router2, b_router2, w_up, w_gate, w_down):
    f = lambda a: np.ascontiguousarray(np.asarray(a, dtype=np.float32))
    x_prompt, x_sample = f(x_prompt), f(x_sample)
    cache_k, cache_v = np.asarray(cache_k, dtype=np.float32), np.asarray(cache_v, dtype=np.float32)
    if 'nc' not in _NC_CACHE:
        _NC_CACHE['nc'] = build_nc()
    nc = _NC_CACHE['nc']
    ident, bones, tri, mult, sel = _consts()
    col = lambda v: f(np.asarray(v).reshape(8, 128).T)
    g1c, g2c = col(norm1_g[0]), col(norm2_g[0])
    ogc = col(np.concatenate([np.asarray(out_gain_a[0]), np.asarray(out_gain_b[0])]))
    qg = np.asarray(q_gain[0]).reshape(4, 128).T
    kg = np.asarray(k_gain[0]).reshape(4, 128).T
    gqk = f(np.concatenate([qg, kg], axis=1))
    vgain = f(np.broadcast_to(np.asarray(v_gain[0]).reshape(1, 512), (128, 512)))
    bst = f(np.asarray(b_spatial[0]).T)
    ws00 = f(np.broadcast_to(np.asarray(w_spatial[0])[:, 0, 0].reshape(1, 8), (128, 8)))
    bs0 = f(np.broadcast_to(np.asarray(b_spatial[0])[:, 0].reshape(1, 8), (128, 8)))
    br = f(np.broadcast_to(np.concatenate([np.asarray(b_router1[0]).reshape(4), np.asarray(b_router2[0]).reshape(32)]
                                          ).reshape(1, 36), (128, 36)))
    wsT = f(np.transpose(np.asarray(w_spatial[0]), (2, 0, 1)))
    w_r = f(np.concatenate([np.asarray(w_router1[0]),
                            np.transpose(np.asarray(w_router2[0]), (1, 0, 2)).reshape(D, 32)], axis=1))
    wg = f(np.asarray(w_gate[0]).reshape(32, D, 256))
    wu = f(np.asarray(w_up[0]).reshape(32, D, 256))
    wd = f(np.asarray(w_down[0]).reshape(32, 256, D))
    w_in0, w_out0 = f(w_in[0]), f(w_out[0])
    in_maps = []
    for c in range(NCORES):
        b, hf = c // 2, c % 2
        xp = np.zeros((48 * 128, D), np.float32)
        if hf == 1:
            xp[:2048] = x_prompt[b, 2048:4096]
        xp[2048:] = x_prompt[b, hf * 4096:(hf + 1) * 4096]
        in_maps.append({
            "xp": xp, "xs": f(x_sample[16 * c:16 * c + 16, 0]),
            "ck": f(cache_k[0, 16 * c:16 * c + 16].reshape(16, 2048, 512)),
            "cv": f(cache_v[0, 16 * c:16 * c + 16].reshape(16, 2048, 512)),
            "w_in": w_in0, "w_out": w_out0, "w_r": w_r, "w_gate": wg, "w_up": wu, "w_down": wd, "wsT": wsT,
            "g1c": g1c, "g2c": g2c, "ogc": ogc, "gqk": gqk, "vgain": vgain, "bst": bst, "ws00": ws00, "bs0": bs0,
            "br": br, "flag": np.full((128, 1), float(hf), np.float32),
            "c_ident": ident, "c_bones": bones, "c_tri": tri, "c_mult": mult,
        })
    res = run_bass_kernel_spmd(nc, in_maps, core_ids=list(range(NCORES)))
    R = res.results
    B, T = 4, 8192
    y_prompt = np.zeros((B, T, D), np.float32)
    y_sample = np.zeros((128, 1, D), np.float32)
    nkp = np.zeros((1, B, 2048, 8, 64), np.float32)
    nvp = np.zeros((1, B, 2048, 8, 64), np.float32)
    nks = np.zeros((1, 128, 1, 8, 64), np.float32)
    nvs = np.zeros((1, 128, 1, 8, 64), np.float32)
    nva = np.zeros((1, 128, 1, 8, 64), np.float32)
    for c in range(NCORES):
        b, hf = c // 2, c % 2
        r = R[c]
        y_prompt[b, hf * 4096:(hf + 1) * 4096] = r["y_p"]
        y_sample[16 * c:16 * c + 16, 0] = r["y_s"]
        if hf == 1:
            nkp[0, b] = r["nk"].reshape(2048, 8, 64)
            nvp[0, b] = r["nv"].reshape(2048, 8, 64)
        nks[0, 16 * c:16 * c + 16, 0] = r["nks"].reshape(16, 8, 64)
        nvs[0, 16 * c:16 * c + 16, 0] = r["nvs"].reshape(16, 8, 64)
        nva[0, 16 * c:16 * c + 16, 0] = r["nva"].reshape(16, 8, 64)
    return (y_prompt, y_sample, nkp, nvp, nks, nvs, nva)
```

```python
import numpy as np
import ml_dtypes
import concourse.bass as bass
import concourse.mybir as mybir
from concourse.bass_utils import run_bass_kernel_spmd

F32 = mybir.dt.float32
BF16 = mybir.dt.bfloat16
U8 = mybir.dt.uint8
ALU = mybir.AluOpType
AF = mybir.ActivationFunctionType
AX = mybir.AxisListType

D = 1024
NCORES = 8
NHALO = 16
NMAIN = 32
RING = 20
NOFF = 17
NSAMP = 16
NTILES = NMAIN + 1
EPS = 1e-6
NDS = 24
GROUPS = [(0, 17), (17, 33)]


class Sched:
    def __init__(self, nc):
        self.nc = nc
        self.ce = ['pe', 'act', 'dve', 'pool']
        self.prog = {e: [] for e in self.ce + ['sp']}
        self.sem = {e: nc.alloc_semaphore("s_" + e) for e in self.ce}
        self.cnt = {e: 0 for e in self.ce}
        self.dsem = [nc.alloc_semaphore("d%d" % i) for i in range(NDS)]
        self.dcnt = [0] * NDS
        self.dnext = 0
        self.waited = {}
        self.lastw = {}
        self.readers = {}

    def _semh(self, sk):
        return self.sem[sk[1]] if sk[0] == 'e' else self.dsem[sk[1]]

    def _deps(self, e, reads, writes):
        need = {}

        def add(tok):
            if tok is None:
                return
            sk, v, te = tok
            if te == e and e == 'pe':
                return
            if v > need.get(sk, 0):
                need[sk] = v
        for k in reads:
            add(self.lastw.get(k))
        for k in writes:
            add(self.lastw.get(k))
            for sk, (v, te) in self.readers.get(k, {}).items():
                add((sk, v, te))
        out = []
        for sk, v in need.items():
            if self.waited.get((e, sk), 0) >= v:
                continue
            self.waited[(e, sk)] = v
            out.append((self._semh(sk), v))
        return out

    def _commit(self, tok, reads, writes):
        sk, v, te = tok
        for k in reads:
            self.readers.setdefault(k, {})[sk] = (v, te)
        for k in writes:
            self.lastw[k] = tok
            self.readers[k] = {}

    def op(self, e, fn, reads=(), writes=()):
        waits = self._deps(e, reads, writes)
        self.cnt[e] += 1
        tok = (('e', e), self.cnt[e], e)
        sem = self.sem[e]

        def emit(eng):
            for s, v in waits:
                eng.wait_ge(s, v)
            fn(eng).then_inc(sem, 1)
        self.prog[e].append(emit)
        self._commit(tok, reads, writes)

    def dma(self, out, in_, reads=(), writes=(), q='sp'):
        waits = self._deps(q, reads, writes)
        i = self.dnext
        self.dnext = (i + 1) % NDS
        prev = self.dcnt[i]
        if prev > 0 and self.waited.get((q, ('d', i)), 0) < prev:
            waits.append((self.dsem[i], prev))
            self.waited[(q, ('d', i))] = prev
        self.dcnt[i] += 16
        tok = (('d', i), self.dcnt[i], 'dma')
        sem = self.dsem[i]

        def emit(eng):
            for s, v in waits:
                eng.wait_ge(s, v)
            eng.dma_start(out=out, in_=in_).then_inc(sem, 16)
        self.prog[q].append(emit)
        self._commit(tok, reads, writes)

    def barrier(self):
        allw = [(('e', e), self.cnt[e]) for e in self.ce if self.cnt[e] > 0]
        allw += [(('d', i), self.dcnt[i]) for i in range(NDS) if self.dcnt[i] > 0]
        for e in self.ce + ['sp']:
            waits = []
            for sk, v in allw:
                if sk == ('e', e) and e == 'pe':
                    continue
                if self.waited.get((e, sk), 0) >= v:
                    continue
                self.waited[(e, sk)] = v
                waits.append((self._semh(sk), v))

            def emit(eng, waits=waits):
                for s, v in waits:
                    eng.wait_ge(s, v)
            self.prog[e].append(emit)
        self.lastw = {}
        self.readers = {}

    def finish(self):
        self.barrier()
        nc = self.nc
        prog = self.prog
        with nc.Block() as block:
            @block.sync
            def _(eng):
                for f in prog['sp']:
                    f(eng)

            @block.tensor
            def _(eng):
                for f in prog['pe']:
                    f(eng)

            @block.scalar
            def _(eng):
                for f in prog['act']:
                    f(eng)

            @block.vector
            def _(eng):
                for f in prog['dve']:
                    f(eng)

            @block.gpsimd
            def _(eng):
                for f in prog['pool']:
                    f(eng)


class Arena:
    def __init__(self, nc, nbytes):
        self.ap = nc.alloc_sbuf_tensor("arena", [128, nbytes], U8).ap()
        self.off = 0
        self.nbytes = nbytes

    def alloc(self, free_shape, dtype):
        esz = 4 if dtype == F32 else 2
        n = int(np.prod(free_shape))
        nb = (n * esz + 31) // 32 * 32
        assert self.off + nb <= self.nbytes, ("arena overflow", self.off, nb)
        v = self.ap[:, self.off:self.off + n * esz].bitcast(dtype)
        self.off += nb
        if len(free_shape) == 2:
            v = v.rearrange("p (a b) -> p a b", b=free_shape[1])
        elif len(free_shape) == 3:
            v = v.rearrange("p (a b c) -> p a b c", b=free_shape[1], c=free_shape[2])
        return v


def build_nc(nhalo=16, nmain=32, groups=((0, 17), (17, 33))):
    global NHALO, NMAIN, NTILES, GROUPS
    NHALO, NMAIN, NTILES, GROUPS = nhalo, nmain, nmain + 1, list(groups)
    nc = bass.Bass("TRN2", target_bir_lowering=False)
    S = Sched(nc)

    def din(name, shape, dt=F32):
        return nc.dram_tensor(name, list(shape), dt, kind="ExternalInput").ap()

    def dout(name, shape, dt=F32):
        return nc.dram_tensor(name, list(shape), dt, kind="ExternalOutput").ap()

    xp = din("xp", [(NHALO + NMAIN) * 128, D])
    xs = din("xs", [NSAMP, D])
    ck = din("ck", [NSAMP, 2048, 512])
    cv = din("cv", [NSAMP, 2048, 512])
    w_in = din("w_in", [D, 2560])
    w_out = din("w_out", [D, D])
    w_r = din("w_r", [D, 36])
    w_gate = din("w_gate", [32, D, 256])
    w_up = din("w_up", [32, D, 256])
    w_down = din("w_down", [32, 256, D])
    wsT_d = din("wsT", [128, 8, 128])
    g1c_d = din("g1c", [128, 8])
    g2c_d = din("g2c", [128, 8])
    ogc_d = din("ogc", [128, 8])
    gqk_d = din("gqk", [128, 8])
    vgain_d = din("vgain", [128, 512])
    bst_d = din("bst", [128, 8])
    ws00_d = din("ws00", [128, 8])
    bs0_d = din("bs0", [128, 8])
    br_d = din("br", [128, 36])
    flag_d = din("flag", [128, 1])
    c_ident = din("c_ident", [128, 128])
    c_bones = din("c_bones", [128, 128])
    c_tri = din("c_tri", [128, 128])
    c_mult = din("c_mult", [128, NOFF, 128])

    y_p = dout("y_p", [NMAIN * 128, D])
    y_s = dout("y_s", [NSAMP, D])
    nk_o = dout("nk", [2048, 512])
    nv_o = dout("nv", [2048, 512])
    nks_o = dout("nks", [NSAMP, 512])
    nvs_o = dout("nvs", [NSAMP, 512])
    nva_o = dout("nva", [NSAMP, 512])

    hs = nc.dram_tensor("hs", [NTILES * 128, D], F32, kind="Internal").ap()
    hnts = nc.dram_tensor("hnts", [128, NTILES, 8, 128], BF16, kind="Internal").ap()

    ar = Arena(nc, 200 * 1024)
    G = ar.alloc([NTILES, 32], F32)
    g2c = ar.alloc([8], F32)
    persist_end = ar.off

    psA = nc.alloc_psum_tensor("psA", [128, 8, 128], F32).ap()
    psB = nc.alloc_psum_tensor("psB", [128, 8, 128], F32).ap()
    psC = nc.alloc_psum_tensor("psC", [128, 2, 512], F32).ap()
    psT = nc.alloc_psum_tensor("psT", [128, 8, 128], BF16).ap()
    psE = nc.alloc_psum_tensor("psE", [128, 512], F32).ap()
    psA_flat = psA.rearrange("p a b -> p (a b)")
    psB_flat = psB.rearrange("p a b -> p (a b)")

    WIN = ar.alloc([8, 2560], BF16)
    WOUT = ar.alloc([8, 1024], BF16)
    WR = ar.alloc([8, 36], BF16)
    WST = ar.alloc([8, 128], BF16)
    MULT = ar.alloc([NOFF, 128], BF16)
    MULTH = ar.alloc([NOFF, 128], BF16)
    identf = ar.alloc([128], F32)
    identb = ar.alloc([128], BF16)
    bonesb = ar.alloc([128], BF16)
    ONES16 = ar.alloc([128], F32)
    g1c = ar.alloc([8], F32)
    ogc = ar.alloc([8], F32)
    GQK = ar.alloc([8], F32)
    VGAIN = ar.alloc([512], F32)
    BST = ar.alloc([8], F32)
    WS00 = ar.alloc([8], F32)
    BS0 = ar.alloc([8], F32)
    BR = ar.alloc([36], F32)
    FLAG = ar.alloc([1], F32)
    EPSC = ar.alloc([1], F32)
    stg_off = ar.off
    STG = [ar.alloc([2560], F32) for _ in range(2)]

    def small_load(dst, src, key):
        S.dma(out=dst, in_=src, writes=[key])

    small_load(g1c, g1c_d, 'g1c')
    small_load(g2c, g2c_d, 'g2c')
    small_load(ogc, ogc_d, 'ogc')
    small_load(GQK, gqk_d, 'gqk')
    small_load(VGAIN, vgain_d, 'vgain')
    small_load(BST, bst_d, 'bst')
    small_load(WS00, ws00_d, 'ws00')
    small_load(BS0, bs0_d, 'bs0')
    small_load(BR, br_d, 'br')
    small_load(FLAG, flag_d, 'flag')
    small_load(identf, c_ident, 'identf')
    S.op('dve', lambda e: e.memset(ONES16, 1.0), writes=['ones16'])
    S.op('dve', lambda e: e.memset(EPSC, EPS), writes=['epsc'])
    S.op('dve', lambda e: e.memset(VA[:, :, :, 64:65], 1.0), writes=['va_ones'])
    for i in range(6):
        S.op('pool', lambda e, i=i: e.memset(VAS[i][:, :, 64:65], 1.0), writes=[('vas', i)])
    S.op('dve', lambda e: e.tensor_copy(out=identb, in_=identf), reads=['identf'], writes=['identb'])
    S.dma(out=STG[0][:, 0:128], in_=c_bones, writes=[('stg', 0)])
    S.op('dve', lambda e: e.tensor_copy(out=bonesb, in_=STG[0][:, 0:128]), reads=[('stg', 0)], writes=['bonesb'])
    S.dma(out=STG[1][:, 0:NOFF * 128], in_=c_mult.rearrange("p a b -> p (a b)"), writes=[('stg', 1)])
    S.op('dve', lambda e: e.tensor_copy(out=MULT.rearrange("p a b -> p (a b)"), in_=STG[1][:, 0:NOFF * 128]),
         reads=[('stg', 1)], writes=['mult'])
    S.op('dve', lambda e: e.tensor_scalar(out=MULTH.rearrange("p a b -> p (a b)"), in0=STG[1][:, 0:NOFF * 128],
                                          scalar1=FLAG[:, 0:1], scalar2=None, op0=ALU.mult),
         reads=[('stg', 1), 'flag'], writes=['multh'])
    S.dma(out=STG[0][:, 0:1024], in_=wsT_d.rearrange("p a b -> p (a b)"), reads=['bonesb'], writes=[('stg', 0)])
    S.dma(out=STG[0][:, 1024:1152], in_=c_tri, writes=[('stg', 0)])
    S.op('dve', lambda e: e.tensor_tensor(
        out=WST, in0=STG[0][:, 0:1024].rearrange("p (a b) -> p a b", b=128),
        in1=STG[0][:, 1024:1152].unsqueeze(1).to_broadcast([128, 8, 128]), op=ALU.mult),
        reads=[('stg', 0)], writes=['wst'])
    S.dma(out=STG[1][:, 0:288].rearrange("p (a b) -> p a b", b=36), in_=w_r.rearrange("(p kc) f -> p kc f", kc=8),
          reads=['mult', 'multh'], writes=[('stg', 1)])
    S.op('dve', lambda e: e.tensor_tensor(
        out=WR, in0=STG[1][:, 0:288].rearrange("p (a b) -> p a b", b=36),
        in1=g2c.unsqueeze(2).to_broadcast([128, 8, 36]), op=ALU.mult),
        reads=[('stg', 1), 'g2c'], writes=['wr'])
    for kc in range(8):
        st = STG[kc % 2]
        S.dma(out=st[:, 0:2560], in_=w_in[kc * 128:(kc + 1) * 128, :], reads=['wst', 'wr'], writes=[('stg', kc % 2)])
        eng = 'dve' if kc % 2 == 0 else 'pool'
        S.op(eng, lambda e, st=st, kc=kc: e.tensor_scalar(out=WIN[:, kc, :], in0=st[:, 0:2560], scalar1=g1c[:, kc:kc + 1],
                                                           scalar2=None, op0=ALU.mult),
             reads=[('stg', kc % 2), 'g1c'], writes=['win'])
    for kc in range(8):
        st = STG[kc % 2]
        S.dma(out=st[:, 0:1024], in_=w_out[kc * 128:(kc + 1) * 128, :], writes=[('stg', kc % 2)])
        eng = 'dve' if kc % 2 == 0 else 'pool'
        S.op(eng, lambda e, st=st, kc=kc: e.tensor_scalar(out=WOUT[:, kc, :], in0=st[:, 0:1024], scalar1=ogc[:, kc:kc + 1],
                                                           scalar2=None, op0=ALU.mult),
             reads=[('stg', kc % 2), 'ogc'], writes=['wout'])

    S.barrier()
    ar.off = stg_off
    XT = [ar.alloc([1024], F32) for _ in range(2)]
    xn = ar.alloc([1024], BF16)
    xnT = ar.alloc([8, 128], BF16)
    sq = ar.alloc([8, 128], BF16)
    rr = ar.alloc([8, 128], F32)
    qkn = ar.alloc([8, 128], F32)
    QT = ar.alloc([4, 2, 128], BF16)
    KT = ar.alloc([RING, 4, 128], BF16)
    VA = ar.alloc([RING, 8, 65], BF16)
    kout = ar.alloc([1024], F32)
    vout = ar.alloc([512], F32)
    u_t = ar.alloc([512], F32)
    gg = ar.alloc([512], F32)
    gsq = ar.alloc([512], F32)
    vg = ar.alloc([512], F32)
    vgb = ar.alloc([512], BF16)
    oa = ar.alloc([512], F32)
    ob = ar.alloc([512], F32)
    oab = ar.alloc([1024], BF16)
    oT = ar.alloc([8, 128], BF16)
    P = [ar.alloc([8, 128], BF16) for _ in range(2)]
    hh = ar.alloc([1024], F32)
    hn = ar.alloc([1024], BF16)
    hnT = ar.alloc([8, 128], BF16)
    junk = ar.alloc([1024], BF16)
    ST = ar.alloc([64], F32)
    RL = ar.alloc([96], F32)
    KC = [ar.alloc([512], F32) for _ in range(2)]
    VC = [ar.alloc([512], F32) for _ in range(2)]
    prod = ar.alloc([512], F32)
    SALL = ar.alloc([24], F32)
    VAS = [ar.alloc([8, 65], BF16) for _ in range(6)]
    PZ = [ar.alloc([24, 16], BF16) for _ in range(2)]
    stage1_end = ar.off
    print('stage1 arena bytes', stage1_end)

    class Defer:
        def __init__(self):
            self.q = []

        def op(self, *a, **k):
            self.q.append(lambda: S.op(*a, **k))

        def dma(self, *a, **k):
            self.q.append(lambda: S.dma(*a, **k))

        def pop(self, n):
            for _ in range(n):
                if self.q:
                    self.q.pop(0)()

        def flush(self):
            self.pop(len(self.q))

    DQ = Defer()

    def rstd(nt, src, dst, scale, tmpkey, Q=None):
        Q = Q or S
        Q.op('act', lambda e: e.activation(out=dst, in_=src, func=AF.Sqrt, bias=EPSC[:nt, 0:1], scale=scale),
             reads=['st'], writes=['st'])
        Q.op('dve', lambda e: e.reciprocal(out=dst, in_=dst), reads=['st'], writes=['st'])

    def transposes_bf(nt, src, dstT, rkey, wkey, interleaved=False):
        def f(e):
            last = None
            for kc in range(8):
                cols = src[:nt, kc:1024:8] if interleaved else src[:nt, kc * 128:(kc + 1) * 128]
                last = e.transpose(out=psT[:, kc, :nt], in_=cols, identity=identb[:nt, :nt])
            return last
        S.op('pe', f, reads=[rkey], writes=['psT'])
        S.op('act', lambda e: e.copy(out=dstT[:, :, :nt], in_=psT[:, :, :nt]), reads=['psT'], writes=[wkey])

    def v3(ap, nt):
        return ap[:nt].rearrange("p (h d) -> p h d", d=64)

    def v4(ap, nt):
        return ap[:nt].rearrange("p (b h d) -> p b h d", b=2, d=64)

    psO = psC
    psO_num = psC[:, :, 0:260].rearrange("p b (h e) -> p b h e", e=65)[:, :, :, 0:64]
    psO_den = psC[:, :, 64:260:65]

    def load_x(g):
        if g == NHALO + NMAIN:
            S.dma(out=XT[g % 2][:NSAMP], in_=xs, writes=[('xt', g % 2)])
        else:
            S.dma(out=XT[g % 2], in_=xp[g * 128:(g + 1) * 128, :], writes=[('xt', g % 2)])

    import os
    CUT = int(os.environ.get('K_CUT', '99'))
    CUT2 = int(os.environ.get('K_CUT2', '99'))
    CUT3 = int(os.environ.get('K_CUT3', '99'))

    def do_tile(g, kind):
        nt = NSAMP if kind == 'sample' else 128
        xt = XT[g % 2]
        xtk = ('xt', g % 2)
        mt = g - NHALO if kind != 'sample' else NMAIN
        if g == 0:
            load_x(0)
        if g + 1 <= NHALO + NMAIN:
            load_x(g + 1)
        S.op('act', lambda e: e.activation(out=junk[:nt], in_=xt[:nt], func=AF.Square, accum_out=ST[:nt, 0:1]),
             reads=[xtk], writes=['junk', 'st'])
        rstd(nt, ST[:nt, 0:1], ST[:nt, 1:2], 1.0 / D, 'st')
        S.op('dve', lambda e: e.tensor_scalar(out=xn[:nt], in0=xt[:nt], scalar1=ST[:nt, 1:2], scalar2=None, op0=ALU.mult),
             reads=[xtk, 'st'], writes=['xn'])
        transposes_bf(nt, xn, xnT, 'xn', 'xnT')
        fcs = list(range(4, 8)) if kind == 'halo' else list(range(8))
        f0, f1 = fcs[0], fcs[-1] + 1
        nf = f1 - f0

        def fqk(e):
            last = None
            for fc in fcs:
                for kc in range(8):
                    last = e.matmul(psA[:, fc, :nt], lhsT=WIN[:, kc, fc * 128:(fc + 1) * 128], rhs=xnT[:, kc, :nt],
                                    start=(kc == 0), stop=(kc == 7))
            return last
        S.op('pe', fqk, reads=['xnT', 'win'], writes=['psA'])

        def fv(e):
            last = None
            dsts = [(psE, 1024)] if kind == 'halo' else [(psE, 1024), (psC[:, 0, :], 1536), (psC[:, 1, :], 2048)]
            for dst, c0 in dsts:
                for kc in range(8):
                    last = e.matmul(dst[:nt, 0:512], lhsT=xnT[:, kc, :nt], rhs=WIN[:, kc, c0:c0 + 512],
                                    start=(kc == 0), stop=(kc == 7))
            return last
        S.op('pe', fv, reads=['xnT', 'win'], writes=['psE'] if kind == 'halo' else ['psE', 'psC'])
        S.op('act', lambda e: e.activation(out=sq[:, f0:f1, :nt], in_=psA[:, f0:f1, :nt], func=AF.Square),
             reads=['psA'], writes=['sq'])

        def fss(e):
            last = None
            for fc in fcs:
                last = e.matmul(psB[:, fc, :nt], lhsT=bonesb, rhs=sq[:, fc, :nt], start=True, stop=True)
            return last
        S.op('pe', fss, reads=['sq', 'bonesb'], writes=['psB'])
        S.op('act', lambda e: e.activation(out=rr[:, f0:f1, :nt], in_=psB[:, f0:f1, :nt], func=AF.Sqrt, bias=EPSC[:, 0:1],
                                           scale=1.0 / 64), reads=['psB'], writes=['rr'])
        S.op('dve', lambda e: e.reciprocal(out=rr[:, f0:f1, :nt], in_=rr[:, f0:f1, :nt]), reads=['rr'], writes=['rr'])
        S.op('dve', lambda e: e.tensor_tensor(out=qkn[:, f0:f1, :nt], in0=psA[:, f0:f1, :nt],
                                              in1=GQK[:, f0:f1].unsqueeze(2).to_broadcast([128, nf, nt]), op=ALU.mult),
             reads=['psA', 'gqk'], writes=['qkn'])
        S.op('dve', lambda e: e.tensor_tensor(out=qkn[:, f0:f1, :nt], in0=qkn[:, f0:f1, :nt], in1=rr[:, f0:f1, :nt],
                                              op=ALU.mult), reads=['qkn', 'rr'], writes=['qkn'])
        slot = g % RING
        if kind != 'sample':
            S.op('dve', lambda e: e.tensor_copy(out=KT[:, slot, :, :], in_=qkn[:, 4:8, :]), reads=['qkn'],
                 writes=[('kt', slot)])
        if kind == 'main':
            if g == NHALO:
                S.op('dve', lambda e: e.memset(QT, 0.0), writes=['qt'])
            S.op('act', lambda e: e.copy(out=QT[0:64, :, 0, :], in_=qkn[0:64, 0:4, :]), reads=['qkn'], writes=['qt'])
            S.op('act', lambda e: e.copy(out=QT[64:128, :, 1, :], in_=qkn[64:128, 0:4, :]), reads=['qkn'],
                 writes=['qt'])
        if kind == 'main' and CUT <= 1:
            return
        KO0 = NHALO + max(NMAIN - 16, 0)
        want_kout = (kind == 'sample') or (kind == 'main' and g >= KO0)
        if want_kout:
            tf = list(range(8)) if kind == 'sample' else list(range(4, 8))

            def ftr(e):
                last = None
                for fc in tf:
                    last = e.transpose(out=psB_flat[:nt, fc * 128:(fc + 1) * 128], in_=qkn[:, fc, :nt], identity=identf)
                return last
            S.op('pe', ftr, reads=['qkn', 'identf'], writes=['psB'])
            t0 = tf[0] * 128
            S.op('dve', lambda e: e.tensor_copy(out=kout[:nt, t0:1024], in_=psB_flat[:nt, t0:1024]), reads=['psB'],
                 writes=['kout'])
            if kind == 'sample':
                S.dma(out=nks_o, in_=kout[:nt, 512:1024], reads=['kout'])
            else:
                r0 = (g - KO0) * 128
                S.dma(out=nk_o[r0:r0 + 128, :], in_=kout[:, 512:1024], reads=['kout'])
        if kind == 'main' and CUT <= 2:
            return
        if kind != 'sample':
            S.op('act', lambda e: e.copy(out=VA[:nt, slot, :, 0:64], in_=v3(psE, nt)), reads=['psE', 'va_ones'],
                 writes=[('va', slot)])
        if want_kout:
            S.op('dve', lambda e: e.tensor_copy(out=vout[:nt], in_=psE[:nt, 0:512]), reads=['psE'], writes=['vout'])
            if kind == 'sample':
                S.dma(out=nvs_o, in_=vout[:nt], reads=['vout'])
            else:
                r0 = (g - KO0) * 128
                S.dma(out=nv_o[r0:r0 + 128, :], in_=vout, reads=['vout'])
        if kind == 'main' and CUT <= 3:
            return
        if kind == 'halo':
            return
        S.op('act', lambda e: e.activation(out=u_t[:nt], in_=psC[:nt, 0, :], func=AF.Gelu_apprx_tanh), reads=['psC'],
             writes=['u'])
        S.op('act', lambda e: e.activation(out=gg[:nt], in_=psC[:nt, 1, :], func=AF.Gelu_apprx_tanh), reads=['psC'],
             writes=['gg'])
        Q = DQ if kind == 'main' else S
        if kind == 'sample':
            DQ.flush()
        Q.op('dve', lambda e: e.tensor_tensor(out=gsq[:nt], in0=gg[:nt], in1=gg[:nt], op=ALU.mult), reads=['gg'],
             writes=['gsq'])
        Q.op('dve', lambda e: e.tensor_reduce(out=ST[:nt, 8:16], in_=v3(gsq, nt), axis=AX.X, op=ALU.add), reads=['gsq'],
             writes=['st'])
        rstd(nt, ST[:nt, 8:16], ST[:nt, 16:24], 1.0 / 64, 'st', Q)
        Q.op('dve', lambda e: e.tensor_tensor(out=v3(vg, nt), in0=v3(gg, nt),
                                              in1=ST[:nt, 16:24].unsqueeze(2).to_broadcast([nt, 8, 64]), op=ALU.mult),
             reads=['gg', 'st'], writes=['vg'])
        Q.op('dve', lambda e: e.tensor_tensor(out=vg[:nt], in0=vg[:nt], in1=VGAIN[:nt], op=ALU.mult),
             reads=['vg', 'vgain'], writes=['vg'])
        if kind == 'sample':
            Q.dma(out=nva_o, in_=vg[:nt], reads=['vg'])
            Q.op('dve', lambda e: e.tensor_tensor(out=v3(oa, nt), in0=v3(vg, nt),
                                                  in1=WS00[:nt].unsqueeze(2).to_broadcast([nt, 8, 64]), op=ALU.mult),
                 reads=['vg', 'ws00'], writes=['oa'])
            Q.op('dve', lambda e: e.tensor_tensor(out=v3(oa, nt), in0=v3(oa, nt),
                                                  in1=BS0[:nt].unsqueeze(2).to_broadcast([nt, 8, 64]), op=ALU.add),
                 reads=['oa', 'bs0'], writes=['oa'])
        else:
            Q.op('act', lambda e: e.copy(out=vgb, in_=vg), reads=['vg'], writes=['vgb'])

            def fmix(e):
                last = None
                for h in range(8):
                    last = e.matmul(psE[:, h * 64:(h + 1) * 64], lhsT=WST[:, h, :], rhs=vgb[:, h * 64:(h + 1) * 64],
                                    start=True, stop=True)
                return last
            Q.op('pe', fmix, reads=['vgb', 'wst'], writes=['psE'])
            Q.op('dve', lambda e: e.tensor_tensor(out=v3(oa, nt), in0=v3(psE, nt),
                                                  in1=BST.unsqueeze(2).to_broadcast([128, 8, 64]), op=ALU.add),
                 reads=['psE', 'bst'], writes=['oa'])
        Q.op('dve', lambda e: e.tensor_tensor(out=oa[:nt], in0=oa[:nt], in1=u_t[:nt], op=ALU.mult), reads=['oa', 'u'],
             writes=['oa'])
        Q.op('act', lambda e: e.activation(out=junk[:nt, 0:512], in_=oa[:nt], func=AF.Square, accum_out=ST[:nt, 24:25]),
             reads=['oa'], writes=['junk', 'st'])
        rstd(nt, ST[:nt, 24:25], ST[:nt, 25:26], 1.0 / 512, 'st', Q)
        Q.op('dve', lambda e: e.tensor_scalar(out=oab[:nt, 0:512], in0=oa[:nt], scalar1=ST[:nt, 25:26], scalar2=None,
                                              op0=ALU.mult), reads=['oa', 'st'], writes=['oab'])
        if kind == 'main' and CUT <= 4:
            return
        if kind == 'main':
            def s_op(i):
                o = NOFF - 1 - i
                kt = g - o
                sl = kt % RING
                ps = psA if i % 2 == 0 else psB
                pk = 'psA' if i % 2 == 0 else 'psB'

                def f(e):
                    last = None
                    for hp in range(4):
                        last = e.matmul(ps[:, 2 * hp:2 * hp + 2, :], lhsT=KT[:, sl, hp, :], rhs=QT[:, hp, :, :],
                                        start=True, stop=True)
                    return last
                S.op('pe', f, reads=[('kt', sl), 'qt'], writes=[pk])
            s_op(0)
            for i in range(NOFF):
                o = NOFF - 1 - i
                kt = g - o
                sl = kt % RING
                ps = psA if i % 2 == 0 else psB
                pk = 'psA' if i % 2 == 0 else 'psB'
                Pi = P[i % 2]
                pik = ('P', i % 2)
                if i + 1 < NOFF:
                    s_op(i + 1)
                if CUT3 < 2:
                    continue
                S.op('act', lambda e, ps=ps, Pi=Pi: e.activation(out=Pi, in_=ps, func=AF.Exp, scale=0.125), reads=[pk],
                     writes=[pik])
                if CUT3 < 3:
                    continue
                M = MULTH if kt < NHALO else MULT
                S.op('dve', lambda e, Pi=Pi, M=M, o=o: e.tensor_tensor(
                    out=Pi, in0=Pi, in1=M[:, o, :].unsqueeze(1).to_broadcast([128, 8, 128]), op=ALU.mult),
                    reads=[pik, 'mult', 'multh'], writes=[pik])

                if CUT3 < 4:
                    continue

                def fpv(e, Pi=Pi, sl=sl, i=i):
                    last = None
                    for h in range(8):
                        last = e.matmul(psO[:, h // 4, (h % 4) * 65:(h % 4) * 65 + 65], lhsT=Pi[:, h, :],
                                        rhs=VA[:, sl, h, :], start=(i == 0 and h % 4 == 0), stop=(i == NOFF - 1 and h % 4 == 3),
                                        skip_group_check=True)
                    return last
                S.op('pe', fpv, reads=[pik, ('va', sl)], writes=['psC'])
                DQ.pop(-(-len(DQ.q) // (NOFF - i)))
            DQ.flush()
            if CUT3 < 5:
                return
            S.op('dve', lambda e: e.reciprocal(out=ST[:, 32:40].rearrange("p (b h) -> p b h", b=2), in_=psO_den),
                 reads=['psC'], writes=['st'])
            if CUT3 < 6:
                return
            S.op('dve', lambda e: e.tensor_tensor(
                out=v4(ob, nt), in0=psO_num,
                in1=ST[:, 32:40].rearrange("p (b h) -> p b h", b=2).unsqueeze(3).to_broadcast([128, 2, 4, 64]),
                op=ALU.mult), reads=['psC', 'st'], writes=['ob'])
        else:
            sample_attention()
        if kind == 'main' and CUT <= 5:
            return
        S.op('act', lambda e: e.activation(out=junk[:nt, 0:512], in_=ob[:nt], func=AF.Square, accum_out=ST[:nt, 26:27]),
             reads=['ob'], writes=['junk', 'st'])
        rstd(nt, ST[:nt, 26:27], ST[:nt, 27:28], 1.0 / 512, 'st')
        S.op('dve', lambda e: e.tensor_scalar(out=oab[:nt, 512:1024], in0=ob[:nt], scalar1=ST[:nt, 27:28], scalar2=None,
                                              op0=ALU.mult), reads=['ob', 'st'], writes=['oab'])
        transposes_bf(nt, oab, oT, 'oab', 'oT')

        def fwo(e):
            last = None
            for half in range(2):
                for kc in range(8):
                    last = e.matmul(psA_flat[:nt, half * 512:(half + 1) * 512], lhsT=oT[:, kc, :nt],
                                    rhs=WOUT[:, kc, half * 512:(half + 1) * 512], start=(kc == 0), stop=(kc == 7))
            return last
        S.op('pe', fwo, reads=['oT', 'wout'], writes=['psA'])
        S.op('dve', lambda e: e.tensor_tensor(out=hh[:nt], in0=psA_flat[:nt], in1=xt[:nt], op=ALU.add),
             reads=['psA', xtk], writes=['hh'])
        S.dma(out=hs[mt * 128:mt * 128 + nt, :], in_=hh[:nt], reads=['hh'], writes=[('hs', mt)])
        if kind == 'main' and CUT <= 6:
            return
        S.op('act', lambda e: e.activation(out=junk[:nt], in_=hh[:nt], func=AF.Square, accum_out=ST[:nt, 28:29]),
             reads=['hh'], writes=['junk', 'st'])
        rstd(nt, ST[:nt, 28:29], ST[:nt, 29:30], 1.0 / D, 'st')
        S.op('dve', lambda e: e.tensor_scalar(out=hn[:nt], in0=hh[:nt], scalar1=ST[:nt, 29:30], scalar2=None, op0=ALU.mult),
             reads=['hh', 'st'], writes=['hn'])
        transposes_bf(nt, hn, hnT, 'hn', 'hnT', interleaved=True)
        S.dma(out=hnts[:, mt, :, :nt], in_=hnT[:, :, :nt], reads=['hnT'], writes=[('hnts', mt)])
        if kind == 'main' and CUT <= 7:
            return

        def frt(e):
            last = None
            for kc in range(8):
                last = e.matmul(psE[:nt, 0:36], lhsT=hnT[:, kc, :nt], rhs=WR[:, kc, :], start=(kc == 0), stop=(kc == 7))
            return last
        S.op('pe', frt, reads=['hnT', 'wr'], writes=['psE'])
        L = RL[:nt, 0:36]
        S.op('dve', lambda e: e.tensor_tensor(out=L, in0=psE[:nt, 0:36], in1=BR[:nt], op=ALU.add), reads=['psE', 'br'],
             writes=['rl'])
        RQ = DQ if kind == 'main' else S
        RQ.op('dve', lambda e: e.tensor_reduce(out=RL[:nt, 36:37], in_=RL[:nt, 0:4], axis=AX.X, op=ALU.max), reads=['rl'],
             writes=['rl'])
        RQ.op('dve', lambda e: e.tensor_scalar(out=RL[:nt, 40:44], in0=RL[:nt, 0:4], scalar1=RL[:nt, 36:37], scalar2=None,
                                              op0=ALU.is_equal), reads=['rl'], writes=['rl'])
        RQ.op('dve', lambda e: e.tensor_scalar(out=RL[:nt, 37:38], in0=RL[:nt, 36:37], scalar1=-1.0, scalar2=None,
                                              op0=ALU.mult), reads=['rl'], writes=['rl'])
        RQ.op('act', lambda e: e.activation(out=RL[:nt, 44:48], in_=RL[:nt, 0:4], func=AF.Exp, bias=RL[:nt, 37:38], scale=1.0,
                                           accum_out=RL[:nt, 38:39]), reads=['rl'], writes=['rl'])
        RQ.op('dve', lambda e: e.reciprocal(out=RL[:nt, 39:40], in_=RL[:nt, 38:39]), reads=['rl'], writes=['rl'])
        RQ.op('dve', lambda e: e.tensor_tensor(
            out=RL[:nt, 48:80].rearrange("p (g x) -> p g x", x=8), in0=RL[:nt, 4:36].rearrange("p (g x) -> p g x", x=8),
            in1=RL[:nt, 40:44].unsqueeze(2).to_broadcast([nt, 4, 8]), op=ALU.mult), reads=['rl'], writes=['rl'])
        RQ.op('dve', lambda e: e.tensor_reduce(out=RL[:nt, 80:88], in_=RL[:nt, 48:80].rearrange("p (g x) -> p x g", x=8),
                                              axis=AX.X, op=ALU.add), reads=['rl'], writes=['rl'])
        RQ.op('dve', lambda e: e.max(out=RL[:nt, 88:96], in_=RL[:nt, 80:88]), reads=['rl'], writes=['rl'])
        RQ.op('dve', lambda e: e.tensor_tensor(out=ST[:nt, 40:41], in0=RL[:nt, 89:90], in1=RL[:nt, 88:89], op=ALU.subtract),
             reads=['rl'], writes=['st'])
        RQ.op('act', lambda e: e.activation(out=ST[:nt, 41:42], in_=ST[:nt, 40:41], func=AF.Exp), reads=['st'], writes=['st'])
        RQ.op('dve', lambda e: e.tensor_scalar(out=ST[:nt, 41:42], in0=ST[:nt, 41:42], scalar1=1.0, scalar2=None, op0=ALU.add),
             reads=['st'], writes=['st'])
        RQ.op('dve', lambda e: e.reciprocal(out=ST[:nt, 42:43], in_=ST[:nt, 41:42]), reads=['st'], writes=['st'])
        RQ.op('dve', lambda e: e.tensor_scalar(out=ST[:nt, 43:44], in0=ST[:nt, 42:43], scalar1=-1.0, scalar2=1.0, op0=ALU.mult,
                                              op1=ALU.add), reads=['st'], writes=['st'])
        RQ.op('dve', lambda e: e.tensor_tensor(out=ST[:nt, 42:44], in0=ST[:nt, 42:44],
                                              in1=RL[:nt, 39:40].to_broadcast([nt, 2]), op=ALU.mult), reads=['st', 'rl'],
             writes=['st'])
        RQ.op('dve', lambda e: e.tensor_scalar(out=ST[:nt, 44:52], in0=RL[:nt, 80:88], scalar1=RL[:nt, 88:89],
                                              scalar2=ST[:nt, 42:43], op0=ALU.is_equal, op1=ALU.mult), reads=['rl', 'st'],
             writes=['st'])
        RQ.op('dve', lambda e: e.tensor_scalar(out=ST[:nt, 52:60], in0=RL[:nt, 80:88], scalar1=RL[:nt, 89:90],
                                              scalar2=ST[:nt, 43:44], op0=ALU.is_equal, op1=ALU.mult), reads=['rl', 'st'],
             writes=['st'])
        RQ.op('dve', lambda e: e.tensor_tensor(out=ST[:nt, 44:52], in0=ST[:nt, 44:52], in1=ST[:nt, 52:60], op=ALU.add),
             reads=['st'], writes=['st'])
        RQ.op('dve', lambda e: e.tensor_tensor(
            out=G[:nt, mt, :].rearrange("p (g x) -> p g x", x=8),
            in0=RL[:nt, 40:44].unsqueeze(2).to_broadcast([nt, 4, 8]),
            in1=ST[:nt, 44:52].unsqueeze(1).to_broadcast([nt, 4, 8]), op=ALU.mult), reads=['rl', 'st'], writes=['G'])

    def sample_attention():
        nt = NSAMP
        qtok = kout[:nt, 0:512]
        ktok = kout[:nt, 512:1024]
        rows = [(1920, 1), (1536, 4), (0, 16)]
        for b in range(NSAMP):
            S.op('dve', lambda e, b=b: e.tensor_scalar(out=gsq[:nt], in0=qtok, scalar1=identf[:nt, b:b + 1], scalar2=None,
                                                        op0=ALU.mult), reads=['kout', 'identf'], writes=['gsq'])

            def fqb(e, b=b):
                return e.matmul(psE[:, 0:512], lhsT=ONES16[0:16, :], rhs=gsq[:nt], start=True, stop=True)
            S.op('pe', fqb, reads=['gsq', 'ones16'], writes=['psE'])
            pz = PZ[b % 2]
            pzk = ('pz', b % 2)
            S.op('pool', lambda e, pz=pz: e.memset(pz, 0.0), writes=[pzk])
            for c, (r0, stp) in enumerate(rows):
                bi = (b * 3 + c) % 2
                kc_t, vc_t = KC[bi], VC[bi]
                S.dma(out=kc_t, in_=ck[b, r0:r0 + 128 * stp:stp, :], writes=[('kc', bi)])
                S.dma(out=vc_t, in_=cv[b, r0:r0 + 128 * stp:stp, :], writes=[('vc', bi)])
                S.op('dve', lambda e, kc_t=kc_t: e.tensor_tensor(out=prod, in0=kc_t, in1=psE[:, 0:512], op=ALU.mult),
                     reads=[('kc', bi), 'psE'], writes=['prod'])
                S.op('dve', lambda e, c=c: e.tensor_reduce(out=SALL[:, c * 8:(c + 1) * 8],
                                                           in_=prod.rearrange("p (h d) -> p h d", d=64), axis=AX.X,
                                                           op=ALU.add), reads=['prod'], writes=['sall'])
                vi = (b % 2) * 3 + c
                S.op('act', lambda e, vi=vi, vc_t=vc_t: e.copy(out=VAS[vi][:, :, 0:64],
                                                               in_=vc_t.rearrange("p (h d) -> p h d", d=64)),
                     reads=[('vc', bi), ('vas', vi)], writes=[('vas', vi)])
            S.op('act', lambda e, pz=pz, b=b: e.activation(out=pz[:, :, b], in_=SALL[:, 0:24], func=AF.Exp, scale=0.125),
                 reads=['sall', pzk], writes=[pzk])

            def fpv(e, b=b, pz=pz):
                last = None
                for h in range(8):
                    for c in range(3):
                        vi = (b % 2) * 3 + c
                        last = e.matmul(psO[:nt, h // 4, (h % 4) * 65:(h % 4) * 65 + 65], lhsT=pz[:, c * 8 + h, :],
                                        rhs=VAS[vi][:, h, :], start=(b == 0 and c == 0 and h % 4 == 0),
                                        stop=(b == NSAMP - 1 and c == 2 and h % 4 == 3), skip_group_check=True)
                return last
            S.op('pe', fpv, reads=[pzk] + [('vas', (b % 2) * 3 + c) for c in range(3)], writes=['psC'])
        S.op('dve', lambda e: e.tensor_tensor(out=prod[:nt], in0=qtok, in1=ktok, op=ALU.mult), reads=['kout'], writes=['prod'])
        S.op('dve', lambda e: e.tensor_reduce(out=ST[:nt, 32:40], in_=prod[:nt].rearrange("p (h d) -> p h d", d=64),
                                              axis=AX.X, op=ALU.add), reads=['prod'], writes=['st'])
        S.op('act', lambda e: e.activation(out=ST[:nt, 32:40], in_=ST[:nt, 32:40], func=AF.Exp, scale=0.125), reads=['st'],
             writes=['st'])
        S.op('dve', lambda e: e.tensor_scalar(out=ST[:nt, 32:40], in0=ST[:nt, 32:40], scalar1=3.0, scalar2=None,
                                              op0=ALU.mult), reads=['st'], writes=['st'])
        es = ST[:nt, 32:40].rearrange("p (b h) -> p b h", b=2)
        dn = ST[:nt, 48:56].rearrange("p (b h) -> p b h", b=2)
        S.op('dve', lambda e: e.tensor_tensor(out=dn, in0=psO_den[:nt], in1=es, op=ALU.add), reads=['psC', 'st'],
             writes=['st'])
        S.op('dve', lambda e: e.reciprocal(out=dn, in_=dn), reads=['st'], writes=['st'])
        S.op('dve', lambda e: e.tensor_tensor(out=v4(ob, nt), in0=v4(vout, nt),
                                              in1=es.unsqueeze(3).to_broadcast([nt, 2, 4, 64]), op=ALU.mult),
             reads=['vout', 'st'], writes=['ob'])
        S.op('dve', lambda e: e.tensor_tensor(out=v4(ob, nt), in0=v4(ob, nt), in1=psO_num[:nt], op=ALU.add),
             reads=['ob', 'psC'], writes=['ob'])
        S.op('dve', lambda e: e.tensor_tensor(out=v4(ob, nt), in0=v4(ob, nt),
                                              in1=dn.unsqueeze(3).to_broadcast([nt, 2, 4, 64]), op=ALU.mult),
             reads=['ob', 'st'], writes=['ob'])

    import os
    UPTO = int(os.environ.get("K_UPTO", "9"))
    if UPTO >= 1:
        for g in range(NHALO):
            do_tile(g, 'halo')
    if UPTO >= 2:
        for g in range(NHALO, NHALO + NMAIN):
            do_tile(g, 'main')
    if UPTO >= 3:
        do_tile(NHALO + NMAIN, 'sample')
    if UPTO < 4:
        DQ.flush()
        S.finish()
        return nc

    DQ.flush()
    S.barrier()
    ar.off = persist_end
    NG = 17
    ACC = ar.alloc([NG, 1024], F32)
    HNT = ar.alloc([NG, 8, 128], BF16)
    WGf = [ar.alloc([8, 256], F32) for _ in range(2)]
    WUf = [ar.alloc([8, 256], F32) for _ in range(2)]
    WDf = [ar.alloc([2, 1024], F32) for _ in range(2)]
    WGb = [ar.alloc([8, 256], BF16) for _ in range(2)]
    WUb = [ar.alloc([8, 256], BF16) for _ in range(2)]
    WDb = [ar.alloc([2, 1024], BF16) for _ in range(2)]
    SIL2 = [ar.alloc([2, 256], F32) for _ in range(2)]
    HID2 = [ar.alloc([2, 256], BF16) for _ in range(2)]
    psAB2 = [psB_flat.rearrange("p (q n) -> p q n", n=256), psC.rearrange("p b (q n) -> p (b q) n", n=256)]
    psAB = [psC[:, 0, :].rearrange("p (a b) -> p a b", b=128), psC[:, 1, :].rearrange("p (a b) -> p a b", b=128)]
    psD = [psA_flat, psB_flat]
    psDk = ['psA', 'psB']
    it = 0
    for (t0, t1) in GROUPS:
        for t in range(t0, t1):
            nt = NSAMP if t == NMAIN else 128
            j = t - t0
            S.dma(out=ACC[:nt, j, :], in_=hs[t * 128:t * 128 + nt, :], writes=[('acc', j, 0), ('acc', j, 1)])
            S.dma(out=HNT[:, j, :, :nt], in_=hnts[:, t, :, :nt], writes=[('hnt', j)])
        def prep(ex):
            wb = ex % 2
            S.dma(out=WGf[wb], in_=w_gate[ex].rearrange("(p kc) f -> p kc f", kc=8), writes=[('wgf', wb)])
            S.dma(out=WUf[wb], in_=w_up[ex].rearrange("(p kc) f -> p kc f", kc=8), writes=[('wuf', wb)])
            S.dma(out=WDf[wb], in_=w_down[ex].rearrange("(kc p) f -> p kc f", p=128), writes=[('wdf', wb)])
            S.op('dve', lambda e, wb=wb: e.tensor_tensor(out=WGb[wb], in0=WGf[wb],
                                                          in1=g2c.unsqueeze(2).to_broadcast([128, 8, 256]), op=ALU.mult),
                 reads=[('wgf', wb)], writes=[('wgb', wb)])
            S.op('dve', lambda e, wb=wb: e.tensor_tensor(out=WUb[wb], in0=WUf[wb],
                                                          in1=g2c.unsqueeze(2).to_broadcast([128, 8, 256]), op=ALU.mult),
                 reads=[('wuf', wb)], writes=[('wub', wb)])
            S.op('act', lambda e, wb=wb: e.copy(out=WDb[wb], in_=WDf[wb]), reads=[('wdf', wb)], writes=[('wdb', wb)])

        tl = list(range(t0, t1))
        batches = []
        while tl:
            if len(tl) >= 2 and tl[1] != NMAIN and tl[0] != NMAIN:
                batches.append(tl[:2]); tl = tl[2:]
            else:
                batches.append(tl[:1]); tl = tl[1:]
        units = [(ex, bt) for ex in range(32) for bt in batches]

        def gu(u):
            ex, bt = units[u]
            wb = ex % 2
            j = bt[0] - t0
            nb = len(bt)
            ncol = 128 * nb if bt[0] != NMAIN else NSAMP
            pab = psAB2[u % 2]
            if nb == 2:
                rhs_of = lambda kc: HNT[:, j:j + 2, kc, :]
                out_of = lambda q: pab[:, q, :].rearrange("p (a b) -> p a b", b=128)
            else:
                rhs_of = lambda kc: HNT[:, j, kc, :ncol]
                out_of = lambda q: pab[:, q, :ncol]

            def fgu(e):
                last = None
                for m, W in enumerate((WGb[wb], WUb[wb])):
                    for fcx in range(2):
                        for kc in range(8):
                            last = e.matmul(out_of(m * 2 + fcx), lhsT=W[:, kc, fcx * 128:(fcx + 1) * 128],
                                            rhs=rhs_of(kc), start=(kc == 0), stop=(kc == 7))
                return last
            S.op('pe', fgu, reads=[('wgb', wb), ('wub', wb)] + [('hnt', t - t0) for t in bt], writes=[('psab', u % 2)])

        prep(0)
        gu(0)
        hcnt = 0
        for u, (ex, bt) in enumerate(units):
            wb = ex % 2
            pb = u % 2
            pab = psAB2[pb]
            pabk = ('psab', pb)
            ncol = 128 * len(bt) if bt[0] != NMAIN else NSAMP
            if bt[0] == t0 and ex + 1 < 32:
                prep(ex + 1)
            if u + 1 < len(units):
                gu(u + 1)
            S.op('act', lambda e, pb=pb, ncol=ncol, pab=pab: e.activation(out=SIL2[pb][:, :, :ncol], in_=pab[:, 0:2, :ncol],
                                                                           func=AF.Silu), reads=[pabk], writes=[('sil', pb)])
            S.op('dve', lambda e, pb=pb, ncol=ncol, pab=pab: e.tensor_tensor(out=HID2[pb][:, :, :ncol],
                                                                              in0=SIL2[pb][:, :, :ncol],
                                                                              in1=pab[:, 2:4, :ncol], op=ALU.mult),
                 reads=[pabk, ('sil', pb)], writes=[('hid', pb)])
            for bi, t in enumerate(bt):
                nt = NSAMP if t == NMAIN else 128
                j = t - t0
                c0 = bi * 128
                for half in range(2):
                    hb = hcnt % 2
                    hcnt += 1
                    pdh = psA_flat[:, hb * 512:(hb + 1) * 512]
                    pdk = ('psd', hb)

                    def fdn(e, wb=wb, pb=pb, nt=nt, pdh=pdh, half=half, c0=c0):
                        last = None
                        for fcx in range(2):
                            last = e.matmul(pdh[:nt, :], lhsT=HID2[pb][:, fcx, c0:c0 + nt],
                                            rhs=WDb[wb][:, fcx, half * 512:(half + 1) * 512], start=(fcx == 0),
                                            stop=(fcx == 1))
                        return last
                    S.op('pe', fdn, reads=[('hid', pb), ('wdb', wb)], writes=[pdk])
                    S.op('dve', lambda e, j=j, nt=nt, pdh=pdh, t=t, ex=ex, half=half: e.scalar_tensor_tensor(
                        out=ACC[:nt, j, half * 512:(half + 1) * 512], in0=pdh[:nt, :], scalar=G[:nt, t, ex:ex + 1],
                        in1=ACC[:nt, j, half * 512:(half + 1) * 512], op0=ALU.mult, op1=ALU.add),
                        reads=[pdk, ('acc', j, half)], writes=[('acc', j, half)])
        for t in range(t0, t1):
            j = t - t0
            if t == NMAIN:
                S.dma(out=y_s, in_=ACC[:NSAMP, j, :], reads=[('acc', j, 0), ('acc', j, 1)])
            else:
                S.dma(out=y_p[t * 128:(t + 1) * 128, :], in_=ACC[:, j, :], reads=[('acc', j, 0), ('acc', j, 1)])
    S.finish()
    return nc


_NC_CACHE = {}


def _consts():
    ident = np.eye(128, dtype=np.float32)
    p = np.arange(128)
    bones = (p[:, None] // 64 == p[None, :] // 64).astype(np.float32)
    tri = (p[:, None] <= p[None, :]).astype(np.float32)
    mult = np.zeros((128, NOFF, 128), np.float32)
    for o in range(NOFF):
        delta = 128 * o + p[None, :] - p[:, None]
        m = ((delta >= 0) & (delta <= 128)).astype(np.float32)
        m += ((delta >= 0) & (delta <= 512) & (delta % 4 == 0)).astype(np.float32)
        m += ((delta >= 0) & (delta <= 2048) & (delta % 16 == 0)).astype(np.float32)
        mult[:, o, :] = m
    sel = np.zeros((16, 16, 128), np.float32)
    for b in range(16):
        sel[b, b, :] = 1.0
    return ident, bones, tri, mult, sel


def kernel(x_prompt, x_sample, cache_k, cache_v, norm1_g, w_in, q_gain, k_gain, v_gain,
           w_spatial, b_spatial, out_gain_a, out_gain_b, w_out, norm2_g, w_router1, b_router1,
           w_router2, b_router2, w_up, w_gate, w_down):
    f = lambda a: np.ascontiguousarray(np.asarray(a, dtype=np.float32))
    x_prompt, x_sample = f(x_prompt), f(x_sample)
    cache_k, cache_v = np.asarray(cache_k, dtype=np.float32), np.asarray(cache_v, dtype=np.float32)
    if 'nc' not in _NC_CACHE:
        _NC_CACHE['nc'] = build_nc()
    nc = _NC_CACHE['nc']
    ident, bones, tri, mult, sel = _consts()
    col = lambda v: f(np.asarray(v).reshape(8, 128).T)
    g1c, g2c = col(norm1_g[0]), f(np.asarray(norm2_g[0]).reshape(128, 8))
    ogc = col(np.concatenate([np.asarray(out_gain_a[0]), np.asarray(out_gain_b[0])]))
    qg = np.asarray(q_gain[0]).reshape(4, 128).T
    kg = np.asarray(k_gain[0]).reshape(4, 128).T
    gqk = f(np.concatenate([qg, kg], axis=1))
    vgain = f(np.broadcast_to(np.asarray(v_gain[0]).reshape(1, 512), (128, 512)))
    bst = f(np.asarray(b_spatial[0]).T)
    ws00 = f(np.broadcast_to(np.asarray(w_spatial[0])[:, 0, 0].reshape(1, 8), (128, 8)))
    bs0 = f(np.broadcast_to(np.asarray(b_spatial[0])[:, 0].reshape(1, 8), (128, 8)))
    br = f(np.broadcast_to(np.concatenate([np.asarray(b_router1[0]).reshape(4), np.asarray(b_router2[0]).reshape(32)]
                                          ).reshape(1, 36), (128, 36)))
    wsT = f(np.transpose(np.asarray(w_spatial[0]), (2, 0, 1)))
    w_r = f(np.concatenate([np.asarray(w_router1[0]),
                            np.transpose(np.asarray(w_router2[0]), (1, 0, 2)).reshape(D, 32)], axis=1))
    wg = f(np.asarray(w_gate[0]).reshape(32, D, 256))
    wu = f(np.asarray(w_up[0]).reshape(32, D, 256))
    wd = f(np.asarray(w_down[0]).reshape(32, 256, D))
    w_in0, w_out0 = f(w_in[0]), f(w_out[0])
    in_maps = []
    for c in range(NCORES):
        b, hf = c // 2, c % 2
        xp = np.zeros((48 * 128, D), np.float32)
        if hf == 1:
            xp[:2048] = x_prompt[b, 2048:4096]
        xp[2048:] = x_prompt[b, hf * 4096:(hf + 1) * 4096]
        in_maps.append({
            "xp": xp, "xs": f(x_sample[16 * c:16 * c + 16, 0]),
            "ck": f(cache_k[0, 16 * c:16 * c + 16].reshape(16, 2048, 512)),
            "cv": f(cache_v[0, 16 * c:16 * c + 16].reshape(16, 2048, 512)),
            "w_in": w_in0, "w_out": w_out0, "w_r": w_r, "w_gate": wg, "w_up": wu, "w_down": wd, "wsT": wsT,
            "g1c": g1c, "g2c": g2c, "ogc": ogc, "gqk": gqk, "vgain": vgain, "bst": bst, "ws00": ws00, "bs0": bs0,
            "br": br, "flag": np.full((128, 1), float(hf), np.float32),
            "c_ident": ident, "c_bones": bones, "c_tri": tri, "c_mult": mult,
        })
    res = run_bass_kernel_spmd(nc, in_maps, core_ids=list(range(NCORES)))
    R = res.results
    B, T = 4, 8192
    y_prompt = np.zeros((B, T, D), np.float32)
    y_sample = np.zeros((128, 1, D), np.float32)
    nkp = np.zeros((1, B, 2048, 8, 64), np.float32)
    nvp = np.zeros((1, B, 2048, 8, 64), np.float32)
    nks = np.zeros((1, 128, 1, 8, 64), np.float32)
    nvs = np.zeros((1, 128, 1, 8, 64), np.float32)
    nva = np.zeros((1, 128, 1, 8, 64), np.float32)
    for c in range(NCORES):
        b, hf = c // 2, c % 2
        r = R[c]
        y_prompt[b, hf * 4096:(hf + 1) * 4096] = r["y_p"]
        y_sample[16 * c:16 * c + 16, 0] = r["y_s"]
        if hf == 1:
            nkp[0, b] = r["nk"].reshape(2048, 8, 64)
            nvp[0, b] = r["nv"].reshape(2048, 8, 64)
        nks[0, 16 * c:16 * c + 16, 0] = r["nks"].reshape(16, 8, 64)
        nvs[0, 16 * c:16 * c + 16, 0] = r["nvs"].reshape(16, 8, 64)
        nva[0, 16 * c:16 * c + 16, 0] = r["nva"].reshape(16, 8, 64)
    return (y_prompt, y_sample, nkp, nvp, nks, nvs, nva)
```

```python
import numpy as np
import ml_dtypes
import concourse.bass as bass
import concourse.mybir as mybir
from concourse.bass_utils import run_bass_kernel_spmd

F32 = mybir.dt.float32
BF16 = mybir.dt.bfloat16
U8 = mybir.dt.uint8
ALU = mybir.AluOpType
AF = mybir.ActivationFunctionType
AX = mybir.AxisListType

D = 1024
NCORES = 8
NHALO = 16
NMAIN = 32
RING = 20
NOFF = 17
NSAMP = 16
NTILES = NMAIN + 1
EPS = 1e-6
NDS = 24
GROUPS = [(0, 17), (17, 33)]


class Sched:
    def __init__(self, nc):
        self.nc = nc
        self.ce = ['pe', 'act', 'dve', 'pool']
        self.prog = {e: [] for e in self.ce + ['sp']}
        self.sem = {e: nc.alloc_semaphore("s_" + e) for e in self.ce}
        self.cnt = {e: 0 for e in self.ce}
        self.dsem = [nc.alloc_semaphore("d%d" % i) for i in range(NDS)]
        self.dcnt = [0] * NDS
        self.dnext = 0
        self.waited = {}
        self.lastw = {}
        self.readers = {}

    def _semh(self, sk):
        return self.sem[sk[1]] if sk[0] == 'e' else self.dsem[sk[1]]

    def _deps(self, e, reads, writes):
        need = {}

        def add(tok):
            if tok is None:
                return
            sk, v, te = tok
            if te == e and e == 'pe':
                return
            if v > need.get(sk, 0):
                need[sk] = v
        for k in reads:
            add(self.lastw.get(k))
        for k in writes:
            add(self.lastw.get(k))
            for sk, (v, te) in self.readers.get(k, {}).items():
                add((sk, v, te))
        out = []
        for sk, v in need.items():
            if self.waited.get((e, sk), 0) >= v:
                continue
            self.waited[(e, sk)] = v
            out.append((self._semh(sk), v))
        return out

    def _commit(self, tok, reads, writes):
        sk, v, te = tok
        for k in reads:
            self.readers.setdefault(k, {})[sk] = (v, te)
        for k in writes:
            self.lastw[k] = tok
            self.readers[k] = {}

    def op(self, e, fn, reads=(), writes=()):
        waits = self._deps(e, reads, writes)
        self.cnt[e] += 1
        tok = (('e', e), self.cnt[e], e)
        sem = self.sem[e]

        def emit(eng):
            for s, v in waits:
                eng.wait_ge(s, v)
            fn(eng).then_inc(sem, 1)
        self.prog[e].append(emit)
        self._commit(tok, reads, writes)

    def dma(self, out, in_, reads=(), writes=(), q='sp'):
        waits = self._deps(q, reads, writes)
        i = self.dnext
        self.dnext = (i + 1) % NDS
        prev = self.dcnt[i]
        if prev > 0 and self.waited.get((q, ('d', i)), 0) < prev:
            waits.append((self.dsem[i], prev))
            self.waited[(q, ('d', i))] = prev
        self.dcnt[i] += 16
        tok = (('d', i), self.dcnt[i], 'dma')
        sem = self.dsem[i]

        def emit(eng):
            for s, v in waits:
                eng.wait_ge(s, v)
            eng.dma_start(out=out, in_=in_).then_inc(sem, 16)
        self.prog[q].append(emit)
        self._commit(tok, reads, writes)

    def barrier(self):
        allw = [(('e', e), self.cnt[e]) for e in self.ce if self.cnt[e] > 0]
        allw += [(('d', i), self.dcnt[i]) for i in range(NDS) if self.dcnt[i] > 0]
        for e in self.ce + ['sp']:
            waits = []
            for sk, v in allw:
                if sk == ('e', e) and e == 'pe':
                    continue
                if self.waited.get((e, sk), 0) >= v:
                    continue
                self.waited[(e, sk)] = v
                waits.append((self._semh(sk), v))

            def emit(eng, waits=waits):
                for s, v in waits:
                    eng.wait_ge(s, v)
            self.prog[e].append(emit)
        self.lastw = {}
        self.readers = {}

    def finish(self):
        self.barrier()
        nc = self.nc
        prog = self.prog
        with nc.Block() as block:
            @block.sync
            def _(eng):
                for f in prog['sp']:
                    f(eng)

            @block.tensor
            def _(eng):
                for f in prog['pe']:
                    f(eng)

            @block.scalar
            def _(eng):
                for f in prog['act']:
                    f(eng)

            @block.vector
            def _(eng):
                for f in prog['dve']:
                    f(eng)

            @block.gpsimd
            def _(eng):
                for f in prog['pool']:
                    f(eng)


class Arena:
    def __init__(self, nc, nbytes):
        self.ap = nc.alloc_sbuf_tensor("arena", [128, nbytes], U8).ap()
        self.off = 0
        self.nbytes = nbytes

    def alloc(self, free_shape, dtype):
        esz = 4 if dtype == F32 else 2
        n = int(np.prod(free_shape))
        nb = (n * esz + 31) // 32 * 32
        assert self.off + nb <= self.nbytes, ("arena overflow", self.off, nb)
        v = self.ap[:, self.off:self.off + n * esz].bitcast(dtype)
        self.off += nb
        if len(free_shape) == 2:
            v = v.rearrange("p (a b) -> p a b", b=free_shape[1])
        elif len(free_shape) == 3:
            v = v.rearrange("p (a b c) -> p a b c", b=free_shape[1], c=free_shape[2])
        return v


def build_nc(nhalo=16, nmain=32, groups=((0, 17), (17, 33))):
    global NHALO, NMAIN, NTILES, GROUPS
    NHALO, NMAIN, NTILES, GROUPS = nhalo, nmain, nmain + 1, list(groups)
    nc = bass.Bass("TRN2", target_bir_lowering=False)
    S = Sched(nc)

    def din(name, shape, dt=F32):
        return nc.dram_tensor(name, list(shape), dt, kind="ExternalInput").ap()

    def dout(name, shape, dt=F32):
        return nc.dram_tensor(name, list(shape), dt, kind="ExternalOutput").ap()

    xp = din("xp", [(NHALO + NMAIN) * 128, D])
    xs = din("xs", [NSAMP, D])
    ck = din("ck", [NSAMP, 2048, 512])
    cv = din("cv", [NSAMP, 2048, 512])
    w_in = din("w_in", [D, 2560])
    w_out = din("w_out", [D, D])
    w_r = din("w_r", [D, 36])
    w_gate = din("w_gate", [32, D, 256])
    w_up = din("w_up", [32, D, 256])
    w_down = din("w_down", [32, 256, D])
    wsT_d = din("wsT", [128, 8, 128])
    g1c_d = din("g1c", [128, 8])
    g2c_d = din("g2c", [128, 8])
    ogc_d = din("ogc", [128, 8])
    gqk_d = din("gqk", [128, 8])
    vgain_d = din("vgain", [128, 512])
    bst_d = din("bst", [128, 8])
    ws00_d = din("ws00", [128, 8])
    bs0_d = din("bs0", [128, 8])
    br_d = din("br", [128, 36])
    flag_d = din("flag", [128, 1])
    c_ident = din("c_ident", [128, 128])
    c_bones = din("c_bones", [128, 128])
    c_tri = din("c_tri", [128, 128])
    c_mult = din("c_mult", [128, NOFF, 128])

    y_p = dout("y_p", [NMAIN * 128, D])
    y_s = dout("y_s", [NSAMP, D])
    nk_o = dout("nk", [2048, 512])
    nv_o = dout("nv", [2048, 512])
    nks_o = dout("nks", [NSAMP, 512])
    nvs_o = dout("nvs", [NSAMP, 512])
    nva_o = dout("nva", [NSAMP, 512])

    hs = nc.dram_tensor("hs", [NTILES * 128, D], F32, kind="Internal").ap()
    hnts = nc.dram_tensor("hnts", [128, NTILES, 8, 128], BF16, kind="Internal").ap()

    ar = Arena(nc, 200 * 1024)
    G = ar.alloc([NTILES, 32], F32)
    g2c = ar.alloc([8], F32)
    persist_end = ar.off

    psA = nc.alloc_psum_tensor("psA", [128, 8, 128], F32).ap()
    psB = nc.alloc_psum_tensor("psB", [128, 8, 128], F32).ap()
    psC = nc.alloc_psum_tensor("psC", [128, 2, 512], F32).ap()
    psT = nc.alloc_psum_tensor("psT", [128, 8, 128], BF16).ap()
    psE = nc.alloc_psum_tensor("psE", [128, 512], F32).ap()
    psA_flat = psA.rearrange("p a b -> p (a b)")
    psB_flat = psB.rearrange("p a b -> p (a b)")

    WIN = ar.alloc([8, 2560], BF16)
    WOUT = ar.alloc([8, 1024], BF16)
    WR = ar.alloc([8, 36], BF16)
    WST = ar.alloc([8, 128], BF16)
    MULT = ar.alloc([NOFF, 128], BF16)
    MULTH = ar.alloc([NOFF, 128], BF16)
    identf = ar.alloc([128], F32)
    identb = ar.alloc([128], BF16)
    bonesb = ar.alloc([128], BF16)
    ONES16 = ar.alloc([128], F32)
    g1c = ar.alloc([8], F32)
    ogc = ar.alloc([8], F32)
    GQK = ar.alloc([8], F32)
    VGAIN = ar.alloc([512], F32)
    BST = ar.alloc([8], F32)
    WS00 = ar.alloc([8], F32)
    BS0 = ar.alloc([8], F32)
    BR = ar.alloc([36], F32)
    FLAG = ar.alloc([1], F32)
    EPSC = ar.alloc([1], F32)
    stg_off = ar.off
    STG = [ar.alloc([2560], F32) for _ in range(2)]

    def small_load(dst, src, key):
        S.dma(out=dst, in_=src, writes=[key])

    small_load(g1c, g1c_d, 'g1c')
    small_load(g2c, g2c_d, 'g2c')
    small_load(ogc, ogc_d, 'ogc')
    small_load(GQK, gqk_d, 'gqk')
    small_load(VGAIN, vgain_d, 'vgain')
    small_load(BST, bst_d, 'bst')
    small_load(WS00, ws00_d, 'ws00')
    small_load(BS0, bs0_d, 'bs0')
    small_load(BR, br_d, 'br')
    small_load(FLAG, flag_d, 'flag')
    small_load(identf, c_ident, 'identf')
    S.op('dve', lambda e: e.memset(ONES16, 1.0), writes=['ones16'])
    S.op('dve', lambda e: e.memset(EPSC, EPS), writes=['epsc'])
    S.op('dve', lambda e: e.memset(VA[:, :, :, 64:65], 1.0), writes=['va_ones'])
    for i in range(6):
        S.op('pool', lambda e, i=i: e.memset(VAS[i][:, :, 64:65], 1.0), writes=[('vas', i)])
    S.op('dve', lambda e: e.tensor_copy(out=identb, in_=identf), reads=['identf'], writes=['identb'])
    S.dma(out=STG[0][:, 0:128], in_=c_bones, writes=[('stg', 0)])
    S.op('dve', lambda e: e.tensor_copy(out=bonesb, in_=STG[0][:, 0:128]), reads=[('stg', 0)], writes=['bonesb'])
    S.dma(out=STG[1][:, 0:NOFF * 128], in_=c_mult.rearrange("p a b -> p (a b)"), writes=[('stg', 1)])
    S.op('dve', lambda e: e.tensor_copy(out=MULT.rearrange("p a b -> p (a b)"), in_=STG[1][:, 0:NOFF * 128]),
         reads=[('stg', 1)], writes=['mult'])
    S.op('dve', lambda e: e.tensor_scalar(out=MULTH.rearrange("p a b -> p (a b)"), in0=STG[1][:, 0:NOFF * 128],
                                          scalar1=FLAG[:, 0:1], scalar2=None, op0=ALU.mult),
         reads=[('stg', 1), 'flag'], writes=['multh'])
    S.dma(out=STG[0][:, 0:1024], in_=wsT_d.rearrange("p a b -> p (a b)"), reads=['bonesb'], writes=[('stg', 0)])
    S.dma(out=STG[0][:, 1024:1152], in_=c_tri, writes=[('stg', 0)])
    S.op('dve', lambda e: e.tensor_tensor(
        out=WST, in0=STG[0][:, 0:1024].rearrange("p (a b) -> p a b", b=128),
        in1=STG[0][:, 1024:1152].unsqueeze(1).to_broadcast([128, 8, 128]), op=ALU.mult),
        reads=[('stg', 0)], writes=['wst'])
    S.dma(out=STG[1][:, 0:288].rearrange("p (a b) -> p a b", b=36), in_=w_r.rearrange("(p kc) f -> p kc f", kc=8),
          reads=['mult', 'multh'], writes=[('stg', 1)])
    S.op('dve', lambda e: e.tensor_tensor(
        out=WR, in0=STG[1][:, 0:288].rearrange("p (a b) -> p a b", b=36),
        in1=g2c.unsqueeze(2).to_broadcast([128, 8, 36]), op=ALU.mult),
        reads=[('stg', 1), 'g2c'], writes=['wr'])
    for kc in range(8):
        st = STG[kc % 2]
        S.dma(out=st[:, 0:2560], in_=w_in[kc * 128:(kc + 1) * 128, :], reads=['wst', 'wr'], writes=[('stg', kc % 2)])
        eng = 'dve' if kc % 2 == 0 else 'pool'
        S.op(eng, lambda e, st=st, kc=kc: e.tensor_scalar(out=WIN[:, kc, :], in0=st[:, 0:2560], scalar1=g1c[:, kc:kc + 1],
                                                           scalar2=None, op0=ALU.mult),
             reads=[('stg', kc % 2), 'g1c'], writes=['win'])
    for kc in range(8):
        st = STG[kc % 2]
        S.dma(out=st[:, 0:1024], in_=w_out[kc * 128:(kc + 1) * 128, :], writes=[('stg', kc % 2)])
        eng = 'dve' if kc % 2 == 0 else 'pool'
        S.op(eng, lambda e, st=st, kc=kc: e.tensor_scalar(out=WOUT[:, kc, :], in0=st[:, 0:1024], scalar1=ogc[:, kc:kc + 1],
                                                           scalar2=None, op0=ALU.mult),
             reads=[('stg', kc % 2), 'ogc'], writes=['wout'])

    S.barrier()
    ar.off = stg_off
    XT = [ar.alloc([1024], F32) for _ in range(2)]
    xn = ar.alloc([1024], BF16)
    xnT = ar.alloc([8, 128], BF16)
    sq = ar.alloc([8, 128], BF16)
    rr = ar.alloc([8, 128], F32)
    qkn = ar.alloc([8, 128], F32)
    QT = ar.alloc([4, 2, 128], BF16)
    KT = ar.alloc([RING, 4, 128], BF16)
    VA = ar.alloc([RING, 8, 65], BF16)
    kout = ar.alloc([1024], F32)
    vout = ar.alloc([512], F32)
    u_t = ar.alloc([512], F32)
    gg = ar.alloc([512], F32)
    gsq = ar.alloc([512], F32)
    vg = ar.alloc([512], F32)
    vgb = ar.alloc([512], BF16)
    oa = ar.alloc([512], F32)
    ob = ar.alloc([512], F32)
    oab = ar.alloc([1024], BF16)
    oT = ar.alloc([8, 128], BF16)
    P = [ar.alloc([8, 128], BF16) for _ in range(2)]
    hh = ar.alloc([1024], F32)
    hn = ar.alloc([1024], BF16)
    hnT = ar.alloc([8, 128], BF16)
    junk = ar.alloc([1024], BF16)
    ST = ar.alloc([64], F32)
    RL = ar.alloc([96], F32)
    KC = [ar.alloc([512], F32) for _ in range(2)]
    VC = [ar.alloc([512], F32) for _ in range(2)]
    prod = ar.alloc([512], F32)
    SALL = ar.alloc([24], F32)
    VAS = [ar.alloc([8, 65], BF16) for _ in range(6)]
    PZ = [ar.alloc([24, 16], BF16) for _ in range(2)]
    stage1_end = ar.off
    print('stage1 arena bytes', stage1_end)

    class Defer:
        def __init__(self):
            self.q = []

        def op(self, *a, **k):
            self.q.append(lambda: S.op(*a, **k))

        def dma(self, *a, **k):
            self.q.append(lambda: S.dma(*a, **k))

        def pop(self, n):
            for _ in range(n):
                if self.q:
                    self.q.pop(0)()

        def flush(self):
            self.pop(len(self.q))

    DQ = Defer()

    def rstd(nt, src, dst, scale, tmpkey, Q=None):
        Q = Q or S
        Q.op('act', lambda e: e.activation(out=dst, in_=src, func=AF.Sqrt, bias=EPSC[:nt, 0:1], scale=scale),
             reads=['st'], writes=['st'])
        Q.op('dve', lambda e: e.reciprocal(out=dst, in_=dst), reads=['st'], writes=['st'])

    def transposes_bf(nt, src, dstT, rkey, wkey, interleaved=False, Q=None):
        def f(e):
            last = None
            for kc in range(8):
                cols = src[:nt, kc:1024:8] if interleaved else src[:nt, kc * 128:(kc + 1) * 128]
                last = e.transpose(out=psT[:, kc, :nt], in_=cols, identity=identb[:nt, :nt])
            return last
        Q = Q or S
        Q.op('pe', f, reads=[rkey], writes=['psT'])
        Q.op('act', lambda e: e.copy(out=dstT[:, :, :nt], in_=psT[:, :, :nt]), reads=['psT'], writes=[wkey])

    def v3(ap, nt):
        return ap[:nt].rearrange("p (h d) -> p h d", d=64)

    def v4(ap, nt):
        return ap[:nt].rearrange("p (b h d) -> p b h d", b=2, d=64)

    psO = psC
    psO_num = psC[:, :, 0:260].rearrange("p b (h e) -> p b h e", e=65)[:, :, :, 0:64]
    psO_den = psC[:, :, 64:260:65]

    def load_x(g):
        if g == NHALO + NMAIN:
            S.dma(out=XT[g % 2][:NSAMP], in_=xs, writes=[('xt', g % 2)])
        else:
            S.dma(out=XT[g % 2], in_=xp[g * 128:(g + 1) * 128, :], writes=[('xt', g % 2)])

    import os
    CUT = int(os.environ.get('K_CUT', '99'))
    CUT2 = int(os.environ.get('K_CUT2', '99'))
    CUT3 = int(os.environ.get('K_CUT3', '99'))

    done12 = set()

    def t12(g, Q):
        nt = NSAMP if g == NHALO + NMAIN else 128
        xt = XT[g % 2]
        xtk = ('xt', g % 2)
        Q.op('act', lambda e: e.activation(out=junk[:nt], in_=xt[:nt], func=AF.Square, accum_out=ST[:nt, 0:1]),
             reads=[xtk], writes=['junk', 'st'])
        rstd(nt, ST[:nt, 0:1], ST[:nt, 1:2], 1.0 / D, 'st', Q)
        Q.op('dve', lambda e: e.tensor_scalar(out=xn[:nt], in0=xt[:nt], scalar1=ST[:nt, 1:2], scalar2=None, op0=ALU.mult),
             reads=[xtk, 'st'], writes=['xn'])
        transposes_bf(nt, xn, xnT, 'xn', 'xnT', Q=Q)
        done12.add(g)

    def do_tile(g, kind):
        nt = NSAMP if kind == 'sample' else 128
        xt = XT[g % 2]
        xtk = ('xt', g % 2)
        mt = g - NHALO if kind != 'sample' else NMAIN
        if g == 0:
            load_x(0)
        if g + 1 <= NHALO + NMAIN:
            load_x(g + 1)
        if g not in done12:
            t12(g, S)
        fcs = list(range(4, 8)) if kind == 'halo' else list(range(8))
        f0, f1 = fcs[0], fcs[-1] + 1
        nf = f1 - f0

        def fqk(e):
            last = None
            for fc in fcs:
                for kc in range(8):
                    last = e.matmul(psA[:, fc, :nt], lhsT=WIN[:, kc, fc * 128:(fc + 1) * 128], rhs=xnT[:, kc, :nt],
                                    start=(kc == 0), stop=(kc == 7))
            return last
        S.op('pe', fqk, reads=['xnT', 'win'], writes=['psA'])

        def fv(e):
            last = None
            dsts = [(psE, 1024)] if kind == 'halo' else [(psE, 1024), (psC[:, 0, :], 1536), (psC[:, 1, :], 2048)]
            for dst, c0 in dsts:
                for kc in range(8):
                    last = e.matmul(dst[:nt, 0:512], lhsT=xnT[:, kc, :nt], rhs=WIN[:, kc, c0:c0 + 512],
                                    start=(kc == 0), stop=(kc == 7))
            return last
        S.op('pe', fv, reads=['xnT', 'win'], writes=['psE'] if kind == 'halo' else ['psE', 'psC'])
        S.op('act', lambda e: e.activation(out=sq[:, f0:f1, :nt], in_=psA[:, f0:f1, :nt], func=AF.Square),
             reads=['psA'], writes=['sq'])

        def fss(e):
            last = None
            for fc in fcs:
                last = e.matmul(psB[:, fc, :nt], lhsT=bonesb, rhs=sq[:, fc, :nt], start=True, stop=True)
            return last
        S.op('pe', fss, reads=['sq', 'bonesb'], writes=['psB'])
        S.op('act', lambda e: e.activation(out=rr[:, f0:f1, :nt], in_=psB[:, f0:f1, :nt], func=AF.Sqrt, bias=EPSC[:, 0:1],
                                           scale=1.0 / 64), reads=['psB'], writes=['rr'])
        S.op('dve', lambda e: e.reciprocal(out=rr[:, f0:f1, :nt], in_=rr[:, f0:f1, :nt]), reads=['rr'], writes=['rr'])
        S.op('dve', lambda e: e.tensor_tensor(out=qkn[:, f0:f1, :nt], in0=psA[:, f0:f1, :nt],
                                              in1=GQK[:, f0:f1].unsqueeze(2).to_broadcast([128, nf, nt]), op=ALU.mult),
             reads=['psA', 'gqk'], writes=['qkn'])
        S.op('dve', lambda e: e.tensor_tensor(out=qkn[:, f0:f1, :nt], in0=qkn[:, f0:f1, :nt], in1=rr[:, f0:f1, :nt],
                                              op=ALU.mult), reads=['qkn', 'rr'], writes=['qkn'])
        slot = g % RING
        if kind != 'sample':
            S.op('dve', lambda e: e.tensor_copy(out=KT[:, slot, :, :], in_=qkn[:, 4:8, :]), reads=['qkn'],
                 writes=[('kt', slot)])
        if kind == 'main':
            if g == NHALO:
                S.op('dve', lambda e: e.memset(QT, 0.0), writes=['qt'])
            S.op('act', lambda e: e.copy(out=QT[0:64, :, 0, :], in_=qkn[0:64, 0:4, :]), reads=['qkn'], writes=['qt'])
            S.op('act', lambda e: e.copy(out=QT[64:128, :, 1, :], in_=qkn[64:128, 0:4, :]), reads=['qkn'],
                 writes=['qt'])
        if kind == 'main' and CUT <= 1:
            return
        KO0 = NHALO + max(NMAIN - 16, 0)
        want_kout = (kind == 'sample') or (kind == 'main' and g >= KO0)
        if want_kout:
            tf = list(range(8)) if kind == 'sample' else list(range(4, 8))

            def ftr(e):
                last = None
                for fc in tf:
                    last = e.transpose(out=psB_flat[:nt, fc * 128:(fc + 1) * 128], in_=qkn[:, fc, :nt], identity=identf)
                return last
            S.op('pe', ftr, reads=['qkn', 'identf'], writes=['psB'])
            t0 = tf[0] * 128
            S.op('dve', lambda e: e.tensor_copy(out=kout[:nt, t0:1024], in_=psB_flat[:nt, t0:1024]), reads=['psB'],
                 writes=['kout'])
            if kind == 'sample':
                S.dma(out=nks_o, in_=kout[:nt, 512:1024], reads=['kout'])
            else:
                r0 = (g - KO0) * 128
                S.dma(out=nk_o[r0:r0 + 128, :], in_=kout[:, 512:1024], reads=['kout'])
        if kind == 'main' and CUT <= 2:
            return
        if kind != 'sample':
            S.op('act', lambda e: e.copy(out=VA[:nt, slot, :, 0:64], in_=v3(psE, nt)), reads=['psE', 'va_ones'],
                 writes=[('va', slot)])
        if want_kout:
            S.op('dve', lambda e: e.tensor_copy(out=vout[:nt], in_=psE[:nt, 0:512]), reads=['psE'], writes=['vout'])
            if kind == 'sample':
                S.dma(out=nvs_o, in_=vout[:nt], reads=['vout'])
            else:
                r0 = (g - KO0) * 128
                S.dma(out=nv_o[r0:r0 + 128, :], in_=vout, reads=['vout'])
        if kind == 'main' and CUT <= 3:
            return
        if kind == 'halo':
            return
        S.op('act', lambda e: e.activation(out=u_t[:nt], in_=psC[:nt, 0, :], func=AF.Gelu_apprx_tanh), reads=['psC'],
             writes=['u'])
        S.op('act', lambda e: e.activation(out=gg[:nt], in_=psC[:nt, 1, :], func=AF.Gelu_apprx_tanh), reads=['psC'],
             writes=['gg'])
        Q = DQ if kind == 'main' else S
        if kind == 'sample':
            DQ.flush()
        Q.op('dve', lambda e: e.tensor_tensor(out=gsq[:nt], in0=gg[:nt], in1=gg[:nt], op=ALU.mult), reads=['gg'],
             writes=['gsq'])
        Q.op('dve', lambda e: e.tensor_reduce(out=ST[:nt, 8:16], in_=v3(gsq, nt), axis=AX.X, op=ALU.add), reads=['gsq'],
             writes=['st'])
        rstd(nt, ST[:nt, 8:16], ST[:nt, 16:24], 1.0 / 64, 'st', Q)
        Q.op('dve', lambda e: e.tensor_tensor(out=v3(vg, nt), in0=v3(gg, nt),
                                              in1=ST[:nt, 16:24].unsqueeze(2).to_broadcast([nt, 8, 64]), op=ALU.mult),
             reads=['gg', 'st'], writes=['vg'])
        Q.op('dve', lambda e: e.tensor_tensor(out=vg[:nt], in0=vg[:nt], in1=VGAIN[:nt], op=ALU.mult),
             reads=['vg', 'vgain'], writes=['vg'])
        if kind == 'sample':
            Q.dma(out=nva_o, in_=vg[:nt], reads=['vg'])
            Q.op('dve', lambda e: e.tensor_tensor(out=v3(oa, nt), in0=v3(vg, nt),
                                                  in1=WS00[:nt].unsqueeze(2).to_broadcast([nt, 8, 64]), op=ALU.mult),
                 reads=['vg', 'ws00'], writes=['oa'])
            Q.op('dve', lambda e: e.tensor_tensor(out=v3(oa, nt), in0=v3(oa, nt),
                                                  in1=BS0[:nt].unsqueeze(2).to_broadcast([nt, 8, 64]), op=ALU.add),
                 reads=['oa', 'bs0'], writes=['oa'])
        else:
            Q.op('act', lambda e: e.copy(out=vgb, in_=vg), reads=['vg'], writes=['vgb'])

            def fmix(e):
                last = None
                for h in range(8):
                    last = e.matmul(psE[:, h * 64:(h + 1) * 64], lhsT=WST[:, h, :], rhs=vgb[:, h * 64:(h + 1) * 64],
                                    start=True, stop=True)
                return last
            Q.op('pe', fmix, reads=['vgb', 'wst'], writes=['psE'])
            Q.op('dve', lambda e: e.tensor_tensor(out=v3(oa, nt), in0=v3(psE, nt),
                                                  in1=BST.unsqueeze(2).to_broadcast([128, 8, 64]), op=ALU.add),
                 reads=['psE', 'bst'], writes=['oa'])
        Q.op('dve', lambda e: e.tensor_tensor(out=oa[:nt], in0=oa[:nt], in1=u_t[:nt], op=ALU.mult), reads=['oa', 'u'],
             writes=['oa'])
        Q.op('act', lambda e: e.activation(out=junk[:nt, 0:512], in_=oa[:nt], func=AF.Square, accum_out=ST[:nt, 24:25]),
             reads=['oa'], writes=['junk', 'st'])
        rstd(nt, ST[:nt, 24:25], ST[:nt, 25:26], 1.0 / 512, 'st', Q)
        Q.op('dve', lambda e: e.tensor_scalar(out=oab[:nt, 0:512], in0=oa[:nt], scalar1=ST[:nt, 25:26], scalar2=None,
                                              op0=ALU.mult), reads=['oa', 'st'], writes=['oab'])
        if kind == 'main' and CUT <= 4:
            return
        if kind == 'main':
            def s_op(i):
                o = NOFF - 1 - i
                kt = g - o
                sl = kt % RING
                ps = psA if i % 2 == 0 else psB
                pk = 'psA' if i % 2 == 0 else 'psB'

                def f(e):
                    last = None
                    for hp in range(4):
                        last = e.matmul(ps[:, 2 * hp:2 * hp + 2, :], lhsT=KT[:, sl, hp, :], rhs=QT[:, hp, :, :],
                                        start=True, stop=True)
                    return last
                S.op('pe', f, reads=[('kt', sl), 'qt'], writes=[pk])
            if g + 1 <= NHALO + NMAIN:
                t12(g + 1, DQ)
            s_op(0)
            for i in range(NOFF):
                o = NOFF - 1 - i
                kt = g - o
                sl = kt % RING
                ps = psA if i % 2 == 0 else psB
                pk = 'psA' if i % 2 == 0 else 'psB'
                Pi = P[i % 2]
                pik = ('P', i % 2)
                if i + 1 < NOFF:
                    s_op(i + 1)
                if CUT3 < 2:
                    continue
                S.op('act', lambda e, ps=ps, Pi=Pi: e.activation(out=Pi, in_=ps, func=AF.Exp, scale=0.125), reads=[pk],
                     writes=[pik])
                if CUT3 < 3:
                    continue
                M = MULTH if kt < NHALO else MULT
                S.op('dve', lambda e, Pi=Pi, M=M, o=o: e.tensor_tensor(
                    out=Pi, in0=Pi, in1=M[:, o, :].unsqueeze(1).to_broadcast([128, 8, 128]), op=ALU.mult),
                    reads=[pik, 'mult', 'multh'], writes=[pik])

                if CUT3 < 4:
                    continue

                def fpv(e, Pi=Pi, sl=sl, i=i):
                    last = None
                    for h in range(8):
                        last = e.matmul(psO[:, h // 4, (h % 4) * 65:(h % 4) * 65 + 65], lhsT=Pi[:, h, :],
                                        rhs=VA[:, sl, h, :], start=(i == 0 and h % 4 == 0), stop=(i == NOFF - 1 and h % 4 == 3),
                                        skip_group_check=True)
                    return last
                S.op('pe', fpv, reads=[pik, ('va', sl)], writes=['psC'])
                DQ.pop(-(-len(DQ.q) // (NOFF - i)))
            DQ.flush()
            if CUT3 < 5:
                return
            S.op('dve', lambda e: e.reciprocal(out=ST[:, 32:40].rearrange("p (b h) -> p b h", b=2), in_=psO_den),
                 reads=['psC'], writes=['st'])
            if CUT3 < 6:
                return
            S.op('dve', lambda e: e.tensor_tensor(
                out=v4(ob, nt), in0=psO_num,
                in1=ST[:, 32:40].rearrange("p (b h) -> p b h", b=2).unsqueeze(3).to_broadcast([128, 2, 4, 64]),
                op=ALU.mult), reads=['psC', 'st'], writes=['ob'])
        else:
            sample_attention()
        if kind == 'main' and CUT <= 5:
            return
        S.op('act', lambda e: e.activation(out=junk[:nt, 0:512], in_=ob[:nt], func=AF.Square, accum_out=ST[:nt, 26:27]),
             reads=['ob'], writes=['junk', 'st'])
        rstd(nt, ST[:nt, 26:27], ST[:nt, 27:28], 1.0 / 512, 'st')
        S.op('dve', lambda e: e.tensor_scalar(out=oab[:nt, 512:1024], in0=ob[:nt], scalar1=ST[:nt, 27:28], scalar2=None,
                                              op0=ALU.mult), reads=['ob', 'st'], writes=['oab'])
        transposes_bf(nt, oab, oT, 'oab', 'oT')

        def fwo(e):
            last = None
            for half in range(2):
                for kc in range(8):
                    last = e.matmul(psA_flat[:nt, half * 512:(half + 1) * 512], lhsT=oT[:, kc, :nt],
                                    rhs=WOUT[:, kc, half * 512:(half + 1) * 512], start=(kc == 0), stop=(kc == 7))
            return last
        S.op('pe', fwo, reads=['oT', 'wout'], writes=['psA'])
        S.op('dve', lambda e: e.tensor_tensor(out=hh[:nt], in0=psA_flat[:nt], in1=xt[:nt], op=ALU.add),
             reads=['psA', xtk], writes=['hh'])
        S.dma(out=hs[mt * 128:mt * 128 + nt, :], in_=hh[:nt], reads=['hh'], writes=[('hs', mt)])
        if kind == 'main' and CUT <= 6:
            return
        S.op('act', lambda e: e.activation(out=junk[:nt], in_=hh[:nt], func=AF.Square, accum_out=ST[:nt, 28:29]),
             reads=['hh'], writes=['junk', 'st'])
        rstd(nt, ST[:nt, 28:29], ST[:nt, 29:30], 1.0 / D, 'st')
        S.op('dve', lambda e: e.tensor_scalar(out=hn[:nt], in0=hh[:nt], scalar1=ST[:nt, 29:30], scalar2=None, op0=ALU.mult),
             reads=['hh', 'st'], writes=['hn'])
        transposes_bf(nt, hn, hnT, 'hn', 'hnT', interleaved=True)
        S.dma(out=hnts[:, mt, :, :nt], in_=hnT[:, :, :nt], reads=['hnT'], writes=[('hnts', mt)])
        if kind == 'main' and CUT <= 7:
            return

        def frt(e):
            last = None
            for kc in range(8):
                last = e.matmul(psE[:nt, 0:36], lhsT=hnT[:, kc, :nt], rhs=WR[:, kc, :], start=(kc == 0), stop=(kc == 7))
            return last
        S.op('pe', frt, reads=['hnT', 'wr'], writes=['psE'])
        L = RL[:nt, 0:36]
        S.op('dve', lambda e: e.tensor_tensor(out=L, in0=psE[:nt, 0:36], in1=BR[:nt], op=ALU.add), reads=['psE', 'br'],
             writes=['rl'])
        RQ = DQ if kind == 'main' else S
        RQ.op('dve', lambda e: e.tensor_reduce(out=RL[:nt, 36:37], in_=RL[:nt, 0:4], axis=AX.X, op=ALU.max), reads=['rl'],
             writes=['rl'])
        RQ.op('dve', lambda e: e.tensor_scalar(out=RL[:nt, 40:44], in0=RL[:nt, 0:4], scalar1=RL[:nt, 36:37], scalar2=None,
                                              op0=ALU.is_equal), reads=['rl'], writes=['rl'])
        RQ.op('dve', lambda e: e.tensor_scalar(out=RL[:nt, 37:38], in0=RL[:nt, 36:37], scalar1=-1.0, scalar2=None,
                                              op0=ALU.mult), reads=['rl'], writes=['rl'])
        RQ.op('act', lambda e: e.activation(out=RL[:nt, 44:48], in_=RL[:nt, 0:4], func=AF.Exp, bias=RL[:nt, 37:38], scale=1.0,
                                           accum_out=RL[:nt, 38:39]), reads=['rl'], writes=['rl'])
        RQ.op('dve', lambda e: e.reciprocal(out=RL[:nt, 39:40], in_=RL[:nt, 38:39]), reads=['rl'], writes=['rl'])
        RQ.op('dve', lambda e: e.tensor_tensor(
            out=RL[:nt, 48:80].rearrange("p (g x) -> p g x", x=8), in0=RL[:nt, 4:36].rearrange("p (g x) -> p g x", x=8),
            in1=RL[:nt, 40:44].unsqueeze(2).to_broadcast([nt, 4, 8]), op=ALU.mult), reads=['rl'], writes=['rl'])
        RQ.op('dve', lambda e: e.tensor_reduce(out=RL[:nt, 80:88], in_=RL[:nt, 48:80].rearrange("p (g x) -> p x g", x=8),
                                              axis=AX.X, op=ALU.add), reads=['rl'], writes=['rl'])
        RQ.op('dve', lambda e: e.max(out=RL[:nt, 88:96], in_=RL[:nt, 80:88]), reads=['rl'], writes=['rl'])
        RQ.op('dve', lambda e: e.tensor_tensor(out=ST[:nt, 40:41], in0=RL[:nt, 89:90], in1=RL[:nt, 88:89], op=ALU.subtract),
             reads=['rl'], writes=['st'])
        RQ.op('act', lambda e: e.activation(out=ST[:nt, 41:42], in_=ST[:nt, 40:41], func=AF.Exp), reads=['st'], writes=['st'])
        RQ.op('dve', lambda e: e.tensor_scalar(out=ST[:nt, 41:42], in0=ST[:nt, 41:42], scalar1=1.0, scalar2=None, op0=ALU.add),
             reads=['st'], writes=['st'])
        RQ.op('dve', lambda e: e.reciprocal(out=ST[:nt, 42:43], in_=ST[:nt, 41:42]), reads=['st'], writes=['st'])
        RQ.op('dve', lambda e: e.tensor_scalar(out=ST[:nt, 43:44], in0=ST[:nt, 42:43], scalar1=-1.0, scalar2=1.0, op0=ALU.mult,
                                              op1=ALU.add), reads=['st'], writes=['st'])
        RQ.op('dve', lambda e: e.tensor_tensor(out=ST[:nt, 42:44], in0=ST[:nt, 42:44],
                                              in1=RL[:nt, 39:40].to_broadcast([nt, 2]), op=ALU.mult), reads=['st', 'rl'],
             writes=['st'])
        RQ.op('dve', lambda e: e.tensor_scalar(out=ST[:nt, 44:52], in0=RL[:nt, 80:88], scalar1=RL[:nt, 88:89],
                                              scalar2=ST[:nt, 42:43], op0=ALU.is_equal, op1=ALU.mult), reads=['rl', 'st'],
             writes=['st'])
        RQ.op('dve', lambda e: e.tensor_scalar(out=ST[:nt, 52:60], in0=RL[:nt, 80:88], scalar1=RL[:nt, 89:90],
                                              scalar2=ST[:nt, 43:44], op0=ALU.is_equal, op1=ALU.mult), reads=['rl', 'st'],
             writes=['st'])
        RQ.op('dve', lambda e: e.tensor_tensor(out=ST[:nt, 44:52], in0=ST[:nt, 44:52], in1=ST[:nt, 52:60], op=ALU.add),
             reads=['st'], writes=['st'])
        RQ.op('dve', lambda e: e.tensor_tensor(
            out=G[:nt, mt, :].rearrange("p (g x) -> p g x", x=8),
            in0=RL[:nt, 40:44].unsqueeze(2).to_broadcast([nt, 4, 8]),
            in1=ST[:nt, 44:52].unsqueeze(1).to_broadcast([nt, 4, 8]), op=ALU.mult), reads=['rl', 'st'], writes=['G'])

    def sample_attention():
        nt = NSAMP
        qtok = kout[:nt, 0:512]
        ktok = kout[:nt, 512:1024]
        rows = [(1920, 1), (1536, 4), (0, 16)]
        for b in range(NSAMP):
            S.op('dve', lambda e, b=b: e.tensor_scalar(out=gsq[:nt], in0=qtok, scalar1=identf[:nt, b:b + 1], scalar2=None,
                                                        op0=ALU.mult), reads=['kout', 'identf'], writes=['gsq'])

            def fqb(e, b=b):
                return e.matmul(psE[:, 0:512], lhsT=ONES16[0:16, :], rhs=gsq[:nt], start=True, stop=True)
            S.op('pe', fqb, reads=['gsq', 'ones16'], writes=['psE'])
            pz = PZ[b % 2]
            pzk = ('pz', b % 2)
            S.op('pool', lambda e, pz=pz: e.memset(pz, 0.0), writes=[pzk])
            for c, (r0, stp) in enumerate(rows):
                bi = (b * 3 + c) % 2
                kc_t, vc_t = KC[bi], VC[bi]
                S.dma(out=kc_t, in_=ck[b, r0:r0 + 128 * stp:stp, :], writes=[('kc', bi)])
                S.dma(out=vc_t, in_=cv[b, r0:r0 + 128 * stp:stp, :], writes=[('vc', bi)])
                S.op('dve', lambda e, kc_t=kc_t: e.tensor_tensor(out=prod, in0=kc_t, in1=psE[:, 0:512], op=ALU.mult),
                     reads=[('kc', bi), 'psE'], writes=['prod'])
                S.op('dve', lambda e, c=c: e.tensor_reduce(out=SALL[:, c * 8:(c + 1) * 8],
                                                           in_=prod.rearrange("p (h d) -> p h d", d=64), axis=AX.X,
                                                           op=ALU.add), reads=['prod'], writes=['sall'])
                vi = (b % 2) * 3 + c
                S.op('act', lambda e, vi=vi, vc_t=vc_t: e.copy(out=VAS[vi][:, :, 0:64],
                                                               in_=vc_t.rearrange("p (h d) -> p h d", d=64)),
                     reads=[('vc', bi), ('vas', vi)], writes=[('vas', vi)])
            S.op('act', lambda e, pz=pz, b=b: e.activation(out=pz[:, :, b], in_=SALL[:, 0:24], func=AF.Exp, scale=0.125),
                 reads=['sall', pzk], writes=[pzk])

            def fpv(e, b=b, pz=pz):
                last = None
                for h in range(8):
                    for c in range(3):
                        vi = (b % 2) * 3 + c
                        last = e.matmul(psO[:nt, h // 4, (h % 4) * 65:(h % 4) * 65 + 65], lhsT=pz[:, c * 8 + h, :],
                                        rhs=VAS[vi][:, h, :], start=(b == 0 and c == 0 and h % 4 == 0),
                                        stop=(b == NSAMP - 1 and c == 2 and h % 4 == 3), skip_group_check=True)
                return last
            S.op('pe', fpv, reads=[pzk] + [('vas', (b % 2) * 3 + c) for c in range(3)], writes=['psC'])
        S.op('dve', lambda e: e.tensor_tensor(out=prod[:nt], in0=qtok, in1=ktok, op=ALU.mult), reads=['kout'], writes=['prod'])
        S.op('dve', lambda e: e.tensor_reduce(out=ST[:nt, 32:40], in_=prod[:nt].rearrange("p (h d) -> p h d", d=64),
                                              axis=AX.X, op=ALU.add), reads=['prod'], writes=['st'])
        S.op('act', lambda e: e.activation(out=ST[:nt, 32:40], in_=ST[:nt, 32:40], func=AF.Exp, scale=0.125), reads=['st'],
             writes=['st'])
        S.op('dve', lambda e: e.tensor_scalar(out=ST[:nt, 32:40], in0=ST[:nt, 32:40], scalar1=3.0, scalar2=None,
                                              op0=ALU.mult), reads=['st'], writes=['st'])
        es = ST[:nt, 32:40].rearrange("p (b h) -> p b h", b=2)
        dn = ST[:nt, 48:56].rearrange("p (b h) -> p b h", b=2)
        S.op('dve', lambda e: e.tensor_tensor(out=dn, in0=psO_den[:nt], in1=es, op=ALU.add), reads=['psC', 'st'],
             writes=['st'])
        S.op('dve', lambda e: e.reciprocal(out=dn, in_=dn), reads=['st'], writes=['st'])
        S.op('dve', lambda e: e.tensor_tensor(out=v4(ob, nt), in0=v4(vout, nt),
                                              in1=es.unsqueeze(3).to_broadcast([nt, 2, 4, 64]), op=ALU.mult),
             reads=['vout', 'st'], writes=['ob'])
        S.op('dve', lambda e: e.tensor_tensor(out=v4(ob, nt), in0=v4(ob, nt), in1=psO_num[:nt], op=ALU.add),
             reads=['ob', 'psC'], writes=['ob'])
        S.op('dve', lambda e: e.tensor_tensor(out=v4(ob, nt), in0=v4(ob, nt),
                                              in1=dn.unsqueeze(3).to_broadcast([nt, 2, 4, 64]), op=ALU.mult),
             reads=['ob', 'st'], writes=['ob'])

    import os
    UPTO = int(os.environ.get("K_UPTO", "9"))
    if UPTO >= 1:
        for g in range(NHALO):
            do_tile(g, 'halo')
    if UPTO >= 2:
        for g in range(NHALO, NHALO + NMAIN):
            do_tile(g, 'main')
    if UPTO >= 3:
        do_tile(NHALO + NMAIN, 'sample')
    if UPTO < 4:
        DQ.flush()
        S.finish()
        return nc

    DQ.flush()
    S.barrier()
    ar.off = persist_end
    NG = 17
    ACC = ar.alloc([NG, 1024], F32)
    HNT = ar.alloc([NG, 8, 128], BF16)
    WGf = [ar.alloc([8, 256], F32) for _ in range(2)]
    WUf = [ar.alloc([8, 256], F32) for _ in range(2)]
    WDf = [ar.alloc([2, 1024], F32) for _ in range(2)]
    WGb = [ar.alloc([8, 256], BF16) for _ in range(2)]
    WUb = [ar.alloc([8, 256], BF16) for _ in range(2)]
    WDb = [ar.alloc([2, 1024], BF16) for _ in range(2)]
    SIL2 = [ar.alloc([2, 256], F32) for _ in range(2)]
    HID2 = [ar.alloc([2, 256], BF16) for _ in range(2)]
    psAB2 = [psB_flat.rearrange("p (q n) -> p q n", n=256), psC.rearrange("p b (q n) -> p (b q) n", n=256)]
    psAB = [psC[:, 0, :].rearrange("p (a b) -> p a b", b=128), psC[:, 1, :].rearrange("p (a b) -> p a b", b=128)]
    psD = [psA_flat, psB_flat]
    psDk = ['psA', 'psB']
    it = 0
    for (t0, t1) in GROUPS:
        for t in range(t0, t1):
            nt = NSAMP if t == NMAIN else 128
            j = t - t0
            S.dma(out=ACC[:nt, j, :], in_=hs[t * 128:t * 128 + nt, :], writes=[('acc', j, 0), ('acc', j, 1)])
            S.dma(out=HNT[:, j, :, :nt], in_=hnts[:, t, :, :nt], writes=[('hnt', j)])
        def prep(ex):
            wb = ex % 2
            S.dma(out=WGf[wb], in_=w_gate[ex].rearrange("(p kc) f -> p kc f", kc=8), writes=[('wgf', wb)])
            S.dma(out=WUf[wb], in_=w_up[ex].rearrange("(p kc) f -> p kc f", kc=8), writes=[('wuf', wb)])
            S.dma(out=WDf[wb], in_=w_down[ex].rearrange("(kc p) f -> p kc f", p=128), writes=[('wdf', wb)])
            S.op('dve', lambda e, wb=wb: e.tensor_tensor(out=WGb[wb], in0=WGf[wb],
                                                          in1=g2c.unsqueeze(2).to_broadcast([128, 8, 256]), op=ALU.mult),
                 reads=[('wgf', wb)], writes=[('wgb', wb)])
            S.op('dve', lambda e, wb=wb: e.tensor_tensor(out=WUb[wb], in0=WUf[wb],
                                                          in1=g2c.unsqueeze(2).to_broadcast([128, 8, 256]), op=ALU.mult),
                 reads=[('wuf', wb)], writes=[('wub', wb)])
            S.op('act', lambda e, wb=wb: e.copy(out=WDb[wb], in_=WDf[wb]), reads=[('wdf', wb)], writes=[('wdb', wb)])

        tl = list(range(t0, t1))
        batches = []
        while tl:
            if len(tl) >= 2 and tl[1] != NMAIN and tl[0] != NMAIN:
                batches.append(tl[:2]); tl = tl[2:]
            else:
                batches.append(tl[:1]); tl = tl[1:]
        units = [(ex, bt) for ex in range(32) for bt in batches]

        def gu(u):
            ex, bt = units[u]
            wb = ex % 2
            j = bt[0] - t0
            nb = len(bt)
            ncol = 128 * nb if bt[0] != NMAIN else NSAMP
            pab = psAB2[u % 2]
            if nb == 2:
                rhs_of = lambda kc: HNT[:, j:j + 2, kc, :]
                out_of = lambda q: pab[:, q, :].rearrange("p (a b) -> p a b", b=128)
            else:
                rhs_of = lambda kc: HNT[:, j, kc, :ncol]
                out_of = lambda q: pab[:, q, :ncol]

            def fgu(e):
                last = None
                for m, W in enumerate((WGb[wb], WUb[wb])):
                    for fcx in range(2):
                        for kc in range(8):
                            last = e.matmul(out_of(m * 2 + fcx), lhsT=W[:, kc, fcx * 128:(fcx + 1) * 128],
                                            rhs=rhs_of(kc), start=(kc == 0), stop=(kc == 7))
                return last
            S.op('pe', fgu, reads=[('wgb', wb), ('wub', wb)] + [('hnt', t - t0) for t in bt], writes=[('psab', u % 2)])

        prep(0)
        gu(0)
        hcnt = 0
        for u, (ex, bt) in enumerate(units):
            wb = ex % 2
            pb = u % 2
            pab = psAB2[pb]
            pabk = ('psab', pb)
            ncol = 128 * len(bt) if bt[0] != NMAIN else NSAMP
            if bt[0] == t0 and ex + 1 < 32:
                prep(ex + 1)
            if u + 1 < len(units):
                gu(u + 1)
            S.op('act', lambda e, pb=pb, ncol=ncol, pab=pab: e.activation(out=SIL2[pb][:, :, :ncol], in_=pab[:, 0:2, :ncol],
                                                                           func=AF.Silu), reads=[pabk], writes=[('sil', pb)])
            S.op('dve', lambda e, pb=pb, ncol=ncol, pab=pab: e.tensor_tensor(out=HID2[pb][:, :, :ncol],
                                                                              in0=SIL2[pb][:, :, :ncol],
                                                                              in1=pab[:, 2:4, :ncol], op=ALU.mult),
                 reads=[pabk, ('sil', pb)], writes=[('hid', pb)])
            for bi, t in enumerate(bt):
                nt = NSAMP if t == NMAIN else 128
                j = t - t0
                c0 = bi * 128
                for half in range(2):
                    hb = hcnt % 2
                    hcnt += 1
                    pdh = psA_flat[:, hb * 512:(hb + 1) * 512]
                    pdk = ('psd', hb)

                    def fdn(e, wb=wb, pb=pb, nt=nt, pdh=pdh, half=half, c0=c0):
                        last = None
                        for fcx in range(2):
                            last = e.matmul(pdh[:nt, :], lhsT=HID2[pb][:, fcx, c0:c0 + nt],
                                            rhs=WDb[wb][:, fcx, half * 512:(half + 1) * 512], start=(fcx == 0),
                                            stop=(fcx == 1))
                        return last
                    S.op('pe', fdn, reads=[('hid', pb), ('wdb', wb)], writes=[pdk])
                    S.op('dve', lambda e, j=j, nt=nt, pdh=pdh, t=t, ex=ex, half=half: e.scalar_tensor_tensor(
                        out=ACC[:nt, j, half * 512:(half + 1) * 512], in0=pdh[:nt, :], scalar=G[:nt, t, ex:ex + 1],
                        in1=ACC[:nt, j, half * 512:(half + 1) * 512], op0=ALU.mult, op1=ALU.add),
                        reads=[pdk, ('acc', j, half)], writes=[('acc', j, half)])
        for t in range(t0, t1):
            j = t - t0
            if t == NMAIN:
                S.dma(out=y_s, in_=ACC[:NSAMP, j, :], reads=[('acc', j, 0), ('acc', j, 1)])
            else:
                S.dma(out=y_p[t * 128:(t + 1) * 128, :], in_=ACC[:, j, :], reads=[('acc', j, 0), ('acc', j, 1)])
    S.finish()
    return nc


_NC_CACHE = {}


def _consts():
    ident = np.eye(128, dtype=np.float32)
    p = np.arange(128)
    bones = (p[:, None] // 64 == p[None, :] // 64).astype(np.float32)
    tri = (p[:, None] <= p[None, :]).astype(np.float32)
    mult = np.zeros((128, NOFF, 128), np.float32)
    for o in range(NOFF):
        delta = 128 * o + p[None, :] - p[:, None]
        m = ((delta >= 0) & (delta <= 128)).astype(np.float32)
        m += ((delta >= 0) & (delta <= 512) & (delta % 4 == 0)).astype(np.float32)
        m += ((delta >= 0) & (delta <= 2048) & (delta % 16 == 0)).astype(np.float32)
        mult[:, o, :] = m
    sel = np.zeros((16, 16, 128), np.float32)
    for b in range(16):
        sel[b, b, :] = 1.0
    return ident, bones, tri, mult, sel


def kernel(x_prompt, x_sample, cache_k, cache_v, norm1_g, w_in, q_gain, k_gain, v_gain,
           w_spatial, b_spatial, out_gain_a, out_gain_b, w_out, norm2_g, w_router1, b_router1,
           w_router2, b_router2, w_up, w_gate, w_down):
    f = lambda a: np.ascontiguousarray(np.asarray(a, dtype=np.float32))
    x_prompt, x_sample = f(x_prompt), f(x_sample)
    cache_k, cache_v = np.asarray(cache_k, dtype=np.float32), np.asarray(cache_v, dtype=np.float32)
    if 'nc' not in _NC_CACHE:
        _NC_CACHE['nc'] = build_nc()
    nc = _NC_CACHE['nc']
    ident, bones, tri, mult, sel = _consts()
    col = lambda v: f(np.asarray(v).reshape(8, 128).T)
    g1c, g2c = col(norm1_g[0]), f(np.asarray(norm2_g[0]).reshape(128, 8))
    ogc = col(np.concatenate([np.asarray(out_gain_a[0]), np.asarray(out_gain_b[0])]))
    qg = np.asarray(q_gain[0]).reshape(4, 128).T
    kg = np.asarray(k_gain[0]).reshape(4, 128).T
    gqk = f(np.concatenate([qg, kg], axis=1))
    vgain = f(np.broadcast_to(np.asarray(v_gain[0]).reshape(1, 512), (128, 512)))
    bst = f(np.asarray(b_spatial[0]).T)
    ws00 = f(np.broadcast_to(np.asarray(w_spatial[0])[:, 0, 0].reshape(1, 8), (128, 8)))
    bs0 = f(np.broadcast_to(np.asarray(b_spatial[0])[:, 0].reshape(1, 8), (128, 8)))
    br = f(np.broadcast_to(np.concatenate([np.asarray(b_router1[0]).reshape(4), np.asarray(b_router2[0]).reshape(32)]
                                          ).reshape(1, 36), (128, 36)))
    wsT = f(np.transpose(np.asarray(w_spatial[0]), (2, 0, 1)))
    w_r = f(np.concatenate([np.asarray(w_router1[0]),
                            np.transpose(np.asarray(w_router2[0]), (1, 0, 2)).reshape(D, 32)], axis=1))
    wg = f(np.asarray(w_gate[0]).reshape(32, D, 256))
    wu = f(np.asarray(w_up[0]).reshape(32, D, 256))
    wd = f(np.asarray(w_down[0]).reshape(32, 256, D))
    w_in0, w_out0 = f(w_in[0]), f(w_out[0])
    in_maps = []
    for c in range(NCORES):
        b, hf = c // 2, c % 2
        xp = np.zeros((48 * 128, D), np.float32)
        if hf == 1:
            xp[:2048] = x_prompt[b, 2048:4096]
        xp[2048:] = x_prompt[b, hf * 4096:(hf + 1) * 4096]
        in_maps.append({
            "xp": xp, "xs": f(x_sample[16 * c:16 * c + 16, 0]),
            "ck": f(cache_k[0, 16 * c:16 * c + 16].reshape(16, 2048, 512)),
            "cv": f(cache_v[0, 16 * c:16 * c + 16].reshape(16, 2048, 512)),
            "w_in": w_in0, "w_out": w_out0, "w_r": w_r, "w_gate": wg, "w_up": wu, "w_down": wd, "wsT": wsT,
            "g1c": g1c, "g2c": g2c, "ogc": ogc, "gqk": gqk, "vgain": vgain, "bst": bst, "ws00": ws00, "bs0": bs0,
            "br": br, "flag": np.full((128, 1), float(hf), np.float32),
            "c_ident": ident, "c_bones": bones, "c_tri": tri, "c_mult": mult,
        })
    res = run_bass_kernel_spmd(nc, in_maps, core_ids=list(range(NCORES)))
    R = res.results
    B, T = 4, 8192
    y_prompt = np.zeros((B, T, D), np.float32)
    y_sample = np.zeros((128, 1, D), np.float32)
    nkp = np.zeros((1, B, 2048, 8, 64), np.float32)
    nvp = np.zeros((1, B, 2048, 8, 64), np.float32)
    nks = np.zeros((1, 128, 1, 8, 64), np.float32)
    nvs = np.zeros((1, 128, 1, 8, 64), np.float32)
    nva = np.zeros((1, 128, 1, 8, 64), np.float32)
    for c in range(NCORES):
        b, hf = c // 2, c % 2
        r = R[c]
        y_prompt[b, hf * 4096:(hf + 1) * 4096] = r["y_p"]
        y_sample[16 * c:16 * c + 16, 0] = r["y_s"]
        if hf == 1:
            nkp[0, b] = r["nk"].reshape(2048, 8, 64)
            nvp[0, b] = r["nv"].reshape(2048, 8, 64)
        nks[0, 16 * c:16 * c + 16, 0] = r["nks"].reshape(16, 8, 64)
        nvs[0, 16 * c:16 * c + 16, 0] = r["nvs"].reshape(16, 8, 64)
        nva[0, 16 * c:16 * c + 16, 0] = r["nva"].reshape(16, 8, 64)
    return (y_prompt, y_sample, nkp, nvp, nks, nvs, nva)
```
